# Optimizing a Trainium2 kernel written in Bass

```python
import math
import jax
import jax.numpy as jnp
from jax import lax
import numpy as np

D_MODEL = 1024
BATCH = 8
SEQ = 4096
DEPTH = 4

GRID_W = 64
CTX_LEN = 256
EPS = 1e-6
FOURIER_GROUPS = 4
FOURIER_GROUP_W = 64
FOURIER_WIDTH = FOURIER_GROUPS * FOURIER_GROUP_W
HYENA_WIDTH = 256
HYENA_SHORT = 3
HYENA_BANDS = 16
HYENA_EMB = 1 + 2 * HYENA_BANDS
HYENA_FFN = 64
HYENA_MIN_DECAY = -math.log(1e-2) / 1.5
HYENA_MAX_DECAY = -math.log(1e-2) / 0.3
POOL_WINDOWS = (2, 4, 8, 16)
POOL_GROUP_W = 64
POOL_WIDTH = len(POOL_WINDOWS) * POOL_GROUP_W
SSM_GROUPS = 2
HEADS_PER_GROUP = 4
SSM_HEADS = SSM_GROUPS * HEADS_PER_GROUP
SSM_HEAD_DIM = 64
SSM_WIDTH = SSM_HEADS * SSM_HEAD_DIM
SSM_STATE = 128
SSM_CONV = 3
SSM_CHUNK = 128
SSM_CONV_CH = SSM_WIDTH + 2 * SSM_GROUPS * SSM_STATE
SSM_IN = SSM_WIDTH + SSM_CONV_CH + 2 * SSM_HEADS
IN_WIDTH = FOURIER_WIDTH + 3 * HYENA_WIDTH + POOL_WIDTH + SSM_IN
IN_SPLITS = (FOURIER_WIDTH, FOURIER_WIDTH + 3 * HYENA_WIDTH, FOURIER_WIDTH + 3 * HYENA_WIDTH + POOL_WIDTH)
N_BRANCHES = 4
BRANCH_WIDTH = FOURIER_WIDTH + HYENA_WIDTH + POOL_WIDTH + SSM_WIDTH
BRANCH_SPLITS = (FOURIER_WIDTH, FOURIER_WIDTH + HYENA_WIDTH, FOURIER_WIDTH + HYENA_WIDTH + POOL_WIDTH)
N_EXPERTS = 32
TOP_K = 4
EXPERT_FF = 1024
SWIGLU_LIMIT = 7.0
SWIGLU_ALPHA = 1.702
MOE_BLOCK = 128

kernel_name = 'hybrid_fourier_hyena_pool_ssd_moe_dit'


def rmsnorm(x, g):
    xf = x.astype(jnp.float32)
    y = xf * lax.rsqrt(jnp.mean(xf * xf, axis=-1, keepdims=True) + EPS)
    return y.astype(x.dtype) * g


def dwconv_centred(x, w, b):
    k_w = w.shape[0]
    n = x.shape[1]
    xp = jnp.pad(x, ((0, 0), ((k_w - 1) // 2, k_w // 2), (0, 0)))
    return sum(w[k] * xp[:, k:k + n] for k in range(k_w)) + b


def fourier_mix(u):
    b, n, _ = u.shape
    ug = u.reshape(b, n, FOURIER_GROUPS, FOURIER_GROUP_W).astype(jnp.float32)
    y = jnp.fft.fft2(ug, axes=(1, 3), norm='ortho').real
    return y.reshape(b, n, FOURIER_WIDTH).astype(u.dtype)


def implicit_filter(n, lp):
    pos = jnp.arange(n, dtype=jnp.float32)
    t = pos / (n - 1)
    ang = 2.0 * math.pi * pos / n
    freqs = jnp.linspace(1e-4, HYENA_BANDS - 1, HYENA_BANDS, dtype=jnp.float32)
    feats = jnp.concatenate([t[:, None], jnp.cos(ang[:, None] * freqs), -jnp.sin(ang[:, None] * freqs)], axis=-1)
    hdn = jnp.sin(feats @ lp['hy_ffn_w1'] + lp['hy_ffn_b1'])
    hdn = jnp.sin(hdn @ lp['hy_ffn_w2'] + lp['hy_ffn_b2'])
    k = (hdn @ lp['hy_ffn_w3']).astype(jnp.float32).reshape(n, 2, HYENA_WIDTH)
    deltas = jnp.linspace(HYENA_MIN_DECAY, HYENA_MAX_DECAY, HYENA_WIDTH, dtype=jnp.float32)
    k = k * jnp.exp(-t[:, None, None] * deltas)
    k_two = jnp.concatenate([k[:, 0], jnp.zeros((1, HYENA_WIDTH), jnp.float32), jnp.flip(k[1:, 1], axis=0)], axis=0)
    return k_two / jnp.sum(jnp.abs(k_two), axis=0, keepdims=True)


def hyena_mix(u, lp):
    b, n, _ = u.shape
    u = dwconv_centred(u, lp['hy_conv_w'], lp['hy_conv_b'])
    x1, x2, v = jnp.split(u, 3, axis=-1)
    k_two = implicit_filter(n, lp)
    vv = (x2 * v).astype(jnp.float32)
    n_fft = 2 * n
    y = jnp.fft.irfft(jnp.fft.rfft(vv, n=n_fft, axis=1) * jnp.fft.rfft(k_two, n=n_fft, axis=0)[None], n=n_fft, axis=1)[:, :n]
    y = y + vv * lp['hy_bias']
    return (x1 * y).astype(u.dtype)


def pool_mix(u, lp, n_rows):
    b, n, cw = u.shape
    row_len = n // n_rows
    ug = u.reshape(b, n_rows, row_len, len(POOL_WINDOWS), POOL_GROUP_W)
    cs = jnp.pad(jnp.cumsum(ug.astype(jnp.float32), axis=2), ((0, 0), (0, 0), (1, 0), (0, 0), (0, 0)))
    pos = jnp.arange(row_len)
    means = []
    for gi, win in enumerate(POOL_WINDOWS):
        lo = jnp.clip(pos - win // 2, 0, row_len)
        hi = jnp.clip(pos + win // 2, 0, row_len)
        csg = cs[:, :, :, gi]
        means.append((csg[:, :, hi] - csg[:, :, lo]) / (hi - lo).astype(jnp.float32)[:, None])
    pooled = jnp.stack(means, axis=3) - ug
    mixed = jnp.einsum('brwgc,gcd->brwgd', pooled, lp['pool_w'])
    return mixed.reshape(b, n, cw) * lp['pool_scale']


def ssd_scan(xs, dt, a, bm, cm, h0):
    b, n_tok, g, r, p = xs.shape
    n_st = bm.shape[-1]
    q = SSM_CHUNK
    nc = n_tok // q
    xs = xs.reshape(b, nc, q, g, r, p)
    dt = dt.reshape(b, nc, q, g, r)
    bm = bm.reshape(b, nc, q, g, n_st)
    cm = cm.reshape(b, nc, q, g, n_st)
    acs = jnp.cumsum(dt * a, axis=2)
    seg = acs[:, :, :, None] - acs[:, :, None, :]
    mask = jnp.tril(jnp.ones((q, q), bool))[:, :, None, None]
    decay_in = jnp.exp(jnp.where(mask, seg, -jnp.inf))
    scores = jnp.einsum('bcign,bcjgn->bcijg', cm, bm)
    xdt = xs * dt[..., None]
    y_diag = jnp.einsum('bcijgr,bcjgrp->bcigrp', scores[..., None] * decay_in, xdt)
    to_end = jnp.exp(acs[:, :, -1:] - acs)
    chunk_state = jnp.einsum('bcjgn,bcjgr,bcjgrp->bcgrpn', bm, to_end, xdt)
    chunk_decay = jnp.exp(acs[:, :, -1])

    def carry_step(h, inp):
        s, d = inp
        return h * d[..., None, None] + s, h

    h_last, h_enter = lax.scan(carry_step, h0, (jnp.moveaxis(chunk_state, 1, 0), jnp.moveaxis(chunk_decay, 1, 0)))
    h_enter = jnp.moveaxis(h_enter, 0, 1)
    y_off = jnp.einsum('bcign,bcgrpn,bcigr->bcigrp', cm, h_enter, jnp.exp(acs))
    return (y_diag + y_off).reshape(b, n_tok, g, r, p), h_last


def ssd_mix(u, lp, init_f, init_b):
    b, n, _ = u.shape
    z, xbc, dt_raw = jnp.split(u, [SSM_WIDTH, SSM_WIDTH + SSM_CONV_CH], axis=-1)
    xbc = jax.nn.silu(dwconv_centred(xbc, lp['ssm_conv_w'], lp['ssm_conv_b']))
    xs, bm, cm = jnp.split(xbc, [SSM_WIDTH, SSM_WIDTH + SSM_GROUPS * SSM_STATE], axis=-1)
    xs = xs.reshape(b, n, SSM_GROUPS, HEADS_PER_GROUP, SSM_HEAD_DIM)
    bm = bm.reshape(b, n, SSM_GROUPS, SSM_STATE)
    cm = cm.reshape(b, n, SSM_GROUPS, SSM_STATE)
    dt = jax.nn.softplus(dt_raw.astype(jnp.float32).reshape(b, n, 2, SSM_GROUPS, HEADS_PER_GROUP)
                         + lp['ssm_dt_bias'].astype(jnp.float32).reshape(2, SSM_GROUPS, HEADS_PER_GROUP))
    a = -jnp.exp(lp['ssm_a_log'].astype(jnp.float32)).reshape(2, SSM_GROUPS, HEADS_PER_GROUP)
    flip = lambda t: jnp.flip(t, axis=1)
    y_f, s_f = ssd_scan(xs, dt[:, :, 0], a[0], bm, cm, init_f)
    y_b, s_b = ssd_scan(flip(xs), flip(dt[:, :, 1]), a[1], flip(bm), flip(cm), init_b)
    y = y_f + flip(y_b) + xs * lp['ssm_d'].reshape(SSM_GROUPS, HEADS_PER_GROUP)[:, :, None]
    y = y.reshape(b, n, SSM_WIDTH) * jax.nn.silu(z)
    return rmsnorm(y, lp['ssm_norm']), (s_f, s_b)


def token_mixer(h, lp, n_rows, init_f, init_b):
    proj = h @ lp['w_in']
    f_in, hy_in, pool_in, ssm_in = jnp.split(proj, IN_SPLITS, axis=-1)
    y_ssm, states = ssd_mix(ssm_in, lp, init_f, init_b)
    ys = (fourier_mix(f_in), hyena_mix(hy_in, lp), pool_mix(pool_in, lp, n_rows), y_ssm)
    w_br = jnp.split(lp['w_branch'], BRANCH_SPLITS, axis=0)
    merged = 0.0
    for k in range(N_BRANCHES):
        gate = jax.nn.sigmoid(h @ lp['w_gate'][k])
        merged = merged + gate * (ys[k] @ w_br[k])
    return merged @ lp['w_out'], states


def moe_ffn(h, lp):
    lead = h.shape[:-1]
    h = h.reshape(-1, D_MODEL)
    n_tok = h.shape[0]
    n_pairs = n_tok * TOP_K
    logits = (h @ lp['router_w'] + lp['router_b']).astype(jnp.float32)
    top_logit, top_e = lax.top_k(logits, TOP_K)
    top_w = jax.nn.softmax(top_logit, axis=-1)
    flat_e = top_e.reshape(-1)
    order = jnp.argsort(flat_e)
    e_sorted = flat_e[order]
    tok_sorted = (order // TOP_K).astype(jnp.int32)
    w_sorted = top_w.reshape(-1)[order]
    counts = jnp.bincount(flat_e, length=N_EXPERTS)
    padded = (counts + MOE_BLOCK - 1) // MOE_BLOCK * MOE_BLOCK
    start = jnp.cumsum(counts) - counts
    pad_end = jnp.cumsum(padded)
    pad_start = pad_end - padded
    dest = pad_start[e_sorted] + jnp.arange(n_pairs) - start[e_sorted]
    n_blk = -(-(n_pairs + N_EXPERTS * (MOE_BLOCK - 1)) // MOE_BLOCK)
    n_rows = n_blk * MOE_BLOCK
    row_tok = jnp.full((n_rows,), n_tok, jnp.int32).at[dest].set(tok_sorted)
    row_w = jnp.zeros((n_rows,), jnp.float32).at[dest].set(w_sorted)
    blk_e = jnp.minimum(jnp.searchsorted(pad_end, jnp.arange(n_blk) * MOE_BLOCK, side='right'), N_EXPERTS - 1)
    h_pad = jnp.concatenate([h, jnp.zeros((1, D_MODEL), h.dtype)], axis=0)
    w_up, b_up, w_down, b_down = lp['exp_w_up'], lp['exp_b_up'], lp['exp_w_down'], lp['exp_b_down']

    def expert_block(args):
        rows, wts, e = args
        hu = h_pad[rows] @ w_up[e] + b_up[e]
        glu = jnp.minimum(hu[:, :EXPERT_FF], SWIGLU_LIMIT)
        lin = jnp.clip(hu[:, EXPERT_FF:], -SWIGLU_LIMIT, SWIGLU_LIMIT)
        act = glu * jax.nn.sigmoid(SWIGLU_ALPHA * glu) * (lin + 1.0)
        y = act @ w_down[e] + b_down[e]
        return y * wts[:, None].astype(y.dtype)

    y_rows = lax.map(expert_block, (row_tok.reshape(n_blk, MOE_BLOCK), row_w.reshape(n_blk, MOE_BLOCK), blk_e))
    y = jax.ops.segment_sum(y_rows.reshape(n_rows, D_MODEL), row_tok, num_segments=n_tok + 1)[:n_tok]
    return y.reshape(*lead, D_MODEL)


def setup_inputs(seed: int = 0) -> dict:
    key = jax.random.key(seed)
    keys = iter(jax.random.split(key, 48))

    def nrm(shape, scale):
        return scale * jax.random.normal(next(keys), shape, jnp.float32)

    def gain(shape):
        return 1.0 + nrm(shape, 0.05)

    dt0 = jnp.exp(jax.random.uniform(next(keys), (DEPTH, 2, SSM_HEADS), jnp.float32, math.log(1e-3), math.log(1e-1)))
    a0 = jax.random.uniform(next(keys), (DEPTH, 2, SSM_HEADS), jnp.float32, 1.0, 16.0)
    branch_scale = jnp.concatenate([jnp.full((BRANCH_WIDTH - SSM_WIDTH, 1), FOURIER_WIDTH ** -0.5, jnp.float32),
                                    jnp.full((SSM_WIDTH, 1), SSM_WIDTH ** -0.5, jnp.float32)], axis=0)
    return {
        'x': nrm((BATCH, SEQ, D_MODEL), 1.0),
        'c': nrm((BATCH, D_MODEL), 1.0),
        'ctx': nrm((BATCH, CTX_LEN, D_MODEL), 1.0),
        'c_ctx': nrm((D_MODEL,), 1.0),
        'w_mod': nrm((DEPTH, D_MODEL, 6 * D_MODEL), 0.3 * D_MODEL ** -0.5),
        'b_mod': nrm((DEPTH, 6 * D_MODEL), 0.02),
        'norm_mix': gain((DEPTH, D_MODEL)),
        'norm_ffn': gain((DEPTH, D_MODEL)),
        'w_in': nrm((DEPTH, D_MODEL, IN_WIDTH), D_MODEL ** -0.5),
        'hy_conv_w': nrm((DEPTH, HYENA_SHORT, 3 * HYENA_WIDTH), HYENA_SHORT ** -0.5),
        'hy_conv_b': nrm((DEPTH, 3 * HYENA_WIDTH), 0.02),
        'hy_ffn_w1': nrm((DEPTH, HYENA_EMB, HYENA_FFN), HYENA_EMB ** -0.5),
        'hy_ffn_b1': nrm((DEPTH, HYENA_FFN), 0.02),
        'hy_ffn_w2': nrm((DEPTH, HYENA_FFN, HYENA_FFN), HYENA_FFN ** -0.5),
        'hy_ffn_b2': nrm((DEPTH, HYENA_FFN), 0.02),
        'hy_ffn_w3': nrm((DEPTH, HYENA_FFN, 2 * HYENA_WIDTH), HYENA_FFN ** -0.5),
        'hy_bias': nrm((DEPTH, HYENA_WIDTH), 0.5),
        'pool_w': nrm((DEPTH, len(POOL_WINDOWS), POOL_GROUP_W, POOL_GROUP_W), POOL_GROUP_W ** -0.5),
        'pool_scale': gain((DEPTH, POOL_WIDTH)),
        'ssm_conv_w': nrm((DEPTH, SSM_CONV, SSM_CONV_CH), SSM_CONV ** -0.5),
        'ssm_conv_b': nrm((DEPTH, SSM_CONV_CH), 0.02),
        'ssm_dt_bias': dt0 + jnp.log(-jnp.expm1(-dt0)),
        'ssm_a_log': jnp.log(a0),
        'ssm_d': 1.0 + nrm((DEPTH, SSM_HEADS), 0.1),
        'ssm_norm': gain((DEPTH, SSM_WIDTH)),
        'w_branch': nrm((DEPTH, BRANCH_WIDTH, D_MODEL), 1.0) * branch_scale,
        'w_gate': nrm((DEPTH, N_BRANCHES, D_MODEL, D_MODEL), D_MODEL ** -0.5),
        'w_out': nrm((DEPTH, D_MODEL, D_MODEL), D_MODEL ** -0.5),
        'router_w': nrm((DEPTH, D_MODEL, N_EXPERTS), D_MODEL ** -0.5),
        'router_b': nrm((DEPTH, N_EXPERTS), 0.01),
        'exp_w_up': nrm((DEPTH, N_EXPERTS, D_MODEL, 2 * EXPERT_FF), D_MODEL ** -0.5),
        'exp_b_up': nrm((DEPTH, N_EXPERTS, 2 * EXPERT_FF), 0.02),
        'exp_w_down': nrm((DEPTH, N_EXPERTS, EXPERT_FF, D_MODEL), EXPERT_FF ** -0.5),
        'exp_b_down': nrm((DEPTH, N_EXPERTS, D_MODEL), 0.02),
        'norm_final': gain((D_MODEL,)),
    }


def reference(x, c, ctx, c_ctx, w_mod, b_mod, norm_mix, norm_ffn, w_in, hy_conv_w, hy_conv_b,
              hy_ffn_w1, hy_ffn_b1, hy_ffn_w2, hy_ffn_b2, hy_ffn_w3, hy_bias, pool_w, pool_scale,
              ssm_conv_w, ssm_conv_b, ssm_dt_bias, ssm_a_log, ssm_d, ssm_norm, w_branch, w_gate, w_out,
              router_w, router_b, exp_w_up, exp_b_up, exp_w_down, exp_b_down, norm_final):
    batch, n_lat = x.shape[0], x.shape[1]
    rows = n_lat // GRID_W
    zero_state = jnp.zeros((batch, SSM_GROUPS, HEADS_PER_GROUP, SSM_HEAD_DIM, SSM_STATE), jnp.float32)
    silu_c = jax.nn.silu(c)
    silu_cc = jax.nn.silu(c_ctx)
    for layer in range(DEPTH):
        lp = {
            'w_in': w_in[layer], 'hy_conv_w': hy_conv_w[layer], 'hy_conv_b': hy_conv_b[layer],
            'hy_ffn_w1': hy_ffn_w1[layer], 'hy_ffn_b1': hy_ffn_b1[layer], 'hy_ffn_w2': hy_ffn_w2[layer],
            'hy_ffn_b2': hy_ffn_b2[layer], 'hy_ffn_w3': hy_ffn_w3[layer], 'hy_bias': hy_bias[layer],
            'pool_w': pool_w[layer], 'pool_scale': pool_scale[layer],
            'ssm_conv_w': ssm_conv_w[layer], 'ssm_conv_b': ssm_conv_b[layer], 'ssm_dt_bias': ssm_dt_bias[layer],
            'ssm_a_log': ssm_a_log[layer], 'ssm_d': ssm_d[layer], 'ssm_norm': ssm_norm[layer],
            'w_branch': w_branch[layer], 'w_gate': w_gate[layer], 'w_out': w_out[layer],
            'router_w': router_w[layer], 'router_b': router_b[layer],
            'exp_w_up': exp_w_up[layer], 'exp_b_up': exp_b_up[layer],
            'exp_w_down': exp_w_down[layer], 'exp_b_down': exp_b_down[layer],
        }
        mod_lat = (silu_c @ w_mod[layer] + b_mod[layer])[:, None, :]
        mod_ctx = (silu_cc @ w_mod[layer] + b_mod[layer])[None, None, :]
        sh1, sc1, g1, sh2, sc2, g2 = jnp.split(mod_lat, 6, axis=-1)
        csh1, csc1, cg1, csh2, csc2, cg2 = jnp.split(mod_ctx, 6, axis=-1)
        h_ctx = rmsnorm(ctx, norm_mix[layer]) * (1.0 + csc1) + csh1
        y_ctx, ctx_states = token_mixer(h_ctx, lp, 1, zero_state, zero_state)
        h_lat = rmsnorm(x, norm_mix[layer]) * (1.0 + sc1) + sh1
        y_lat, _ = token_mixer(h_lat, lp, rows, ctx_states[0], ctx_states[1])
        x = x + g1 * y_lat
        h_lat = rmsnorm(x, norm_ffn[layer]) * (1.0 + sc2) + sh2
        x = x + g2 * moe_ffn(h_lat, lp)
        if layer < DEPTH - 1:
            ctx = ctx + cg1 * y_ctx
            h_ctx = rmsnorm(ctx, norm_ffn[layer]) * (1.0 + csc2) + csh2
            ctx = ctx + cg2 * moe_ffn(h_ctx, lp)
    return rmsnorm(x, norm_final)
```

```python
import os
import math
import contextlib
import numpy as np
import ml_dtypes
import concourse.bass as bass
import concourse.mybir as mybir
from concourse.bass_utils import run_bass_kernel_spmd

F32 = mybir.dt.float32
BF16 = mybir.dt.bfloat16
I32 = mybir.dt.int32
AF = mybir.ActivationFunctionType
ALU = mybir.AluOpType
AX = mybir.AxisListType

SEM_ROT = 30000
DMA_SLOTS = 6

D = 1024
NLAT = 4096
NCTX = 256
NTOK = NLAT + NCTX
NT = NTOK // 128
DEPTH = 4
INW = 2832
NE = 32
CAP = 1280
ESTR = CAP + 8
BIG = 65536.0
EPS = 1e-6
HY_MIN = -math.log(1e-2) / 1.5
HY_MAX = -math.log(1e-2) / 0.3


class Prog:
    ENG = ("pe", "act", "dve", "pool", "sp")

    def __init__(self, nc):
        self.nc = nc
        self.q = {e: [] for e in self.ENG}
        self.cnt = {e: 0 for e in self.ENG}
        self.gen = {e: 0 for e in self.ENG}
        self.clock = {e: {} for e in self.ENG}
        self.res = {}
        self.semnames = []
        self.dma_slots = {e: [[self._newsem(f"dq_{e}_{i}"), 0] for i in range(DMA_SLOTS)]
                          for e in ("sp", "act", "pool")}
        self.dma_i = {e: 0 for e in ("sp", "act", "pool")}
        self.cursem = {e: self._newsem(f"s_{e}_0") for e in self.ENG}
        self.nops = 0

    def _newsem(self, name):
        self.semnames.append(name)
        return name

    def op(self, eng, fn, reads=(), writes=(), dma=False):
        deps = []
        for k in reads:
            r = self.res.get(k)
            if r is not None and r[0] is not None:
                deps.append(r[0])
        for k in writes:
            r = self.res.get(k)
            if r is not None:
                if r[0] is not None:
                    deps.append(r[0])
                deps.extend(r[1])
        clk = self.clock[eng]
        if dma:
            slots = self.dma_slots[eng]
            slot = slots[self.dma_i[eng] % len(slots)]
            self.dma_i[eng] += 1
            if slot[1] > 0:
                deps.append((slot[0], slot[1], None))
            if slot[1] + 16 > SEM_ROT:
                slot[0] = self._newsem(slot[0] + "r")
                slot[1] = 0
            slot[1] += 16
            ev_sem, ev_val, inc = slot[0], slot[1], 16
        else:
            if self.cnt[eng] + 1 > SEM_ROT:
                self.gen[eng] += 1
                self.cursem[eng] = self._newsem(f"s_{eng}_{self.gen[eng]}")
                self.cnt[eng] = 0
            self.cnt[eng] += 1
            ev_sem, ev_val, inc = self.cursem[eng], self.cnt[eng], 1
        waits = {}
        for (s, v, c) in deps:
            if eng == "pe" and s.startswith("s_pe_"):
                continue
            if clk.get(s, 0) >= v:
                continue
            if waits.get(s, 0) < v:
                waits[s] = v
        for (s, v, c) in deps:
            if s in waits:
                if c:
                    for ks, kv in c.items():
                        if clk.get(ks, 0) < kv:
                            clk[ks] = kv
                if clk.get(s, 0) < v:
                    clk[s] = v
        evclk = dict(clk)
        evclk[ev_sem] = ev_val
        ev = (ev_sem, ev_val, evclk)
        self.q[eng].append((list(waits.items()), fn, ev_sem, inc))
        self.nops += 1
        for k in writes:
            self.res[k] = [ev, []]
        for k in reads:
            if k in writes:
                continue
            r = self.res.get(k)
            if r is None:
                self.res[k] = [None, [ev]]
            else:
                r[1].append(ev)
                if len(r[1]) > 24:
                    best = {}
                    for e_ in r[1]:
                        if e_[0] not in best or best[e_[0]][1] < e_[1]:
                            best[e_[0]] = e_
                    r[1] = list(best.values())
        return ev

    def barrier(self, engines=None):
        latest = {}
        for r in self.res.values():
            evs = list(r[1])
            if r[0] is not None:
                evs.append(r[0])
            for (s, v, c) in evs:
                if latest.get(s, 0) < v:
                    latest[s] = v
        for e in self.ENG:
            for s, v in self.dma_slots.get(e, []):
                if v > 0 and latest.get(s, 0) < v:
                    latest[s] = v
            if self.cnt[e] > 0 and latest.get(self.cursem[e], 0) < self.cnt[e]:
                latest[self.cursem[e]] = self.cnt[e]
        for e in (engines or self.ENG):
            clk = self.clock[e]
            waits = [(s, v) for s, v in latest.items() if clk.get(s, 0) < v]
            for s, v in waits:
                clk[s] = v
            if waits:
                self.q[e].append((waits, None, None, 0))
        self.res = {}

    def emit(self):
        nc = self.nc
        sems = {n: nc.alloc_semaphore(name=n) for n in self.semnames}
        engmap = {"pe": "tensor", "act": "scalar", "dve": "vector", "pool": "gpsimd", "sp": "sync"}
        with nc.Block() as block:
            for e in self.ENG:
                ops = self.q[e]

                def body(eng, ops=ops):
                    for waits, fn, ev_sem, inc in ops:
                        for s, v in waits:
                            eng.wait_ge(sems[s], v)
                        if fn is not None:
                            ins = fn(eng)
                            ins.then_inc(sems[ev_sem], inc)

                getattr(block, engmap[e])(body)


def _nm(ap):
    return ap.name


class Kern:
    def __init__(self, nc, io, cfg):
        self.nc = nc
        self.io = io
        self.cfg = cfg
        self.P = Prog(nc)
        self.uid = 0
        self.psi = 0

    def sb(self, es, name, shape, dt=F32):
        self.uid += 1
        return es.enter_context(self.nc.sbuf_tensor(f"{name}_{self.uid}", list(shape), dt))

    def dram(self, name, shape, dt=F32):
        kind = "ExternalOutput" if name in self.cfg.get("dump", ()) else "Internal"
        return self.nc.dram_tensor(name, list(shape), dt, kind=kind).ap()

    def ps(self):
        t = self.psf[self.psi % len(self.psf)]
        self.psi += 1
        return t

    def hold_ps(self, n):
        held = [self.psf.pop() for _ in range(n)]
        return held

    def release_ps(self, held):
        self.psf.extend(held)

    def _rw(self, outs, ins, r, w):
        reads = list(r)
        writes = list(w)
        for a in ins:
            if a is None or isinstance(a, (int, float)):
                continue
            n = _nm(a)
            if n.startswith("ps"):
                writes.append(n)
            else:
                reads.append(n)
        for a in outs:
            writes.append(_nm(a))
        return reads, writes

    def dma(self, out, in_, eng="sp", r=None, w=None, **kw):
        reads = list(r) if r is not None else [_nm(in_)]
        writes = list(w) if w is not None else [_nm(out)]
        self.P.op(eng, lambda e: e.dma_start(out=out, in_=in_, **kw), reads, writes, dma=True)

    def mm(self, out, lhsT, rhs, start=True, stop=True):
        reads, writes = self._rw([out], [lhsT, rhs], (), ())
        self.P.op("pe", lambda e: e.matmul(out, lhsT=lhsT, rhs=rhs, start=start, stop=stop), reads, writes)

    def tr(self, out, in_, ident):
        reads, writes = self._rw([out], [in_, ident], (), ())
        self.P.op("pe", lambda e: e.transpose(out=out, in_=in_, identity=ident), reads, writes)

    def act(self, out, in_, func, bias=None, scale=None, accum_out=None, eng="act"):
        kw = {}
        if bias is not None:
            kw["bias"] = bias
        if scale is not None:
            kw["scale"] = scale
        outs = [out]
        if accum_out is not None:
            kw["accum_out"] = accum_out
            outs.append(accum_out)
        reads, writes = self._rw(outs, [in_, bias, scale], (), ())
        self.P.op(eng, lambda e: e.activation(out=out, in_=in_, func=func, **kw), reads, writes)

    def cp(self, out, in_, eng="dve"):
        reads, writes = self._rw([out], [in_], (), ())
        if eng == "act":
            self.P.op("act", lambda e: e.copy(out=out, in_=in_), reads, writes)
        else:
            self.P.op(eng, lambda e: e.tensor_copy(out=out, in_=in_), reads, writes)

    def tt(self, out, in0, in1, op, eng="dve"):
        reads, writes = self._rw([out], [in0, in1], (), ())
        self.P.op(eng, lambda e: e.tensor_tensor(out=out, in0=in0, in1=in1, op=op), reads, writes)

    def ts(self, out, in0, s1, s2, op0, op1=None, eng="dve"):
        reads, writes = self._rw([out], [in0, s1, s2], (), ())
        if op1 is None:
            self.P.op(eng, lambda e: e.tensor_scalar(out=out, in0=in0, scalar1=s1, scalar2=None, op0=op0), reads, writes)
        else:
            self.P.op(eng, lambda e: e.tensor_scalar(out=out, in0=in0, scalar1=s1, scalar2=s2, op0=op0, op1=op1), reads, writes)

    def stt(self, out, in0, scalar, in1, op0, op1, accum_out=None, eng="dve"):
        outs = [out]
        kw = {}
        if accum_out is not None:
            kw["accum_out"] = accum_out
            outs.append(accum_out)
        reads, writes = self._rw(outs, [in0, scalar, in1], (), ())
        self.P.op(eng, lambda e: e.scalar_tensor_tensor(out=out, in0=in0, scalar=scalar, in1=in1, op0=op0, op1=op1, **kw), reads, writes)

    def memset(self, ap, val, eng="dve"):
        reads, writes = self._rw([ap], [], (), ())
        self.P.op(eng, lambda e: e.memset(ap, val), reads, writes)

    def vmax(self, out, in_):
        reads, writes = self._rw([out], [in_], (), ())
        self.P.op("dve", lambda e: e.max(out=out, in_=in_), reads, writes)

    def recip(self, out, in_):
        reads, writes = self._rw([out], [in_], (), ())
        self.P.op("dve", lambda e: e.reciprocal(out=out, in_=in_), reads, writes)

    def rsum(self, out, in_):
        reads, writes = self._rw([out], [in_], (), ())
        self.P.op("dve", lambda e: e.reduce_sum(out=out, in_=in_, axis=AX.X), reads, writes)

    def build(self):
        nc, io, cfg = self.nc, self.io, self.cfg
        layers = cfg.get("layers", DEPTH)
        with contextlib.ExitStack() as es:
            self.psf = [es.enter_context(nc.psum_tensor(f"psF{i}", [128, 512], F32)) for i in range(6)]
            self.psb = [es.enter_context(nc.psum_tensor(f"psB{i}", [128, 1024], BF16)) for i in range(2)]
            self.identf = self.sb(es, "identf", [128, 128])
            self.identb = self.sb(es, "identb", [128, 128], BF16)
            self.tri = self.sb(es, "tri", [128, 4, 128])
            self.trib = self.sb(es, "trib", [128, 4, 128], BF16)
            self.misc = self.sb(es, "misc", [128, 8])
            self.dma(self.identf[:], io["cst_ident"])
            self.dma(self.tri[:], io["cst_tri"].rearrange("a p n -> p a n"))
            self.dma(self.misc[:], io["cst_misc"])
            self.p_hb = [self.sb(es, f"p_hb{j}", [128, D], BF16) for j in range(2)]
            self.p_idx = [self.sb(es, f"p_idx{j}", [128, 1], I32) for j in range(8)]
            self.p_gk = [self.sb(es, f"p_gk{j}", [128, D]) for j in range(4)]
            self.cp(self.identb[:], self.identf[:])
            self.cp(self.trib[:], self.tri[:])
            self.XR = self.dram("XR", [NTOK, D])
            self.MOD = self.dram("MOD", [DEPTH, 2, 6 * D])
            self.FM = self.dram("FM", [2048 + 256, NTOK])
            self.TM = self.dram("TM", [NTOK, 528])
            self.YS = self.dram("YS", [1280, NTOK], BF16)
            self.MG = self.dram("MG", [D, NTOK], BF16)
            self.X1 = self.dram("X1", [256, NTOK])
            self.VV = self.dram("VV", [256, NTOK])
            self.XBC = self.dram("XBC", [1024, NTOK], BF16)
            self.YF = self.dram("YF", [NTOK, 512])
            self.XG = self.dram("XG", [NE * ESTR, D], BF16)
            self.YG = self.dram("YG", [NE * ESTR, D])
            self.dma(self.XR[0:NCTX, :], io["ctx"], w=[("XR", 0), ("XR", 1)])
            for q in range(4):
                self.dma(self.XR[NCTX + q * 1024:NCTX + (q + 1) * 1024, :], io["x"][q * 1024:(q + 1) * 1024, :], w=[("XRi", q)])
            with contextlib.ExitStack() as es0:
                z = self.sb(es0, "zrow", [8, D])
                self.memset(z[:], 0.0)
                for e in range(NE):
                    self.dma(self.YG[e * ESTR + CAP:(e + 1) * ESTR, :], z[:], w=[("YGz", e)])
                self.P.barrier()
            self.phase_mods()
            for l in range(layers):
                self.layer(l)
            self.phase_final()
            self.P.barrier(["sp"])
            self.P.emit()

    def phase_mods(self):
        io = self.io
        with contextlib.ExitStack() as es:
            cc = self.sb(es, "cc", [128, 8, 2])
            ccs = self.sb(es, "ccs", [128, 8, 2])
            self.dma(cc[:, :, 0], io["c"].rearrange("o (k p) -> (o p) k", p=128), allow_slow_non_contiguous=True)
            self.dma(cc[:, :, 1], io["c_ctx"].rearrange("o (k p) -> (o p) k", p=128), allow_slow_non_contiguous=True)
            self.act(ccs[:], cc[:], AF.Silu)
            wm = [self.sb(es, f"wm{i}", [128, 8, 512]) for i in range(2)]
            bm = self.sb(es, "bm", [2, 6 * D])
            mo = self.sb(es, "mo", [2, 6 * D])
            for l in range(DEPTH):
                self.dma(bm[:], io["b_mod"][l:l + 1, :].partition_broadcast(2))
                for nb in range(12):
                    w = wm[nb % 2]
                    for k in range(8):
                        self.dma(w[:, k, :], io["w_mod"][l, k * 128:(k + 1) * 128, nb * 512:(nb + 1) * 512])
                    p = self.ps()
                    for k in range(8):
                        self.mm(p[0:2, :], ccs[:, k, :], w[:, k, :], start=(k == 0), stop=(k == 7))
                    self.tt(mo[:, nb * 512:(nb + 1) * 512], p[0:2, :], bm[:, nb * 512:(nb + 1) * 512], ALU.add)
                self.dma(self.MOD[l], mo[:], w=[("MOD", l)])
        self.P.barrier()

    def bc_row(self, dst, src_row):
        self.dma(dst, src_row.partition_broadcast(128))

    def phase_norm(self, l, which, hT, t0=0, extra=None):
        io = self.io
        gname = "norm_mix" if which == 1 else "norm_ffn"
        o_sh, o_sc = (0, 1) if which == 1 else (3, 4)
        with contextlib.ExitStack() as es:
            A = [self.sb(es, f"A{j}", [128, D]) for j in range(2)]
            B = [self.sb(es, f"Bv{j}", [128, D]) for j in range(2)]
            g = self.sb(es, "g", [128, D])
            self.bc_row(g[:], io[gname][l:l + 1, :])
            for j in range(2):
                row = 1 - j
                self.bc_row(A[j][:], self.MOD[l, row:row + 1, o_sc * D:(o_sc + 1) * D])
                self.bc_row(B[j][:], self.MOD[l, row:row + 1, o_sh * D:(o_sh + 1) * D])
                self.stt(A[j][:], A[j][:], 1.0, g[:], ALU.add, ALU.mult)
            xt = [self.sb(es, f"xt{j}", [128, D]) for j in range(2)]
            sq = self.sb(es, "sq", [128, D])
            hf = [self.sb(es, f"hf{j}", [128, D]) for j in range(2)]
            hb = self.p_hb
            ss = [self.sb(es, f"ss{j}", [128, 1]) for j in range(2)]
            for i in range(t0, NT):
                j = 0 if i < 2 else 1
                b = i % 2
                self.dma(xt[b][:], self.XR[i * 128:(i + 1) * 128, :], r=[("XR", i)])
                self.act(sq[:], xt[b][:], AF.Square, accum_out=ss[b][:])
                self.ts(ss[b][:], ss[b][:], 1.0 / D, EPS, ALU.mult, ALU.add)
                self.act(ss[b][:], ss[b][:], AF.Sqrt)
                self.recip(ss[b][:], ss[b][:])
                self.stt(hf[b][:], xt[b][:], ss[b][:, 0:1], A[j][:], ALU.mult, ALU.mult)
                self.tt(hf[b][:], hf[b][:], B[j][:], ALU.add)
                self.cp(hb[b][:], hf[b][:], eng="act")
                if hT is not None:
                    pb = self.psb[i % 2]
                    for k in range(8):
                        self.tr(pb[:, k * 128:(k + 1) * 128], hb[b][:, k * 128:(k + 1) * 128], self.identb[:])
                    self.cp(hT[:, :, i * 128:(i + 1) * 128], pb[:].rearrange("p (k n) -> p k n", k=8))
                if extra is not None:
                    extra(i, hf[b], hb[b])
            self.P.barrier()

    def layer(self, l):
        cfg = self.cfg
        ph = cfg.get("phases", "WFHPSGM")
        if "W" in ph:
            self.phase_proj(l)
        if "F" in ph:
            self.phase_fourier(l)
        if "H" in ph:
            self.phase_hyena(l)
        if "P" in ph:
            self.phase_pool(l)
        if "S" in ph:
            self.phase_ssd(l)
        if "G" in ph:
            self.phase_merge(l)
        if "M" in ph:
            self.phase_moe(l)

    def tokblocks(self):
        return [(0, 256)] + [(NCTX + 512 * j, 512) for j in range(8)]

    def phase_proj(self, l):
        io = self.io
        with contextlib.ExitStack() as es:
            hT = self.sb(es, "hT", [128, 8, NTOK], BF16)
            wi = self.sb(es, "wi", [128, 8, INW], BF16)
            wst = [self.sb(es, f"wst{j}", [128, INW]) for j in range(2)]
            for k in range(8):
                self.dma(wst[k % 2][:], io["w_in"][l, k * 128:(k + 1) * 128, :])
                self.cp(wi[:, k, :], wst[k % 2][:], eng=("act" if k % 2 else "pool"))
            self.phase_norm(l, 1, hT)
            ob = [self.sb(es, f"ob{j}", [128, 512]) for j in range(3)]
            fm_cols = [(0, 256), (256, 768), (1792, 1024), (1024, 256)]
            fm_chunks = []
            for c0, n in fm_cols:
                for j in range(n // 128):
                    fm_chunks.append(c0 + j * 128)
            n = 0
            for (t0, tw) in self.tokblocks():
                for oc, c0 in enumerate(fm_chunks):
                    p = self.ps()
                    for k in range(8):
                        self.mm(p[:, 0:tw], wi[:, k, c0:c0 + 128], hT[:, k, t0:t0 + tw], start=(k == 0), stop=(k == 7))
                    o = ob[n % 3]
                    n += 1
                    if n % 2:
                        self.cp(o[:, 0:tw], p[:, 0:tw], eng="act")
                    else:
                        self.cp(o[:, 0:tw], p[:, 0:tw])
                    self.dma(self.FM[oc * 128:(oc + 1) * 128, t0:t0 + tw], o[:, 0:tw], w=[("FM", oc, t0)])
            for i in range(NT):
                p = self.ps()
                for k in range(8):
                    self.mm(p[:, 0:512], hT[:, k, i * 128:(i + 1) * 128], wi[:, k, 1280:1792], start=(k == 0), stop=(k == 7))
                p2 = self.ps()
                for k in range(8):
                    self.mm(p2[:, 0:16], hT[:, k, i * 128:(i + 1) * 128], wi[:, k, 2816:2832], start=(k == 0), stop=(k == 7))
                o = ob[n % 3]
                n += 1
                self.cp(o[:, 0:512], p[:, 0:512], eng="act")
                self.dma(self.TM[i * 128:(i + 1) * 128, 0:512], o[:, 0:512], w=[("TM", i)])
                o = ob[n % 3]
                n += 1
                self.cp(o[:, 0:16], p2[:, 0:16])
                self.dma(self.TM[i * 128:(i + 1) * 128, 512:528], o[:, 0:16], w=[("TMd", i)])
        self.P.barrier()

    def phase_fourier(self, l):
        io = self.io
        with contextlib.ExitStack() as es:
            fT = self.sb(es, "fT", [128, 2, NTOK], BF16)
            for c in range(2):
                self.dma(fT[:, c, :], self.FM[c * 128:(c + 1) * 128, :], eng="pool")
            bd = self.sb(es, "bd", [128, 2, 128], BF16)
            self.dma(bd[:], io["cst_bd64"].rearrange("a p n -> p a n"), eng="pool")
            U = self.sb(es, "U", [128, NT, 512], BF16)
            for i in range(NT):
                p = self.ps()
                for a in range(2):
                    for c in range(2):
                        self.mm(p[:, a * 256 + c * 128:a * 256 + (c + 1) * 128], fT[:, c, i * 128:(i + 1) * 128], bd[:, a, :])
                self.cp(U[:, i, :], p[:], eng=("act" if i % 2 else "dve"))
            ys = [self.sb(es, f"ysf{j}", [128, 512], BF16) for j in range(2)]
            n = 0
            tbc = self.sb(es, "tbc", [128, 2, 2, 256], BF16)
            self.dma(tbc[:].rearrange("p a r c -> p (a r) c"), io["cst_dft4c"].rearrange("a (r p) c -> p (a r) c", p=128))
            sc_c = 1.0 / math.sqrt(NCTX * 64.0)
            for c in range(2):
                p = self.ps()
                cnt = 0
                for tc in range(2):
                    for a in range(2):
                        self.mm(p[:, 0:256], U[:, tc, a * 256 + c * 128:a * 256 + (c + 1) * 128], tbc[:, a, tc, :], start=(cnt == 0), stop=(cnt == 3))
                        cnt += 1
                o = ys[n % 2]
                n += 1
                self.act(o[:, 0:256], p[:, 0:256], AF.Copy, scale=sc_c)
                self.dma(self.YS[c * 128:(c + 1) * 128, 0:256], o[:, 0:256], w=[("YS", c, 0)])
            tbs = [self.sb(es, f"tb{j}", [128, 2, 32, 256], BF16) for j in range(2)]
            sc_l = 1.0 / math.sqrt(NLAT * 64.0)
            for kb in range(16):
                tb = tbs[kb % 2]
                for a in range(2):
                    self.dma(tb[:, a, :, :], io["cst_dft4"][kb, :, a, :, :])
                for c in range(2):
                    p = self.ps()
                    cnt = 0
                    for tc in range(32):
                        for a in range(2):
                            self.mm(p[:, 0:256], U[:, 2 + tc, a * 256 + c * 128:a * 256 + (c + 1) * 128], tb[:, a, tc, :], start=(cnt == 0), stop=(cnt == 63))
                            cnt += 1
                    o = ys[n % 2]
                    n += 1
                    self.act(o[:, 0:256], p[:, 0:256], AF.Copy, scale=sc_l)
                    self.dma(self.YS[c * 128:(c + 1) * 128, NCTX + kb * 256:NCTX + (kb + 1) * 256], o[:, 0:256], w=[("YS", c, kb + 1)])
        self.P.barrier()

    def conv_chunk(self, es_bufs, src_rows, wcol, bcol, out_ap_fn):
        xin, o = es_bufs
        self.dma(xin[:, 1:1 + NCTX], src_rows[:, 0:NCTX])
        self.dma(xin[:, 259:259 + NLAT], src_rows[:, NCTX:NTOK])
        for (b0, n, o0) in ((0, NCTX, 0), (258, NLAT, NCTX)):
            self.ts(o[:, o0:o0 + n], xin[:, b0:b0 + n], wcol[:, 0:1], bcol, ALU.mult, ALU.add)
            self.stt(o[:, o0:o0 + n], xin[:, b0 + 1:b0 + 1 + n], wcol[:, 1:2], o[:, o0:o0 + n], ALU.mult, ALU.add)
            self.stt(o[:, o0:o0 + n], xin[:, b0 + 2:b0 + 2 + n], wcol[:, 2:3], o[:, o0:o0 + n], ALU.mult, ALU.add)
        out_ap_fn(o)

    def phase_hyena(self, l):
        io = self.io
        with contextlib.ExitStack() as es_outer:
            Kh = self.sb(es_outer, "Kh", [128, NT, 512], BF16)
            KN = self.sb(es_outer, "KN", [1, 2, 256])
            rn = self.sb(es_outer, "rn", [128, 2, 2])
            with contextlib.ExitStack() as es:
                w1 = self.sb(es, "w1", [33, 64]); w2 = self.sb(es, "w2", [64, 64]); w3 = self.sb(es, "w3", [64, 512])
                b1 = self.sb(es, "b1", [64, 1]); b2 = self.sb(es, "b2", [64, 1])
                self.dma(w1[:], io["hy_ffn_w1"][l]); self.dma(w2[:], io["hy_ffn_w2"][l]); self.dma(w3[:], io["hy_ffn_w3"][l])
                self.dma(b1[:], io["hy_ffn_b1"][l:l + 1, :].rearrange("o n -> n o"), allow_slow_non_contiguous=True)
                self.dma(b2[:], io["hy_ffn_b2"][l:l + 1, :].rearrange("o n -> n o"), allow_slow_non_contiguous=True)
                d2 = self.sb(es, "d2", [128, 512])
                self.dma(d2[:], io["cst_delta2"])
                tneg = self.sb(es, "tneg", [128, NT])
                self.dma(tneg[:], io["cst_tneg"])
                KSD = self.sb(es, "KSD", [128, NT, 512], BF16)
                ft = self.sb(es, "ft", [33, 512])
                h1 = self.sb(es, "h1", [64, 512]); h2 = self.sb(es, "h2", [64, 512])
                rrf = self.sb(es, "rrf", [64, 512]); rri = self.sb(es, "rri", [64, 512], I32)
                kd = self.sb(es, "kd", [128, 512]); ka = self.sb(es, "ka", [128, 512]); dec = self.sb(es, "dec", [128, 512])
                held = self.hold_ps(3)
                pn = held[0:2]
                pny = held[2]
                for seq, (nseq, tile0, fkey) in enumerate(((NCTX, 0, "cst_featc"), (NLAT, 2, "cst_feat"))):
                    ntile = nseq // 128
                    for jb in range(0, nseq, 512):
                        bw = min(512, nseq - jb)
                        self.dma(ft[:, 0:bw], io[fkey][:, jb:jb + bw])
                        for (src, wgt, bias, dst, kk) in ((ft, w1, b1, h1, 33), (h1, w2, b2, h2, 64)):
                            p = self.ps()
                            self.mm(p[0:64, 0:bw], wgt[0:kk, :], src[0:kk, 0:bw])
                            self.ts(dst[:, 0:bw], p[0:64, 0:bw], bias[:, 0:1], None, ALU.add)
                            self.ts(rrf[:, 0:bw], dst[:, 0:bw], 1.0 / (2 * math.pi), None, ALU.mult)
                            self.cp(rri[:, 0:bw], rrf[:, 0:bw])
                            self.cp(rrf[:, 0:bw], rri[:, 0:bw])
                            self.stt(dst[:, 0:bw], rrf[:, 0:bw], -2 * math.pi, dst[:, 0:bw], ALU.mult, ALU.add)
                            self.act(dst[:, 0:bw], dst[:, 0:bw], AF.Sin)
                        for jt in range(bw // 128):
                            ti = tile0 + (jb // 128) + jt
                            p = self.ps()
                            self.mm(p[:], h2[:, jt * 128:(jt + 1) * 128], w3[:])
                            self.act(dec[:], d2[:], AF.Exp, scale=tneg[:, ti:ti + 1])
                            self.tt(kd[:], p[:], dec[:], ALU.mult)
                            if jb == 0 and jt == 0:
                                self.ts(kd[:, 256:512], kd[:, 256:512], self.misc[:, 1:2], None, ALU.mult)
                            self.act(ka[:], kd[:], AF.Abs)
                            first = (jb == 0 and jt == 0)
                            last = (jb + jt * 128 + 128 == nseq)
                            for c in range(2):
                                for hlf in range(2):
                                    self.mm(pn[c][:, seq:seq + 1], ka[:, hlf * 256 + c * 128:hlf * 256 + (c + 1) * 128], self.tri[:, 3, 0:1],
                                            start=(first and hlf == 0), stop=(last and hlf == 1))
                            self.tt(KSD[:, ti, 0:256], kd[:, 0:256], kd[:, 256:512], ALU.add)
                            self.tt(KSD[:, ti, 256:512], kd[:, 0:256], kd[:, 256:512], ALU.subtract)
                            self.tt(ka[:, 0:256], kd[:, 0:256], kd[:, 256:512], ALU.add)
                            self.mm(pny[0:1, seq * 256:(seq + 1) * 256], self.misc[:, 0:1], ka[:, 0:256], start=first, stop=last)
                    for c in range(2):
                        self.recip(rn[:, c, seq:seq + 1], pn[c][:, seq:seq + 1])
                    self.cp(KN[:, seq, :], pny[0:1, seq * 256:(seq + 1) * 256])
                self.release_ps(held)
                tbc = self.sb(es, "tbc8", [128, 2, 2, 256], BF16)
                self.dma(tbc[:].rearrange("p a r c -> p (a r) c"), io["cst_dft8c"].rearrange("a (r p) c -> p (a r) c", p=128))
                for ftile in range(2):
                    p = self.ps()
                    for a in range(2):
                        for jc in range(2):
                            self.mm(p[:, a * 256:(a + 1) * 256], tbc[:, a, jc, ftile * 128:(ftile + 1) * 128], KSD[:, jc, a * 256:(a + 1) * 256],
                                    start=(jc == 0), stop=(jc == 1))
                    self.cp(Kh[:, ftile, :], p[:], eng="act")
                tbs = [self.sb(es, f"tb8{j}", [128, 2, 32, 256], BF16) for j in range(2)]
                for fb in range(16):
                    tb = tbs[fb % 2]
                    for a in range(2):
                        self.dma(tb[:, a, :, :], io["cst_dft8"][fb, :, a, :, :])
                    for fsub in range(2):
                        ftile = fb * 2 + fsub
                        p = self.ps()
                        for a in range(2):
                            for jc in range(32):
                                self.mm(p[:, a * 256:(a + 1) * 256], tb[:, a, jc, fsub * 128:(fsub + 1) * 128], KSD[:, 2 + jc, a * 256:(a + 1) * 256],
                                        start=(jc == 0), stop=(jc == 31))
                        self.cp(Kh[:, 2 + ftile, :], p[:], eng=("act" if ftile % 2 else "dve"))
            self.P.barrier()
            with contextlib.ExitStack() as es:
                cw = self.sb(es, "cw", [128, 6, 3]); cb = self.sb(es, "cb", [128, 6])
                for k in range(3):
                    self.dma(cw[:, :, k], io["hy_conv_w"][l, k:k + 1, :].rearrange("o (c p) -> (o p) c", p=128), allow_slow_non_contiguous=True)
                self.dma(cb[:], io["hy_conv_b"][l:l + 1, :].rearrange("o (c p) -> (o p) c", p=128), allow_slow_non_contiguous=True)
                xin = self.sb(es, "xin", [128, NTOK + 4])
                self.memset(xin[:], 0.0)
                oo = [self.sb(es, f"cvo{j}", [128, NTOK]) for j in range(3)]
                for c in range(2):
                    self.conv_chunk((xin, oo[0]), self.FM[256 + c * 128:256 + (c + 1) * 128, :], cw[:, c, :], cb[:, c:c + 1],
                                    lambda o, c=c: self.dma(self.X1[c * 128:(c + 1) * 128, :], o[:], w=[("X1", c)]))
                    self.conv_chunk((xin, oo[1]), self.FM[256 + (2 + c) * 128:256 + (3 + c) * 128, :], cw[:, 2 + c, :], cb[:, 2 + c:3 + c], lambda o: None)
                    self.conv_chunk((xin, oo[2]), self.FM[256 + (4 + c) * 128:256 + (5 + c) * 128, :], cw[:, 4 + c, :], cb[:, 4 + c:5 + c], lambda o: None)
                    self.tt(oo[1][:], oo[1][:], oo[2][:], ALU.mult)
                    self.dma(self.VV[c * 128:(c + 1) * 128, :], oo[1][:], w=[("VV", c)])
            self.P.barrier()
            with contextlib.ExitStack() as es:
                Pb = self.sb(es, "Pb", [128, NT, 512], BF16)
                PN = self.sb(es, "PN", [1, 2, 256], BF16)
                with contextlib.ExitStack() as es2:
                    vvb = self.sb(es2, "vvb", [128, 2, NTOK], BF16)
                    for c in range(2):
                        self.dma(vvb[:, c, :], self.VV[c * 128:(c + 1) * 128, :], eng="pool")
                    VVt = self.sb(es2, "VVt", [128, NT, 256], BF16)
                    for i in range(NT):
                        pb = self.psb[i % 2]
                        for c in range(2):
                            self.tr(pb[:, c * 128:(c + 1) * 128], vvb[:, c, i * 128:(i + 1) * 128], self.identb[:])
                        self.cp(VVt[:, i, :], pb[:, 0:256], eng=("act" if i % 2 else "dve"))
                    altb = self.sb(es2, "altb", [128, 1], BF16)
                    self.cp(altb[:], self.misc[:, 0:1])
                    vh = self.sb(es2, "vh", [128, 512]); khf = self.sb(es2, "khf", [128, 512])
                    t1 = self.sb(es2, "t1", [128, 256]); t2 = self.sb(es2, "t2", [128, 256])
                    held2 = self.hold_ps(1)
                    pny = held2[0]
                    vn = self.sb(es2, "vn", [1, 256])

                    def spec_product(ti, p, is_f0):
                        self.cp(vh[:], p[:], eng="act")
                        self.cp(khf[:], Kh[:, ti, :], eng="pool")
                        self.tt(t1[:], vh[:, 0:256], khf[:, 0:256], ALU.mult)
                        self.tt(t2[:], vh[:, 256:512], khf[:, 256:512], ALU.mult)
                        self.tt(t1[:], t1[:], t2[:], ALU.subtract)
                        if is_f0:
                            self.ts(t1[:], t1[:], self.misc[:, 2:3], None, ALU.mult)
                        self.cp(Pb[:, ti, 0:256], t1[:], eng="act")
                        self.tt(t1[:], vh[:, 0:256], khf[:, 256:512], ALU.mult, eng="pool")
                        self.tt(t2[:], vh[:, 256:512], khf[:, 0:256], ALU.mult, eng="pool")
                        self.tt(t1[:], t1[:], t2[:], ALU.add, eng="pool")
                        if is_f0:
                            self.ts(t1[:], t1[:], self.misc[:, 2:3], None, ALU.mult)
                        self.cp(Pb[:, ti, 256:512], t1[:], eng="act")

                    tbc = self.sb(es2, "tbc8b", [128, 2, 2, 256], BF16)
                    self.dma(tbc[:].rearrange("p a r c -> p (a r) c"), io["cst_dft8c"].rearrange("a (r p) c -> p (a r) c", p=128))
                    for ftile in range(2):
                        p = self.ps()
                        for a in range(2):
                            for tc in range(2):
                                self.mm(p[:, a * 256:(a + 1) * 256], tbc[:, a, tc, ftile * 128:(ftile + 1) * 128], VVt[:, tc, :], start=(tc == 0), stop=(tc == 1))
                        spec_product(ftile, p, ftile == 0)
                    for tc in range(2):
                        self.mm(pny[0:1, 0:256], altb[:, 0:1], VVt[:, tc, :], start=(tc == 0), stop=(tc == 1))
                    self.tt(vn[:], pny[0:1, 0:256], KN[:, 0, :], ALU.mult)
                    self.ts(PN[:, 0, :], vn[:], 0.5, None, ALU.mult)
                    tbs = [self.sb(es2, f"tb8b{j}", [128, 2, 32, 256], BF16) for j in range(2)]
                    for fb in range(16):
                        tb = tbs[fb % 2]
                        for a in range(2):
                            self.dma(tb[:, a, :, :], io["cst_dft8"][fb, :, a, :, :])
                        for fsub in range(2):
                            ftile = fb * 2 + fsub
                            p = self.ps()
                            for a in range(2):
                                for tc in range(32):
                                    self.mm(p[:, a * 256:(a + 1) * 256], tb[:, a, tc, fsub * 128:(fsub + 1) * 128], VVt[:, 2 + tc, :], start=(tc == 0), stop=(tc == 31))
                            spec_product(2 + ftile, p, ftile == 0)
                    for tc in range(32):
                        self.mm(pny[0:1, 256:512], altb[:, 0:1], VVt[:, 2 + tc, :], start=(tc == 0), stop=(tc == 31))
                    self.tt(vn[:], pny[0:1, 256:512], KN[:, 1, :], ALU.mult)
                    self.ts(PN[:, 1, :], vn[:], 0.5, None, ALU.mult)
                    self.release_ps(held2)
                self.P.barrier()
                with contextlib.ExitStack() as es2:
                    hb_ = self.sb(es2, "hyb", [128, 2])
                    self.dma(hb_[:], io["hy_bias"][l:l + 1, :].rearrange("o (c p) -> (o p) c", p=128), allow_slow_non_contiguous=True)
                    altrow = self.sb(es2, "altrow", [1, 512], BF16)
                    self.dma(altrow[:], io["cst_altrow"][:, 0:512])
                    yv = [self.sb(es2, f"yv{j}", [128, 512]) for j in range(2)]
                    vvt = [self.sb(es2, f"vvt{j}", [128, 512]) for j in range(2)]
                    x1t = [self.sb(es2, f"x1t{j}", [128, 512]) for j in range(2)]
                    yo = [self.sb(es2, f"yho{j}", [128, 512], BF16) for j in range(2)]
                    scl = self.sb(es2, "scl", [128, 2, 2])
                    self.ts(scl[:, :, 0:1], rn[:, :, 0:1], 2.0 / (2 * NCTX), None, ALU.mult)
                    self.ts(scl[:, :, 1:2], rn[:, :, 1:2], 2.0 / (2 * NLAT), None, ALU.mult)
                    n = 0

                    def finish(c, seq, p, t0, tw):
                        nonlocal n
                        b = n % 2
                        n += 1
                        self.dma(vvt[b][:, 0:tw], self.VV[c * 128:(c + 1) * 128, t0:t0 + tw])
                        self.dma(x1t[b][:, 0:tw], self.X1[c * 128:(c + 1) * 128, t0:t0 + tw])
                        self.act(yv[b][:, 0:tw], p[:, 0:tw], AF.Copy, scale=scl[:, c, seq:seq + 1])
                        self.stt(yv[b][:, 0:tw], vvt[b][:, 0:tw], hb_[:, c:c + 1], yv[b][:, 0:tw], ALU.mult, ALU.add)
                        self.tt(yo[b][:, 0:tw], yv[b][:, 0:tw], x1t[b][:, 0:tw], ALU.mult)
                        self.dma(self.YS[256 + c * 128:256 + (c + 1) * 128, t0:t0 + tw], yo[b][:, 0:tw], w=[("YS", 2 + c, t0)])

                    tbc = self.sb(es2, "tbc8c", [128, 2, 2, 256], BF16)
                    self.dma(tbc[:].rearrange("p a r c -> p (a r) c"), io["cst_dft8c"].rearrange("a (r p) c -> p (a r) c", p=128))
                    for c in range(2):
                        p = self.ps()
                        cnt = 0
                        for fc in range(2):
                            for a in range(2):
                                self.mm(p[:, 0:256], Pb[:, fc, a * 256 + c * 128:a * 256 + (c + 1) * 128], tbc[:, a, fc, :], start=(cnt == 0), stop=False)
                                cnt += 1
                        self.mm(p[:, 0:256], PN[:, 0, c * 128:(c + 1) * 128], altrow[:, 0:256], start=False, stop=True)
                        finish(c, 0, p, 0, 256)
                    tbs = [self.sb(es2, f"tb8c{j}", [128, 2, 32, 256], BF16) for j in range(2)]
                    for tbk in range(16):
                        tb = tbs[tbk % 2]
                        for a in range(2):
                            self.dma(tb[:, a, :, :], io["cst_dft8"][tbk, :, a, :, :])
                        for c in range(2):
                            p = self.ps()
                            cnt = 0
                            for fc in range(32):
                                for a in range(2):
                                    self.mm(p[:, 0:256], Pb[:, 2 + fc, a * 256 + c * 128:a * 256 + (c + 1) * 128], tb[:, a, fc, :], start=(cnt == 0), stop=False)
                                    cnt += 1
                            self.mm(p[:, 0:256], PN[:, 1, c * 128:(c + 1) * 128], altrow[:, 0:256], start=False, stop=True)
                            finish(c, 1, p, NCTX + tbk * 256, 256)
        self.P.barrier()

    def phase_pool(self, l):
        io = self.io
        with contextlib.ExitStack() as es:
            pT = self.sb(es, "pinT", [128, 2, NTOK], BF16)
            for c in range(2):
                self.dma(pT[:, c, :], self.FM[2048 + c * 128:2048 + (c + 1) * 128, :], eng="pool")
            bdwf = self.sb(es, "bdwf", [128, 2, 128])
            self.memset(bdwf[:], 0.0)
            for g in range(4):
                pg, gi = g // 2, g % 2
                self.dma(bdwf[gi * 64:(gi + 1) * 64, pg, gi * 64:(gi + 1) * 64], io["pool_w"][l, g])
            bdw = self.sb(es, "bdw", [128, 2, 128], BF16)
            self.cp(bdw[:], bdwf[:])
            psc = self.sb(es, "psc", [128, 2])
            self.dma(psc[:], io["pool_scale"][l:l + 1, :].rearrange("o (c p) -> (o p) c", p=128), allow_slow_non_contiguous=True)
            pm = self.sb(es, "pm", [128, 4, 128], BF16)
            self.dma(pm[:], io["cst_pool"].rearrange("g s t -> s g t"), eng="pool")
            pmc = self.sb(es, "pmc", [128, 4, 2, 256], BF16)
            self.dma(pmc[:].rearrange("p g r t -> p (g r) t"), io["cst_poolc"].rearrange("g (r p) t -> p (g r) t", p=128), eng="pool")
            Az = [self.sb(es, f"Az{j}", [128, 4, 128], BF16) for j in range(2)]
            for j in range(2):
                self.memset(Az[j][:], 0.0)
            po = [self.sb(es, f"po{j}", [128, 2, 128], BF16) for j in range(2)]

            def make_az(i, az):
                p = self.ps()
                for pg in range(2):
                    self.mm(p[:, pg * 128:(pg + 1) * 128], pT[:, pg, i * 128:(i + 1) * 128], bdw[:, pg, :])
                pv = p[:, 0:256].rearrange("p (pg gi d) -> p pg gi d", pg=2, gi=2)
                azv = az[:].rearrange("p (pg gi) (h d) -> p pg gi h d", pg=2, h=2)
                self.cp(azv[:, :, 0, 0, :], pv[:, :, 0, :])
                self.cp(azv[:, :, 1, 1, :], pv[:, :, 1, :], eng="act")

            make_az(0, Az[0])
            make_az(1, Az[1])
            for tt_ in range(2):
                p = self.ps()
                for pg in range(2):
                    cnt = 0
                    for st in range(2):
                        for gi in range(2):
                            g = pg * 2 + gi
                            self.mm(p[:, pg * 128:(pg + 1) * 128], Az[st][:, g, :], pmc[:, g, st, tt_ * 128:(tt_ + 1) * 128], start=(cnt == 0), stop=(cnt == 3))
                            cnt += 1
                o = po[tt_ % 2]
                for pg in range(2):
                    self.act(o[:, pg, :], p[:, pg * 128:(pg + 1) * 128], AF.Copy, scale=psc[:, pg:pg + 1])
                    self.dma(self.YS[512 + pg * 128:512 + (pg + 1) * 128, tt_ * 128:(tt_ + 1) * 128], o[:, pg, :], w=[("YS", 4 + pg, tt_)])
            self.P.barrier()
            for i in range(2, NT):
                az = Az[i % 2]
                make_az(i, az)
                p = self.ps()
                for pg in range(2):
                    for gi in range(2):
                        g = pg * 2 + gi
                        self.mm(p[:, pg * 128:(pg + 1) * 128], az[:, g, :], pm[:, g, :], start=(gi == 0), stop=(gi == 1))
                o = po[i % 2]
                for pg in range(2):
                    self.act(o[:, pg, :], p[:, pg * 128:(pg + 1) * 128], AF.Copy, scale=psc[:, pg:pg + 1])
                    self.dma(self.YS[512 + pg * 128:512 + (pg + 1) * 128, i * 128:(i + 1) * 128], o[:, pg, :], w=[("YS", 4 + pg, i)])
        self.P.barrier()

    def phase_ssd(self, l):
        io = self.io
        with contextlib.ExitStack() as es:
            cw = self.sb(es, "scw", [128, 8, 3]); cb = self.sb(es, "scb", [128, 8])
            for k in range(3):
                self.dma(cw[:, :, k], io["ssm_conv_w"][l, k:k + 1, :].rearrange("o (c p) -> (o p) c", p=128), allow_slow_non_contiguous=True)
            self.dma(cb[:], io["ssm_conv_b"][l:l + 1, :].rearrange("o (c p) -> (o p) c", p=128), allow_slow_non_contiguous=True)
            xin = self.sb(es, "sxin", [128, NTOK + 4])
            self.memset(xin[:], 0.0)
            oo = [self.sb(es, f"scvo{j}", [128, NTOK]) for j in range(2)]
            ob = [self.sb(es, f"scvb{j}", [128, NTOK], BF16) for j in range(2)]
            for c in range(8):
                def fin(o, c=c):
                    self.act(ob[c % 2][:], o[:], AF.Silu)
                    self.dma(self.XBC[c * 128:(c + 1) * 128, :], ob[c % 2][:], w=[("XBC", c)])
                self.conv_chunk((xin, oo[c % 2]), self.FM[1024 + c * 128:1024 + (c + 1) * 128, :], cw[:, c, :], cb[:, c:c + 1], fin)
        self.P.barrier()
        with contextlib.ExitStack() as es:
            dtb = self.sb(es, "dtb", [128, 16]); abc = self.sb(es, "abc", [128, 16]); dsk = self.sb(es, "dsk", [128, 8])
            snw = self.sb(es, "snw", [128, 512])
            self.bc_row(dtb[:], io["ssm_dt_bias"][l:l + 1].rearrange("o a b -> o (a b)"))
            self.bc_row(abc[:], io["ssm_a_log"][l:l + 1].rearrange("o a b -> o (a b)"))
            self.bc_row(dsk[:], io["ssm_d"][l:l + 1, :])
            self.bc_row(snw[:], io["ssm_norm"][l:l + 1, :])
            self.act(abc[:], abc[:], AF.Exp)
            self.ts(abc[:], abc[:], -1.0, None, ALU.mult)
            H = self.sb(es, "Hst", [128, 512])
            Hb = self.sb(es, "Hstb", [128, 512], BF16)
            xb = [self.sb(es, f"xbct{j}", [128, 8, 128], BF16) for j in range(2)]
            tmt = [self.sb(es, f"tmt{j}", [128, 528]) for j in range(2)]
            xs_t = self.sb(es, "xs_t", [128, 512])
            Bt = self.sb(es, "Bt", [128, 256], BF16)
            dt = self.sb(es, "dt", [128, 8]); dta = self.sb(es, "dta", [128, 8]); tq = self.sb(es, "tq", [128, 8])
            dtax = self.sb(es, "dtax", [128, 8, 128])
            acs = self.sb(es, "acs", [128, 8]); tot = self.sb(es, "tot", [128, 8]); eacs = self.sb(es, "eacs", [128, 8])
            tend = self.sb(es, "tend", [128, 8]); dect = self.sb(es, "dect", [128, 8])
            scm = self.sb(es, "scm", [128, 2, 128])
            seg = self.sb(es, "seg", [128, 4, 128]); M = self.sb(es, "Mm", [128, 8, 128], BF16)
            xdt = self.sb(es, "xdt", [128, 512], BF16); xdtw = self.sb(es, "xdtw", [128, 512], BF16)
            yt = self.sb(es, "yt", [128, 512]); yf = self.sb(es, "yf", [128, 512]); zt = self.sb(es, "zt", [128, 512])
            ysq = self.sb(es, "ysq", [128, 512]); yb = self.sb(es, "yb16", [128, 512], BF16)
            ssq = self.sb(es, "ssq", [128, 1])
            yso = [self.sb(es, f"yso{j}", [128, 4, 128], BF16) for j in range(2)]

            def v3(ap, a, b):
                return ap.rearrange("p (a b) -> p a b", a=a)

            for d in range(2):
                self.memset(H[:], 0.0)
                self.memset(Hb[:], 0.0)
                order = list(range(NT)) if d == 0 else [1, 0] + list(range(NT - 1, 1, -1))
                trisel = self.tri[:, d, :]
                for n_, i in enumerate(order):
                    b = n_ % 2
                    self.dma(xb[b][:], self.XBC.rearrange("(c p) n -> p c n", p=128)[:, :, i * 128:(i + 1) * 128])
                    self.dma(tmt[b][:], self.TM[i * 128:(i + 1) * 128, :])
                    pb = self.psb[n_ % 2]
                    for c in range(6):
                        self.tr(pb[:, c * 128:(c + 1) * 128], xb[b][:, c, :], self.identb[:])
                    self.cp(xs_t[:], pb[:, 0:512], eng="act")
                    self.cp(Bt[:], pb[:, 512:768])
                    self.tt(dt[:], tmt[b][:, 512 + d * 8:520 + d * 8], dtb[:, d * 8:(d + 1) * 8], ALU.add)
                    self.act(tq[:], dt[:], AF.Abs)
                    self.act(tq[:], tq[:], AF.Exp, scale=-1.0)
                    self.act(tq[:], tq[:], AF.Ln, bias=1.0, scale=1.0)
                    self.stt(dt[:], dt[:], 0.0, tq[:], ALU.max, ALU.add)
                    self.tt(dta[:], dt[:], abc[:, d * 8:(d + 1) * 8], ALU.mult)
                    self.cp(dtax[:], dta[:].unsqueeze(2).to_broadcast([128, 8, 128]))
                    p = self.ps()
                    self.mm(p[:, 0:8], trisel, dta[:])
                    self.mm(p[:, 8:16], self.tri[:, 3, :], dta[:])
                    self.cp(acs[:], p[:, 0:8])
                    self.cp(tot[:], p[:, 8:16])
                    self.act(eacs[:], acs[:], AF.Exp)
                    self.tt(tend[:], tot[:], acs[:], ALU.subtract)
                    self.act(tend[:], tend[:], AF.Exp)
                    self.act(dect[:], tot[:], AF.Exp)
                    p = self.ps()
                    for g in range(2):
                        self.mm(p[:, g * 128:(g + 1) * 128], xb[b][:, 4 + g, :], xb[b][:, 6 + g, :])
                    self.tt(scm[:], v3(p[:, 0:256], 2, 128), trisel.unsqueeze(1).to_broadcast([128, 2, 128]), ALU.mult)
                    for g in range(2):
                        p = self.ps()
                        for r in range(4):
                            self.mm(p[:, r * 128:(r + 1) * 128], dtax[:, g * 4 + r, :], trisel)
                        self.tt(seg[:], v3(p[:], 4, 128), acs[:, g * 4:(g + 1) * 4].unsqueeze(2).to_broadcast([128, 4, 128]), ALU.subtract)
                        self.ts(seg[:], seg[:], 0.0, None, ALU.min)
                        self.act(seg[:], seg[:], AF.Exp)
                        self.tt(M[:, g * 4:(g + 1) * 4, :], seg[:], scm[:, g, :].unsqueeze(1).to_broadcast([128, 4, 128]), ALU.mult)
                    self.tt(v3(xdt[:], 8, 64), v3(xs_t[:], 8, 64), dt[:].unsqueeze(2).to_broadcast([128, 8, 64]), ALU.mult)
                    self.tt(tq[:], dt[:], tend[:], ALU.mult)
                    self.tt(v3(xdtw[:], 8, 64), v3(xs_t[:], 8, 64), tq[:].unsqueeze(2).to_broadcast([128, 8, 64]), ALU.mult)
                    pd = self.ps()
                    for hh in range(8):
                        self.mm(pd[:, hh * 64:(hh + 1) * 64], M[:, hh, :], xdt[:, hh * 64:(hh + 1) * 64])
                    po_ = self.ps()
                    for g in range(2):
                        self.mm(po_[:, g * 256:(g + 1) * 256], xb[b][:, 6 + g, :], Hb[:, g * 256:(g + 1) * 256])
                    self.tt(v3(yt[:], 8, 64), v3(po_[:], 8, 64), eacs[:].unsqueeze(2).to_broadcast([128, 8, 64]), ALU.mult)
                    self.tt(yt[:], yt[:], pd[:], ALU.add)
                    pst = self.ps()
                    for g in range(2):
                        self.mm(pst[:, g * 256:(g + 1) * 256], Bt[:, g * 128:(g + 1) * 128], xdtw[:, g * 256:(g + 1) * 256])
                    self.tt(v3(H[:], 8, 64), v3(H[:], 8, 64), dect[:].unsqueeze(2).to_broadcast([128, 8, 64]), ALU.mult)
                    self.tt(H[:], H[:], pst[:], ALU.add)
                    self.cp(Hb[:], H[:], eng="act")
                    if d == 0:
                        self.tt(v3(yf[:], 8, 64), v3(xs_t[:], 8, 64), dsk[:].unsqueeze(2).to_broadcast([128, 8, 64]), ALU.mult)
                        self.tt(yf[:], yf[:], yt[:], ALU.add)
                        self.dma(self.YF[i * 128:(i + 1) * 128, :], yf[:], w=[("YF", i)])
                    else:
                        self.dma(yf[:], self.YF[i * 128:(i + 1) * 128, :], r=[("YF", i)])
                        self.tt(yt[:], yt[:], yf[:], ALU.add)
                        self.act(zt[:], tmt[b][:, 0:512], AF.Silu)
                        self.tt(yt[:], yt[:], zt[:], ALU.mult)
                        self.act(ysq[:], yt[:], AF.Square, accum_out=ssq[:])
                        self.ts(ssq[:], ssq[:], 1.0 / 512, EPS, ALU.mult, ALU.add)
                        self.act(ssq[:], ssq[:], AF.Sqrt)
                        self.recip(ssq[:], ssq[:])
                        self.stt(yb[:], yt[:], ssq[:, 0:1], snw[:], ALU.mult, ALU.mult)
                        pb2 = self.psb[(n_ + 1) % 2]
                        for c in range(4):
                            self.tr(pb2[:, c * 128:(c + 1) * 128], yb[:, c * 128:(c + 1) * 128], self.identb[:])
                        o = yso[n_ % 2]
                        self.cp(o[:], pb2[:, 0:512].rearrange("p (c n) -> p c n", c=4), eng="act")
                        self.dma(self.YS.rearrange("(c p) n -> p c n", p=128)[:, 6:10, i * 128:(i + 1) * 128], o[:], w=[("YS", 6, i)])
                self.P.barrier()

    def phase_merge(self, l):
        io = self.io
        with contextlib.ExitStack() as es:
            hT = self.sb(es, "hT2", [128, 8, NTOK], BF16)
            self.phase_norm(l, 1, hT)
            wg = self.sb(es, "wg", [128, 4, 8, 512], BF16)
            wbr = self.sb(es, "wbr", [128, 10, 512], BF16)
            gst = [self.sb(es, f"gst{j}", [128, 4, 512]) for j in range(2)]
            ysb = [self.sb(es, f"ysb{j}", [128, 10, 512], BF16) for j in range(2)]
            gt = [self.sb(es, f"gt{j}", [128, 512]) for j in range(2)]
            acc = self.sb(es, "macc", [128, 512]); tmp = self.sb(es, "mtmp", [128, 512])
            mo = [self.sb(es, f"mo{j}", [128, 512], BF16) for j in range(2)]
            br_k = [(0, 2), (2, 4), (4, 6), (6, 10)]
            ng = 0
            nblk = 0
            for half in range(2):
                hc = slice(half * 512, (half + 1) * 512)
                for k in range(4):
                    for q in range(2):
                        st = gst[ng % 2]
                        ng += 1
                        self.dma(st[:], io["w_gate"][l, k, q * 512:(q + 1) * 512, hc].rearrange("(c p) n -> p c n", p=128))
                        self.cp(wg[:, k, q * 4:(q + 1) * 4, :], st[:], eng=("act" if ng % 2 else "pool"))
                for (q0, qn) in ((0, 4), (4, 4), (8, 2)):
                    st = gst[ng % 2]
                    ng += 1
                    self.dma(st[:, 0:qn, :], io["w_branch"][l, q0 * 128:(q0 + qn) * 128, hc].rearrange("(c p) n -> p c n", p=128))
                    self.cp(wbr[:, q0:q0 + qn, :], st[:, 0:qn, :], eng=("act" if ng % 2 else "pool"))
                for bi, (t0, tw) in enumerate(self.tokblocks()):
                    yb_ = ysb[nblk % 2]
                    nblk += 1
                    self.dma(yb_[:, :, 0:tw], self.YS.rearrange("(c p) n -> p c n", p=128)[:, :, t0:t0 + tw])
                    for o4 in range(4):
                        oc = half * 4 + o4
                        oc_s = slice(o4 * 128, (o4 + 1) * 128)
                        for k in range(4):
                            pg_ = self.ps()
                            for c in range(8):
                                self.mm(pg_[:, 0:tw], wg[:, k, c, oc_s], hT[:, c, t0:t0 + tw], start=(c == 0), stop=(c == 7))
                            g_ = gt[k % 2]
                            self.act(g_[:, 0:tw], pg_[:, 0:tw], AF.Sigmoid)
                            pb_ = self.ps()
                            c0, c1 = br_k[k]
                            for c in range(c0, c1):
                                self.mm(pb_[:, 0:tw], wbr[:, c, oc_s], yb_[:, c, 0:tw], start=(c == c0), stop=(c == c1 - 1))
                            if k == 0:
                                self.tt(acc[:, 0:tw], pb_[:, 0:tw], g_[:, 0:tw], ALU.mult)
                            else:
                                self.tt(tmp[:, 0:tw], pb_[:, 0:tw], g_[:, 0:tw], ALU.mult)
                                self.tt(acc[:, 0:tw], acc[:, 0:tw], tmp[:, 0:tw], ALU.add, eng="pool")
                        o = mo[o4 % 2]
                        self.cp(o[:, 0:tw], acc[:, 0:tw], eng="act")
                        self.dma(self.MG[oc * 128:(oc + 1) * 128, t0:t0 + tw], o[:, 0:tw], w=[("MG", oc, bi)])
        self.P.barrier()
        with contextlib.ExitStack() as es:
            wo = self.sb(es, "wo", [128, 8, D], BF16)
            wost = [self.sb(es, f"wost{j}", [128, D]) for j in range(2)]
            for k in range(8):
                self.dma(wost[k % 2][:], io["w_out"][l, k * 128:(k + 1) * 128, :])
                self.cp(wo[:, k, :], wost[k % 2][:], eng=("act" if k % 2 else "dve"))
            G = [self.sb(es, f"G{j}", [128, D]) for j in range(2)]
            for j in range(2):
                self.bc_row(G[j][:], self.MOD[l, 1 - j:2 - j, 2 * D:3 * D])
            mt = [self.sb(es, f"mt{j}", [128, 8, 128], BF16) for j in range(2)]
            xt = [self.sb(es, f"xo{j}", [128, D]) for j in range(2)]
            yy = self.sb(es, "yy", [128, D])
            for i in range(NT):
                if l == DEPTH - 1 and i < 2:
                    continue
                b = i % 2
                j = 0 if i < 2 else 1
                self.dma(mt[b][:], self.MG.rearrange("(c p) n -> p c n", p=128)[:, :, i * 128:(i + 1) * 128])
                self.dma(xt[b][:], self.XR[i * 128:(i + 1) * 128, :], r=[("XR", i)])
                for hf in range(2):
                    p = self.ps()
                    for k in range(8):
                        self.mm(p[:], mt[b][:, k, :], wo[:, k, hf * 512:(hf + 1) * 512], start=(k == 0), stop=(k == 7))
                    self.tt(yy[:, hf * 512:(hf + 1) * 512], p[:], G[j][:, hf * 512:(hf + 1) * 512], ALU.mult)
                self.tt(xt[b][:], xt[b][:], yy[:], ALU.add, eng="pool")
                self.dma(self.XR[i * 128:(i + 1) * 128, :], xt[b][:], w=[("XR", i)])
        self.P.barrier()

    def phase_moe(self, l):
        io = self.io
        t0 = 2 if l == DEPTH - 1 else 0
        with contextlib.ExitStack() as es_outer:
            SL = self.sb(es_outer, "SL", [128, NT, 4], I32)
            WS = self.sb(es_outer, "WS", [128, NT, 4])
            with contextlib.ExitStack() as es:
                rw = self.sb(es, "rw", [128, 8, NE])
                self.dma(rw[:], io["router_w"][l].rearrange("(k p) n -> p k n", p=128))
                rb = self.sb(es, "rb", [128, NE])
                self.bc_row(rb[:], io["router_b"][l:l + 1, :])
                ebase = self.sb(es, "ebase", [128, NE])
                self.dma(ebase[:], io["cst_ebase"])
                carry = self.sb(es, "carry", [128, NE])
                self.memset(carry[:], 0.0)
                hTf = self.sb(es, "hTf", [128, 8, 128])
                lg = self.sb(es, "lg", [128, NE]); mx = self.sb(es, "mx", [128, 8]); msk = self.sb(es, "msk", [128, NE])
                mskb = self.sb(es, "mskb", [128, NE], BF16)
                ex = self.sb(es, "ex", [128, NE]); den = self.sb(es, "den", [128, 1]); nmx = self.sb(es, "nmx", [128, 1])
                pos = self.sb(es, "pos", [128, NE]); okm = self.sb(es, "okm", [128, NE]); sv = self.sb(es, "sv", [128, NE])
                mx2 = self.sb(es, "mx2", [128, 8]); slf = self.sb(es, "slf", [128, 4]); junk = self.sb(es, "junk", [128, NE])

                sidx = self.p_idx

                def route(i, hf, hb):
                    for hh in range(2):
                        p = self.ps()
                        for k in range(4):
                            self.tr(p[:, k * 128:(k + 1) * 128], hf[:, (hh * 4 + k) * 128:(hh * 4 + k + 1) * 128], self.identf[:])
                        self.cp(hTf[:, hh * 4:(hh + 1) * 4, :], p[:].rearrange("p (k n) -> p k n", k=4), eng=("act" if hh else "dve"))
                    p = self.ps()
                    for k in range(8):
                        self.mm(p[:, 0:NE], hTf[:, k, :], rw[:, k, :], start=(k == 0), stop=(k == 7))
                    self.tt(lg[:], p[:, 0:NE], rb[:], ALU.add)
                    self.vmax(mx[:], lg[:])
                    self.ts(msk[:], lg[:], mx[:, 3:4], None, ALU.is_ge)
                    self.ts(nmx[:], mx[:, 0:1], -1.0, None, ALU.mult)
                    self.act(ex[:], lg[:], AF.Exp, bias=nmx[:, 0:1], scale=1.0)
                    self.tt(ex[:], ex[:], msk[:], ALU.mult)
                    self.rsum(den[:], ex[:])
                    self.recip(den[:], den[:])
                    self.ts(ex[:], ex[:], den[:, 0:1], None, ALU.mult)
                    self.cp(mskb[:], msk[:])
                    p = self.ps()
                    self.mm(p[:, 0:NE], self.trib[:, 2, :], mskb[:])
                    self.mm(p[:, NE:2 * NE], self.trib[:, 3, :], mskb[:])
                    self.tt(pos[:], p[:, 0:NE], carry[:], ALU.add)
                    self.tt(carry[:], carry[:], p[:, NE:2 * NE], ALU.add)
                    self.ts(okm[:], pos[:], float(CAP), None, ALU.is_lt)
                    self.tt(ex[:], ex[:], okm[:], ALU.mult)
                    self.ts(pos[:], pos[:], float(CAP), None, ALU.min)
                    self.tt(pos[:], pos[:], ebase[:], ALU.add)
                    self.ts(sv[:], pos[:], -1.0, BIG, ALU.mult, ALU.add)
                    self.tt(sv[:], sv[:], msk[:], ALU.mult)
                    self.vmax(mx2[:], sv[:])
                    self.ts(slf[:], mx2[:, 0:4], -1.0, BIG, ALU.mult, ALU.add)
                    self.cp(SL[:, i, :], slf[:])
                    for k in range(4):
                        self.stt(junk[:], sv[:], mx2[:, k:k + 1], ex[:], ALU.is_equal, ALU.mult, accum_out=WS[:, i, k:k + 1])
                    for k in range(4):
                        idxt = sidx[(i % 2) * 4 + k]
                        self.cp(idxt[:], SL[:, i, k:k + 1])
                        idx = idxt[:, :]
                        rr, ww = self._rw([], [hb[:], idx], (), ())
                        self.P.op("pool", lambda e, idx=idx, hb=hb: e.indirect_dma_start(
                            out=self.XG[:, :], out_offset=bass.IndirectOffsetOnAxis(ap=idx, axis=0),
                            in_=hb[:], in_offset=None),
                            rr, [("XGs", i, k)], dma=True)

                self.phase_norm(l, 2, None, t0=t0, extra=route)
            self.P.barrier()
            with contextlib.ExitStack() as es:
                wu = [self.sb(es, f"wu{j}", [128, 8, 2048], BF16) for j in range(2)]
                wd = [self.sb(es, f"wd{j}", [128, 8, D], BF16) for j in range(2)]
                bu = [self.sb(es, f"bu{j}", [128, 16]) for j in range(2)]
                bd_ = [self.sb(es, f"bdn{j}", [128, D]) for j in range(2)]
                xg = [self.sb(es, f"xg{j}", [128, D], BF16) for j in range(2)]
                xgT = [self.sb(es, f"xgT{j}", [128, 8, 512], BF16) for j in range(2)]
                actT = self.sb(es, "actT", [128, 8, 512], BF16)
                gq = [self.sb(es, f"gq{j}", [128, 512]) for j in range(2)]
                sg = [self.sb(es, f"sg{j}", [128, 512]) for j in range(2)]
                lq = [self.sb(es, f"lq{j}", [128, 512]) for j in range(2)]
                yo = [self.sb(es, f"yo{j}", [128, D]) for j in range(2)]
                blocks = []
                c0 = 0
                while c0 < CAP:
                    w_ = min(512, CAP - c0)
                    blocks.append((c0, w_))
                    c0 += w_
                nb = 0
                ny = 0
                stg = [self.sb(es, f"stg{j}", [128, 2048]) for j in range(3)]
                nst = [0]

                def loader(e):
                    eb_ = e % 2
                    self.dma(bu[eb_][:], io["exp_b_up"][l, e:e + 1, :].rearrange("o (c p) -> (o p) c", p=128), allow_slow_non_contiguous=True)
                    self.bc_row(bd_[eb_][:], io["exp_b_down"][l, e:e + 1, :])
                    yield
                    for k in range(8):
                        st = stg[nst[0] % 3]
                        nst[0] += 1
                        self.dma(st[:], io["exp_w_up"][l, e, k * 128:(k + 1) * 128, :])
                        self.cp(wu[eb_][:, k, :], st[:], eng="act")
                        yield
                    for k in range(8):
                        st = stg[nst[0] % 3]
                        nst[0] += 1
                        self.dma(st[:, 0:D], io["exp_w_down"][l, e, k * 128:(k + 1) * 128, :])
                        self.cp(wd[eb_][:, k, :], st[:, 0:D], eng="pool")
                        yield

                for _ in loader(0):
                    pass
                for e in range(NE):
                    eb = e % 2
                    nxt = loader(e + 1) if e + 1 < NE else iter(())
                    for (c0, w_) in blocks:
                        xT = xgT[nb % 2]
                        nb += 1
                        for s in range(w_ // 128):
                            r0 = e * ESTR + c0 + s * 128
                            xt_ = xg[s % 2]
                            self.dma(xt_[:], self.XG[r0:r0 + 128, :], r=[("XGl", r0)])
                            pb = self.psb[s % 2]
                            for k in range(8):
                                self.tr(pb[:, k * 128:(k + 1) * 128], xt_[:, k * 128:(k + 1) * 128], self.identb[:])
                            self.cp(xT[:, :, s * 128:(s + 1) * 128], pb[:].rearrange("p (k n) -> p k n", k=8), eng=("act" if s % 2 else "dve"))
                        for fc in range(8):
                            pg_ = self.ps()
                            for k in range(8):
                                self.mm(pg_[:, 0:w_], wu[eb][:, k, fc * 128:(fc + 1) * 128], xT[:, k, 0:w_], start=(k == 0), stop=(k == 7))
                            pl_ = self.ps()
                            for k in range(8):
                                self.mm(pl_[:, 0:w_], wu[eb][:, k, 1024 + fc * 128:1024 + (fc + 1) * 128], xT[:, k, 0:w_], start=(k == 0), stop=(k == 7))
                            g_ = gq[fc % 2]; s_ = sg[fc % 2]; l_ = lq[fc % 2]
                            self.ts(g_[:, 0:w_], pg_[:, 0:w_], bu[eb][:, fc:fc + 1], 7.0, ALU.add, ALU.min)
                            self.act(s_[:, 0:w_], g_[:, 0:w_], AF.Sigmoid, scale=1.702)
                            self.ts(l_[:, 0:w_], pl_[:, 0:w_], bu[eb][:, 8 + fc:9 + fc], 7.0, ALU.add, ALU.min)
                            self.ts(l_[:, 0:w_], l_[:, 0:w_], -7.0, 1.0, ALU.max, ALU.add, eng="pool")
                            self.tt(g_[:, 0:w_], g_[:, 0:w_], s_[:, 0:w_], ALU.mult, eng="pool")
                            self.tt(actT[:, fc, 0:w_], g_[:, 0:w_], l_[:, 0:w_], ALU.mult, eng="pool")
                            next(nxt, None)
                        for s in range(w_ // 128):
                            r0 = e * ESTR + c0 + s * 128
                            o = yo[ny % 2]
                            ny += 1
                            for hf in range(2):
                                p = self.ps()
                                for fc in range(8):
                                    self.mm(p[:], actT[:, fc, s * 128:(s + 1) * 128], wd[eb][:, fc, hf * 512:(hf + 1) * 512], start=(fc == 0), stop=(fc == 7))
                                self.tt(o[:, hf * 512:(hf + 1) * 512], p[:], bd_[eb][:, hf * 512:(hf + 1) * 512], ALU.add)
                            self.dma(self.YG[r0:r0 + 128, :], o[:], w=[("YGs", r0)])
                    for _ in nxt:
                        pass
            self.P.barrier()
            with contextlib.ExitStack() as es:
                G2 = [self.sb(es, f"G2{j}", [128, D]) for j in range(2)]
                for j in range(2):
                    self.bc_row(G2[j][:], self.MOD[l, 1 - j:2 - j, 5 * D:6 * D])
                gk = self.p_gk
                xt = [self.sb(es, f"xc{j}", [128, D]) for j in range(2)]
                acc = self.sb(es, "cacc", [128, D])
                cidx = self.p_idx
                for i in range(t0, NT):
                    b = i % 2
                    j = 0 if i < 2 else 1
                    self.dma(xt[b][:], self.XR[i * 128:(i + 1) * 128, :], r=[("XR", i)])
                    for k in range(4):
                        idxt = cidx[(i % 2) * 4 + k]
                        self.cp(idxt[:], SL[:, i, k:k + 1])
                        idx = idxt[:, :]
                        gk_ = gk[k]
                        rr, ww = self._rw([gk_[:]], [idx], (), ())
                        self.P.op("pool", lambda e, idx=idx, gk_=gk_: e.indirect_dma_start(
                            out=gk_[:], out_offset=None, in_=self.YG[:, :],
                            in_offset=bass.IndirectOffsetOnAxis(ap=idx, axis=0)), rr, ww, dma=True)
                    self.ts(acc[:], gk[0][:], WS[:, i, 0:1], None, ALU.mult)
                    for k in range(1, 4):
                        self.stt(acc[:], gk[k][:], WS[:, i, k:k + 1], acc[:], ALU.mult, ALU.add)
                    self.tt(acc[:], acc[:], G2[j][:], ALU.mult)
                    self.tt(xt[b][:], xt[b][:], acc[:], ALU.add)
                    self.dma(self.XR[i * 128:(i + 1) * 128, :], xt[b][:], w=[("XR", i)])
        self.P.barrier()

    def phase_final(self):
        io = self.io
        with contextlib.ExitStack() as es:
            g = self.sb(es, "gfin", [128, D])
            self.bc_row(g[:], io["norm_final"])
            xt = [self.sb(es, f"xf{j}", [128, D]) for j in range(2)]
            sq = self.sb(es, "sqf", [128, D])
            ss = [self.sb(es, f"ssf{j}", [128, 1]) for j in range(2)]
            o = [self.sb(es, f"of{j}", [128, D]) for j in range(2)]
            for i in range(2, NT):
                b = i % 2
                self.dma(xt[b][:], self.XR[i * 128:(i + 1) * 128, :], r=[("XR", i)])
                self.act(sq[:], xt[b][:], AF.Square, accum_out=ss[b][:])
                self.ts(ss[b][:], ss[b][:], 1.0 / D, EPS, ALU.mult, ALU.add)
                self.act(ss[b][:], ss[b][:], AF.Sqrt)
                self.recip(ss[b][:], ss[b][:])
                self.stt(o[b][:], xt[b][:], ss[b][:, 0:1], g[:], ALU.mult, ALU.mult)
                self.dma(io["out"][(i - 2) * 128:(i - 1) * 128, :], o[b][:], w=[("out", i)])


W_NAMES = ['w_mod', 'b_mod', 'norm_mix', 'norm_ffn', 'w_in', 'hy_conv_w', 'hy_conv_b', 'hy_ffn_w1', 'hy_ffn_b1',
           'hy_ffn_w2', 'hy_ffn_b2', 'hy_ffn_w3', 'hy_bias', 'pool_w', 'pool_scale', 'ssm_conv_w', 'ssm_conv_b',
           'ssm_dt_bias', 'ssm_a_log', 'ssm_d', 'ssm_norm', 'w_branch', 'w_gate', 'w_out', 'router_w', 'router_b',
           'exp_w_up', 'exp_b_up', 'exp_w_down', 'exp_b_down']

_CONST_CACHE = {}


def make_constants():
    if _CONST_CACHE:
        return _CONST_CACHE
    bf = ml_dtypes.bfloat16
    c = {}
    c["cst_ident"] = np.eye(128, dtype=np.float32)
    t = np.arange(128)
    tri = np.zeros((4, 128, 128), np.float32)
    tri[0] = (t[:, None] <= t[None, :])
    tri[1] = (t[:, None] >= t[None, :])
    tri[2] = (t[:, None] < t[None, :])
    tri[3] = 1.0
    c["cst_tri"] = tri
    misc = np.ones((128, 8), np.float32)
    misc[:, 0] = (-1.0) ** t
    misc[0, 1] = 0.0
    misc[0, 2] = 0.5
    c["cst_misc"] = misc
    c["cst_altrow"] = ((-1.0) ** np.arange(4096)).astype(np.float32).reshape(1, 4096).astype(bf)
    m = np.arange(64)
    a64 = 2 * np.pi * np.outer(m, m) / 64.0
    bd = np.zeros((2, 128, 128), np.float32)
    for g in range(2):
        bd[0, g * 64:(g + 1) * 64, g * 64:(g + 1) * 64] = np.cos(a64)
        bd[1, g * 64:(g + 1) * 64, g * 64:(g + 1) * 64] = np.sin(a64)
    c["cst_bd64"] = bd

    def dft(n, period):
        k = np.arange(n, dtype=np.int64)
        ph = (np.outer(k, k) % period).astype(np.float64) * (2 * np.pi / period)
        out = np.empty((2, n, n), bf)
        out[0] = np.cos(ph).astype(np.float32).astype(bf)
        out[1] = (-np.sin(ph)).astype(np.float32).astype(bf)
        return out

    def tiled(t):
        return np.ascontiguousarray(t.reshape(2, 32, 128, 16, 256).transpose(3, 2, 0, 1, 4))

    c["cst_dft4"] = tiled(dft(NLAT, NLAT))
    c["cst_dft4c"] = dft(NCTX, NCTX)
    c["cst_dft8"] = tiled(dft(NLAT, 2 * NLAT))
    c["cst_dft8c"] = dft(NCTX, 2 * NCTX)

    def poolmat(row_len, nrows):
        n = row_len * nrows
        out = np.zeros((4, n, n), np.float32)
        pos = np.arange(row_len)
        for gi, win in enumerate((2, 4, 8, 16)):
            lo = np.clip(pos - win // 2, 0, row_len)
            hi = np.clip(pos + win // 2, 0, row_len)
            blk = np.zeros((row_len, row_len), np.float32)
            for tt in range(row_len):
                blk[tt, lo[tt]:hi[tt]] = 1.0 / float(hi[tt] - lo[tt])
            blk -= np.eye(row_len, dtype=np.float32)
            for r in range(nrows):
                out[gi, r * row_len:(r + 1) * row_len, r * row_len:(r + 1) * row_len] = blk.T
        return out

    c["cst_pool"] = poolmat(64, 2)
    c["cst_poolc"] = poolmat(256, 1)

    def feats(n):
        pos = np.arange(n, dtype=np.float32)
        tt = pos / np.float32(n - 1)
        ang = (np.float32(2.0 * math.pi) * pos / np.float32(n)).astype(np.float32)
        freqs = np.linspace(1e-4, 15, 16, dtype=np.float32)
        f = np.concatenate([tt[:, None], np.cos(ang[:, None] * freqs), -np.sin(ang[:, None] * freqs)], axis=-1).astype(np.float32)
        return np.ascontiguousarray(f.T), tt

    fl, tl = feats(NLAT)
    fc, tc = feats(NCTX)
    c["cst_feat"] = fl
    c["cst_featc"] = fc
    tneg = np.zeros((128, NT), np.float32)
    tneg[:, 0:2] = -tc.reshape(2, 128).T
    tneg[:, 2:] = -tl.reshape(32, 128).T
    c["cst_tneg"] = tneg
    deltas = np.linspace(HY_MIN, HY_MAX, 256, dtype=np.float32)
    c["cst_delta2"] = np.ascontiguousarray(np.broadcast_to(np.concatenate([deltas, deltas])[None, :], (128, 512))).astype(np.float32)
    c["cst_ebase"] = np.ascontiguousarray(np.broadcast_to((np.arange(NE) * ESTR).astype(np.float32)[None, :], (128, NE)))
    _CONST_CACHE.update(c)
    return c


def build_program(cfg):
    nc = bass.Bass("TRN2", target_bir_lowering=False)
    io = {}

    def inp(name, shape, dt=F32):
        io[name] = nc.dram_tensor(name, list(shape), dt, kind="ExternalInput").ap()

    inp("x", [NLAT, D]); inp("ctx", [NCTX, D]); inp("c", [1, D]); inp("c_ctx", [1, D])
    shapes = cfg["shapes"]
    for n in W_NAMES:
        inp(n, shapes[n])
    inp("norm_final", [1, D])
    consts = make_constants()
    for n, a in consts.items():
        inp(n, a.shape, BF16 if a.dtype == ml_dtypes.bfloat16 else F32)
    io["out"] = nc.dram_tensor("out", [NLAT, D], F32, kind="ExternalOutput").ap()
    k = Kern(nc, io, cfg)
    k.build()
    return nc, k


def kernel(**inputs):
    cfg = {"shapes": {n: list(inputs[n].shape) for n in W_NAMES}}
    env = os.environ
    if env.get("MK_LAYERS"):
        cfg["layers"] = int(env["MK_LAYERS"])
    if env.get("MK_PHASES"):
        cfg["phases"] = env["MK_PHASES"]
    if env.get("MK_DUMP"):
        cfg["dump"] = tuple(env["MK_DUMP"].split(","))
    nc, k = build_program(cfg)
    consts = make_constants()
    shared = {n: np.ascontiguousarray(inputs[n], dtype=np.float32) for n in W_NAMES}
    shared["norm_final"] = np.ascontiguousarray(inputs["norm_final"], dtype=np.float32).reshape(1, D)
    shared["c_ctx"] = np.ascontiguousarray(inputs["c_ctx"], dtype=np.float32).reshape(1, D)
    shared.update(consts)
    ncore = int(env.get("MK_CORES", "8"))
    in_maps = []
    for b in range(ncore):
        m = dict(shared)
        m["x"] = np.ascontiguousarray(inputs["x"][b], dtype=np.float32)
        m["ctx"] = np.ascontiguousarray(inputs["ctx"][b], dtype=np.float32)
        m["c"] = np.ascontiguousarray(inputs["c"][b], dtype=np.float32).reshape(1, D)
        in_maps.append(m)
    res = run_bass_kernel_spmd(nc, in_maps, core_ids=list(range(ncore)))
    out = np.zeros((8, NLAT, D), np.float32)
    for b in range(ncore):
        out[b] = res.results[b]["out"]
    if cfg.get("dump"):
        kernel.last_results = res.results
    return out
```

```python
import os
import math
import contextlib
import numpy as np
import ml_dtypes
import concourse.bass as bass
import concourse.mybir as mybir
from concourse.bass_utils import run_bass_kernel_spmd

F32 = mybir.dt.float32
BF16 = mybir.dt.bfloat16
I32 = mybir.dt.int32
AF = mybir.ActivationFunctionType
ALU = mybir.AluOpType
AX = mybir.AxisListType

SEM_ROT = 30000
DMA_SLOTS = 6

D = 1024
NLAT = 4096
NCTX = 256
NTOK = NLAT + NCTX
NT = NTOK // 128
DEPTH = 4
INW = 2832
NE = 32
CAP = 1280
ESTR = CAP + 8
BIG = 65536.0
EPS = 1e-6
HY_MIN = -math.log(1e-2) / 1.5
HY_MAX = -math.log(1e-2) / 0.3


class Prog:
    ENG = ("pe", "act", "dve", "pool", "sp")

    def __init__(self, nc):
        self.nc = nc
        self.q = {e: [] for e in self.ENG}
        self.cnt = {e: 0 for e in self.ENG}
        self.gen = {e: 0 for e in self.ENG}
        self.clock = {e: {} for e in self.ENG}
        self.res = {}
        self.semnames = []
        self.dma_slots = {e: [[self._newsem(f"dq_{e}_{i}"), 0] for i in range(DMA_SLOTS)]
                          for e in ("sp", "act", "pool")}
        self.dma_i = {e: 0 for e in ("sp", "act", "pool")}
        self.cursem = {e: self._newsem(f"s_{e}_0") for e in self.ENG}
        self.nops = 0

    def _newsem(self, name):
        self.semnames.append(name)
        return name

    def op(self, eng, fn, reads=(), writes=(), dma=False):
        deps = []
        for k in reads:
            r = self.res.get(k)
            if r is not None and r[0] is not None:
                deps.append(r[0])
        for k in writes:
            r = self.res.get(k)
            if r is not None:
                if r[0] is not None:
                    deps.append(r[0])
                deps.extend(r[1])
        clk = self.clock[eng]
        if dma:
            slots = self.dma_slots[eng]
            slot = slots[self.dma_i[eng] % len(slots)]
            self.dma_i[eng] += 1
            if slot[1] > 0:
                deps.append((slot[0], slot[1], None))
            if slot[1] + 16 > SEM_ROT:
                slot[0] = self._newsem(slot[0] + "r")
                slot[1] = 0
            slot[1] += 16
            ev_sem, ev_val, inc = slot[0], slot[1], 16
        else:
            if self.cnt[eng] + 1 > SEM_ROT:
                self.gen[eng] += 1
                self.cursem[eng] = self._newsem(f"s_{eng}_{self.gen[eng]}")
                self.cnt[eng] = 0
            self.cnt[eng] += 1
            ev_sem, ev_val, inc = self.cursem[eng], self.cnt[eng], 1
        waits = {}
        for (s, v, c) in deps:
            if eng == "pe" and s.startswith("s_pe_"):
                continue
            if clk.get(s, 0) >= v:
                continue
            if waits.get(s, 0) < v:
                waits[s] = v
        for (s, v, c) in deps:
            if s in waits:
                if c:
                    for ks, kv in c.items():
                        if clk.get(ks, 0) < kv:
                            clk[ks] = kv
                if clk.get(s, 0) < v:
                    clk[s] = v
        evclk = dict(clk)
        evclk[ev_sem] = ev_val
        ev = (ev_sem, ev_val, evclk)
        self.q[eng].append((list(waits.items()), fn, ev_sem, inc))
        self.nops += 1
        for k in writes:
            self.res[k] = [ev, []]
        for k in reads:
            if k in writes:
                continue
            r = self.res.get(k)
            if r is None:
                self.res[k] = [None, [ev]]
            else:
                r[1].append(ev)
                if len(r[1]) > 24:
                    best = {}
                    for e_ in r[1]:
                        if e_[0] not in best or best[e_[0]][1] < e_[1]:
                            best[e_[0]] = e_
                    r[1] = list(best.values())
        return ev

    def barrier(self, engines=None):
        latest = {}
        for r in self.res.values():
            evs = list(r[1])
            if r[0] is not None:
                evs.append(r[0])
            for (s, v, c) in evs:
                if latest.get(s, 0) < v:
                    latest[s] = v
        for e in self.ENG:
            for s, v in self.dma_slots.get(e, []):
                if v > 0 and latest.get(s, 0) < v:
                    latest[s] = v
            if self.cnt[e] > 0 and latest.get(self.cursem[e], 0) < self.cnt[e]:
                latest[self.cursem[e]] = self.cnt[e]
        for e in (engines or self.ENG):
            clk = self.clock[e]
            waits = [(s, v) for s, v in latest.items() if clk.get(s, 0) < v]
            for s, v in waits:
                clk[s] = v
            if waits:
                self.q[e].append((waits, None, None, 0))
        self.res = {}

    def emit(self):
        nc = self.nc
        sems = {n: nc.alloc_semaphore(name=n) for n in self.semnames}
        engmap = {"pe": "tensor", "act": "scalar", "dve": "vector", "pool": "gpsimd", "sp": "sync"}
        with nc.Block() as block:
            for e in self.ENG:
                ops = self.q[e]

                def body(eng, ops=ops):
                    for waits, fn, ev_sem, inc in ops:
                        for s, v in waits:
                            eng.wait_ge(sems[s], v)
                        if fn is not None:
                            ins = fn(eng)
                            ins.then_inc(sems[ev_sem], inc)

                getattr(block, engmap[e])(body)


def _nm(ap):
    return ap.name


class Kern:
    def __init__(self, nc, io, cfg):
        self.nc = nc
        self.io = io
        self.cfg = cfg
        self.P = Prog(nc)
        self.uid = 0
        self.psi = 0

    def sb(self, es, name, shape, dt=F32):
        self.uid += 1
        return es.enter_context(self.nc.sbuf_tensor(f"{name}_{self.uid}", list(shape), dt))

    def dram(self, name, shape, dt=F32):
        kind = "ExternalOutput" if name in self.cfg.get("dump", ()) else "Internal"
        return self.nc.dram_tensor(name, list(shape), dt, kind=kind).ap()

    def ps(self):
        t = self.psf[self.psi % len(self.psf)]
        self.psi += 1
        return t

    def hold_ps(self, n):
        held = [self.psf.pop() for _ in range(n)]
        return held

    def release_ps(self, held):
        self.psf.extend(held)

    def _rw(self, outs, ins, r, w):
        reads = list(r)
        writes = list(w)
        for a in ins:
            if a is None or isinstance(a, (int, float)):
                continue
            n = _nm(a)
            if n.startswith("ps"):
                writes.append(n)
            else:
                reads.append(n)
        for a in outs:
            writes.append(_nm(a))
        return reads, writes

    def dma(self, out, in_, eng="sp", r=None, w=None, **kw):
        reads = list(r) if r is not None else [_nm(in_)]
        writes = list(w) if w is not None else [_nm(out)]
        self.P.op(eng, lambda e: e.dma_start(out=out, in_=in_, **kw), reads, writes, dma=True)

    def mm(self, out, lhsT, rhs, start=True, stop=True):
        reads, writes = self._rw([out], [lhsT, rhs], (), ())
        self.P.op("pe", lambda e: e.matmul(out, lhsT=lhsT, rhs=rhs, start=start, stop=stop), reads, writes)

    def tr(self, out, in_, ident):
        reads, writes = self._rw([out], [in_, ident], (), ())
        self.P.op("pe", lambda e: e.transpose(out=out, in_=in_, identity=ident), reads, writes)

    def act(self, out, in_, func, bias=None, scale=None, accum_out=None, eng="act"):
        kw = {}
        if bias is not None:
            kw["bias"] = bias
        if scale is not None:
            kw["scale"] = scale
        outs = [out]
        if accum_out is not None:
            kw["accum_out"] = accum_out
            outs.append(accum_out)
        reads, writes = self._rw(outs, [in_, bias, scale], (), ())
        self.P.op(eng, lambda e: e.activation(out=out, in_=in_, func=func, **kw), reads, writes)

    def cp(self, out, in_, eng="dve"):
        reads, writes = self._rw([out], [in_], (), ())
        if eng == "act":
            self.P.op("act", lambda e: e.copy(out=out, in_=in_), reads, writes)
        else:
            self.P.op(eng, lambda e: e.tensor_copy(out=out, in_=in_), reads, writes)

    def tt(self, out, in0, in1, op, eng="dve"):
        reads, writes = self._rw([out], [in0, in1], (), ())
        self.P.op(eng, lambda e: e.tensor_tensor(out=out, in0=in0, in1=in1, op=op), reads, writes)

    def ts(self, out, in0, s1, s2, op0, op1=None, eng="dve"):
        reads, writes = self._rw([out], [in0, s1, s2], (), ())
        if op1 is None:
            self.P.op(eng, lambda e: e.tensor_scalar(out=out, in0=in0, scalar1=s1, scalar2=None, op0=op0), reads, writes)
        else:
            self.P.op(eng, lambda e: e.tensor_scalar(out=out, in0=in0, scalar1=s1, scalar2=s2, op0=op0, op1=op1), reads, writes)

    def stt(self, out, in0, scalar, in1, op0, op1, accum_out=None, eng="dve"):
        outs = [out]
        kw = {}
        if accum_out is not None:
            kw["accum_out"] = accum_out
            outs.append(accum_out)
        reads, writes = self._rw(outs, [in0, scalar, in1], (), ())
        self.P.op(eng, lambda e: e.scalar_tensor_tensor(out=out, in0=in0, scalar=scalar, in1=in1, op0=op0, op1=op1, **kw), reads, writes)

    def memset(self, ap, val, eng="dve"):
        reads, writes = self._rw([ap], [], (), ())
        self.P.op(eng, lambda e: e.memset(ap, val), reads, writes)

    def vmax(self, out, in_):
        reads, writes = self._rw([out], [in_], (), ())
        self.P.op("dve", lambda e: e.max(out=out, in_=in_), reads, writes)

    def recip(self, out, in_):
        reads, writes = self._rw([out], [in_], (), ())
        self.P.op("dve", lambda e: e.reciprocal(out=out, in_=in_), reads, writes)

    def rsum(self, out, in_):
        reads, writes = self._rw([out], [in_], (), ())
        self.P.op("dve", lambda e: e.reduce_sum(out=out, in_=in_, axis=AX.X), reads, writes)

    def build(self):
        nc, io, cfg = self.nc, self.io, self.cfg
        layers = cfg.get("layers", DEPTH)
        with contextlib.ExitStack() as es:
            self.psf = [es.enter_context(nc.psum_tensor(f"psF{i}", [128, 512], F32)) for i in range(6)]
            self.psb = [es.enter_context(nc.psum_tensor(f"psB{i}", [128, 1024], BF16)) for i in range(2)]
            self.identf = self.sb(es, "identf", [128, 128])
            self.identb = self.sb(es, "identb", [128, 128], BF16)
            self.tri = self.sb(es, "tri", [128, 4, 128])
            self.trib = self.sb(es, "trib", [128, 4, 128], BF16)
            self.misc = self.sb(es, "misc", [128, 8])
            self.dma(self.identf[:], io["cst_ident"])
            self.dma(self.tri[:], io["cst_tri"].rearrange("a p n -> p a n"))
            self.dma(self.misc[:], io["cst_misc"])
            self.p_hb = [self.sb(es, f"p_hb{j}", [128, D], BF16) for j in range(2)]
            self.p_idx = [self.sb(es, f"p_idx{j}", [128, 1], I32) for j in range(8)]
            self.p_gk = [self.sb(es, f"p_gk{j}", [128, D]) for j in range(4)]
            self.cp(self.identb[:], self.identf[:])
            self.cp(self.trib[:], self.tri[:])
            self.XR = self.dram("XR", [NTOK, D])
            self.MOD = self.dram("MOD", [DEPTH, 2, 6 * D])
            self.FM = self.dram("FM", [2048 + 256, NTOK])
            self.TM = self.dram("TM", [NTOK, 528])
            self.YS = self.dram("YS", [1280, NTOK], BF16)
            self.MG = self.dram("MG", [D, NTOK], BF16)
            self.X1 = self.dram("X1", [256, NTOK])
            self.VV = self.dram("VV", [256, NTOK])
            self.XBC = self.dram("XBC", [1024, NTOK], BF16)
            self.YF = self.dram("YF", [NTOK, 512])
            self.XG = self.dram("XG", [NE * ESTR, D], BF16)
            self.YG = self.dram("YG", [NE * ESTR, D])
            self.dma(self.XR[0:NCTX, :], io["ctx"], w=[("XR", 0), ("XR", 1)])
            for q in range(4):
                self.dma(self.XR[NCTX + q * 1024:NCTX + (q + 1) * 1024, :], io["x"][q * 1024:(q + 1) * 1024, :], w=[("XRi", q)])
            with contextlib.ExitStack() as es0:
                z = self.sb(es0, "zrow", [8, D])
                self.memset(z[:], 0.0)
                for e in range(NE):
                    self.dma(self.YG[e * ESTR + CAP:(e + 1) * ESTR, :], z[:], w=[("YGz", e)])
                self.P.barrier()
            self.phase_mods()
            for l in range(layers):
                self.layer(l)
            self.phase_final()
            self.P.barrier(["sp"])
            self.P.emit()

    def phase_mods(self):
        io = self.io
        with contextlib.ExitStack() as es:
            cc = self.sb(es, "cc", [128, 8, 2])
            ccs = self.sb(es, "ccs", [128, 8, 2])
            self.dma(cc[:, :, 0], io["c"].rearrange("o (k p) -> (o p) k", p=128), allow_slow_non_contiguous=True)
            self.dma(cc[:, :, 1], io["c_ctx"].rearrange("o (k p) -> (o p) k", p=128), allow_slow_non_contiguous=True)
            self.act(ccs[:], cc[:], AF.Silu)
            wm = [self.sb(es, f"wm{i}", [128, 8, 512]) for i in range(2)]
            bm = self.sb(es, "bm", [2, 6 * D])
            mo = self.sb(es, "mo", [2, 6 * D])
            for l in range(DEPTH):
                self.dma(bm[:], io["b_mod"][l:l + 1, :].partition_broadcast(2))
                for nb in range(12):
                    w = wm[nb % 2]
                    for k in range(8):
                        self.dma(w[:, k, :], io["w_mod"][l, k * 128:(k + 1) * 128, nb * 512:(nb + 1) * 512])
                    p = self.ps()
                    for k in range(8):
                        self.mm(p[0:2, :], ccs[:, k, :], w[:, k, :], start=(k == 0), stop=(k == 7))
                    self.tt(mo[:, nb * 512:(nb + 1) * 512], p[0:2, :], bm[:, nb * 512:(nb + 1) * 512], ALU.add)
                self.dma(self.MOD[l], mo[:], w=[("MOD", l)])
        self.P.barrier()

    def bc_row(self, dst, src_row):
        self.dma(dst, src_row.partition_broadcast(128))

    def phase_norm(self, l, which, hT, t0=0, extra=None):
        io = self.io
        gname = "norm_mix" if which == 1 else "norm_ffn"
        o_sh, o_sc = (0, 1) if which == 1 else (3, 4)
        with contextlib.ExitStack() as es:
            A = [self.sb(es, f"A{j}", [128, D]) for j in range(2)]
            B = [self.sb(es, f"Bv{j}", [128, D]) for j in range(2)]
            g = self.sb(es, "g", [128, D])
            self.bc_row(g[:], io[gname][l:l + 1, :])
            for j in range(2):
                row = 1 - j
                self.bc_row(A[j][:], self.MOD[l, row:row + 1, o_sc * D:(o_sc + 1) * D])
                self.bc_row(B[j][:], self.MOD[l, row:row + 1, o_sh * D:(o_sh + 1) * D])
                self.stt(A[j][:], A[j][:], 1.0, g[:], ALU.add, ALU.mult)
            xt = [self.sb(es, f"xt{j}", [128, D]) for j in range(2)]
            sq = self.sb(es, "sq", [128, D])
            hf = [self.sb(es, f"hf{j}", [128, D]) for j in range(2)]
            hb = self.p_hb
            ss = [self.sb(es, f"ss{j}", [128, 1]) for j in range(2)]
            for i in range(t0, NT):
                j = 0 if i < 2 else 1
                b = i % 2
                self.dma(xt[b][:], self.XR[i * 128:(i + 1) * 128, :], r=[("XR", i)])
                self.act(sq[:], xt[b][:], AF.Square, accum_out=ss[b][:])
                self.ts(ss[b][:], ss[b][:], 1.0 / D, EPS, ALU.mult, ALU.add)
                self.act(ss[b][:], ss[b][:], AF.Sqrt)
                self.recip(ss[b][:], ss[b][:])
                self.stt(hf[b][:], xt[b][:], ss[b][:, 0:1], A[j][:], ALU.mult, ALU.mult)
                self.tt(hf[b][:], hf[b][:], B[j][:], ALU.add)
                self.cp(hb[b][:], hf[b][:], eng="act")
                if hT is not None:
                    pb = self.psb[i % 2]
                    for k in range(8):
                        self.tr(pb[:, k * 128:(k + 1) * 128], hb[b][:, k * 128:(k + 1) * 128], self.identb[:])
                    self.cp(hT[:, :, i * 128:(i + 1) * 128], pb[:].rearrange("p (k n) -> p k n", k=8))
                if extra is not None:
                    extra(i, hf[b], hb[b])
            self.P.barrier()

    def layer(self, l):
        cfg = self.cfg
        ph = cfg.get("phases", "WFHPSGM")
        if "W" in ph:
            self.phase_proj(l)
        if "F" in ph:
            self.phase_fourier(l)
        if "H" in ph:
            self.phase_hyena(l)
        if "P" in ph:
            self.phase_pool(l)
        if "S" in ph:
            self.phase_ssd(l)
        if "G" in ph:
            self.phase_merge(l)
        if "M" in ph:
            self.phase_moe(l)

    def tokblocks(self):
        return [(0, 256)] + [(NCTX + 512 * j, 512) for j in range(8)]

    def phase_proj(self, l):
        io = self.io
        with contextlib.ExitStack() as es:
            hT = self.sb(es, "hT", [128, 8, NTOK], BF16)
            wi = self.sb(es, "wi", [128, 8, INW], BF16)
            wst = [self.sb(es, f"wst{j}", [128, INW]) for j in range(2)]
            for k in range(8):
                self.dma(wst[k % 2][:], io["w_in"][l, k * 128:(k + 1) * 128, :])
                self.cp(wi[:, k, :], wst[k % 2][:], eng=("act" if k % 2 else "pool"))
            self.phase_norm(l, 1, hT)
            ob = [self.sb(es, f"ob{j}", [128, 512]) for j in range(3)]
            fm_cols = [(0, 256), (256, 768), (1792, 1024), (1024, 256)]
            fm_chunks = []
            for c0, n in fm_cols:
                for j in range(n // 128):
                    fm_chunks.append(c0 + j * 128)
            n = 0
            for (t0, tw) in self.tokblocks():
                for oc, c0 in enumerate(fm_chunks):
                    p = self.ps()
                    for k in range(8):
                        self.mm(p[:, 0:tw], wi[:, k, c0:c0 + 128], hT[:, k, t0:t0 + tw], start=(k == 0), stop=(k == 7))
                    o = ob[n % 3]
                    n += 1
                    if n % 2:
                        self.cp(o[:, 0:tw], p[:, 0:tw], eng="act")
                    else:
                        self.cp(o[:, 0:tw], p[:, 0:tw])
                    self.dma(self.FM[oc * 128:(oc + 1) * 128, t0:t0 + tw], o[:, 0:tw], w=[("FM", oc, t0)])
            for i in range(NT):
                p = self.ps()
                for k in range(8):
                    self.mm(p[:, 0:512], hT[:, k, i * 128:(i + 1) * 128], wi[:, k, 1280:1792], start=(k == 0), stop=(k == 7))
                p2 = self.ps()
                for k in range(8):
                    self.mm(p2[:, 0:16], hT[:, k, i * 128:(i + 1) * 128], wi[:, k, 2816:2832], start=(k == 0), stop=(k == 7))
                o = ob[n % 3]
                n += 1
                self.cp(o[:, 0:512], p[:, 0:512], eng="act")
                self.dma(self.TM[i * 128:(i + 1) * 128, 0:512], o[:, 0:512], w=[("TM", i)])
                o = ob[n % 3]
                n += 1
                self.cp(o[:, 0:16], p2[:, 0:16])
                self.dma(self.TM[i * 128:(i + 1) * 128, 512:528], o[:, 0:16], w=[("TMd", i)])
        self.P.barrier()

    def phase_fourier(self, l):
        io = self.io
        with contextlib.ExitStack() as es:
            fT = self.sb(es, "fT", [128, 2, NTOK], BF16)
            for c in range(2):
                self.dma(fT[:, c, :], self.FM[c * 128:(c + 1) * 128, :], eng="pool")
            bd = self.sb(es, "bd", [128, 2, 128], BF16)
            self.dma(bd[:], io["cst_bd64"].rearrange("a p n -> p a n"), eng="pool")
            U = self.sb(es, "U", [128, NT, 512], BF16)
            for i in range(NT):
                p = self.ps()
                for a in range(2):
                    for c in range(2):
                        self.mm(p[:, a * 256 + c * 128:a * 256 + (c + 1) * 128], fT[:, c, i * 128:(i + 1) * 128], bd[:, a, :])
                self.cp(U[:, i, :], p[:], eng=("act" if i % 2 else "dve"))
            ys = [self.sb(es, f"ysf{j}", [128, 512], BF16) for j in range(2)]
            n = 0
            tbc = self.sb(es, "tbc", [128, 2, 2, 256], BF16)
            self.dma(tbc[:].rearrange("p a r c -> p (a r) c"), io["cst_dft4c"].rearrange("a (r p) c -> p (a r) c", p=128))
            sc_c = 1.0 / math.sqrt(NCTX * 64.0)
            for c in range(2):
                p = self.ps()
                cnt = 0
                for tc in range(2):
                    for a in range(2):
                        self.mm(p[:, 0:256], U[:, tc, a * 256 + c * 128:a * 256 + (c + 1) * 128], tbc[:, a, tc, :], start=(cnt == 0), stop=(cnt == 3))
                        cnt += 1
                o = ys[n % 2]
                n += 1
                self.act(o[:, 0:256], p[:, 0:256], AF.Copy, scale=sc_c)
                self.dma(self.YS[c * 128:(c + 1) * 128, 0:256], o[:, 0:256], w=[("YS", c, 0)])
            tbs = [self.sb(es, f"tb{j}", [128, 2, 32, 256], BF16) for j in range(2)]
            sc_l = 1.0 / math.sqrt(NLAT * 64.0)
            for kb in range(16):
                tb = tbs[kb % 2]
                for a in range(2):
                    self.dma(tb[:, a, :, :], io["cst_dft4"][kb, :, a, :, :])
                for c in range(2):
                    p = self.ps()
                    cnt = 0
                    for tc in range(32):
                        for a in range(2):
                            self.mm(p[:, 0:256], U[:, 2 + tc, a * 256 + c * 128:a * 256 + (c + 1) * 128], tb[:, a, tc, :], start=(cnt == 0), stop=(cnt == 63))
                            cnt += 1
                    o = ys[n % 2]
                    n += 1
                    self.act(o[:, 0:256], p[:, 0:256], AF.Copy, scale=sc_l)
                    self.dma(self.YS[c * 128:(c + 1) * 128, NCTX + kb * 256:NCTX + (kb + 1) * 256], o[:, 0:256], w=[("YS", c, kb + 1)])
        self.P.barrier()

    def conv_chunk(self, es_bufs, src_rows, wcol, bcol, out_ap_fn):
        xin, o = es_bufs
        self.dma(xin[:, 1:1 + NCTX], src_rows[:, 0:NCTX])
        self.dma(xin[:, 259:259 + NLAT], src_rows[:, NCTX:NTOK])
        for (b0, n, o0) in ((0, NCTX, 0), (258, NLAT, NCTX)):
            self.ts(o[:, o0:o0 + n], xin[:, b0:b0 + n], wcol[:, 0:1], bcol, ALU.mult, ALU.add)
            self.stt(o[:, o0:o0 + n], xin[:, b0 + 1:b0 + 1 + n], wcol[:, 1:2], o[:, o0:o0 + n], ALU.mult, ALU.add)
            self.stt(o[:, o0:o0 + n], xin[:, b0 + 2:b0 + 2 + n], wcol[:, 2:3], o[:, o0:o0 + n], ALU.mult, ALU.add)
        out_ap_fn(o)

    def phase_hyena(self, l):
        io = self.io
        with contextlib.ExitStack() as es_outer:
            Kh = self.sb(es_outer, "Kh", [128, NT, 512], BF16)
            KN = self.sb(es_outer, "KN", [1, 2, 256])
            rn = self.sb(es_outer, "rn", [128, 2, 2])
            with contextlib.ExitStack() as es:
                w1 = self.sb(es, "w1", [33, 64]); w2 = self.sb(es, "w2", [64, 64]); w3 = self.sb(es, "w3", [64, 512])
                b1 = self.sb(es, "b1", [64, 1]); b2 = self.sb(es, "b2", [64, 1])
                self.dma(w1[:], io["hy_ffn_w1"][l]); self.dma(w2[:], io["hy_ffn_w2"][l]); self.dma(w3[:], io["hy_ffn_w3"][l])
                self.dma(b1[:], io["hy_ffn_b1"][l:l + 1, :].rearrange("o n -> n o"), allow_slow_non_contiguous=True)
                self.dma(b2[:], io["hy_ffn_b2"][l:l + 1, :].rearrange("o n -> n o"), allow_slow_non_contiguous=True)
                d2 = self.sb(es, "d2", [128, 512])
                self.dma(d2[:], io["cst_delta2"])
                tneg = self.sb(es, "tneg", [128, NT])
                self.dma(tneg[:], io["cst_tneg"])
                KSD = self.sb(es, "KSD", [128, NT, 512], BF16)
                ft = self.sb(es, "ft", [33, 512])
                h1 = self.sb(es, "h1", [64, 512]); h2 = self.sb(es, "h2", [64, 512])
                rrf = self.sb(es, "rrf", [64, 512]); rri = self.sb(es, "rri", [64, 512], I32)
                kd = self.sb(es, "kd", [128, 512]); ka = self.sb(es, "ka", [128, 512]); dec = self.sb(es, "dec", [128, 512])
                held = self.hold_ps(3)
                pn = held[0:2]
                pny = held[2]
                for seq, (nseq, tile0, fkey) in enumerate(((NCTX, 0, "cst_featc"), (NLAT, 2, "cst_feat"))):
                    ntile = nseq // 128
                    for jb in range(0, nseq, 512):
                        bw = min(512, nseq - jb)
                        self.dma(ft[:, 0:bw], io[fkey][:, jb:jb + bw])
                        for (src, wgt, bias, dst, kk) in ((ft, w1, b1, h1, 33), (h1, w2, b2, h2, 64)):
                            p = self.ps()
                            self.mm(p[0:64, 0:bw], wgt[0:kk, :], src[0:kk, 0:bw])
                            self.ts(dst[:, 0:bw], p[0:64, 0:bw], bias[:, 0:1], None, ALU.add)
                            self.ts(rrf[:, 0:bw], dst[:, 0:bw], 1.0 / (2 * math.pi), None, ALU.mult)
                            self.cp(rri[:, 0:bw], rrf[:, 0:bw])
                            self.cp(rrf[:, 0:bw], rri[:, 0:bw])
                            self.stt(dst[:, 0:bw], rrf[:, 0:bw], -2 * math.pi, dst[:, 0:bw], ALU.mult, ALU.add)
                            self.act(dst[:, 0:bw], dst[:, 0:bw], AF.Sin)
                        for jt in range(bw // 128):
                            ti = tile0 + (jb // 128) + jt
                            p = self.ps()
                            self.mm(p[:], h2[:, jt * 128:(jt + 1) * 128], w3[:])
                            self.act(dec[:], d2[:], AF.Exp, scale=tneg[:, ti:ti + 1])
                            self.tt(kd[:], p[:], dec[:], ALU.mult)
                            if jb == 0 and jt == 0:
                                self.ts(kd[:, 256:512], kd[:, 256:512], self.misc[:, 1:2], None, ALU.mult)
                            self.act(ka[:], kd[:], AF.Abs)
                            first = (jb == 0 and jt == 0)
                            last = (jb + jt * 128 + 128 == nseq)
                            for c in range(2):
                                for hlf in range(2):
                                    self.mm(pn[c][:, seq:seq + 1], ka[:, hlf * 256 + c * 128:hlf * 256 + (c + 1) * 128], self.tri[:, 3, 0:1],
                                            start=(first and hlf == 0), stop=(last and hlf == 1))
                            self.tt(KSD[:, ti, 0:256], kd[:, 0:256], kd[:, 256:512], ALU.add)
                            self.tt(KSD[:, ti, 256:512], kd[:, 0:256], kd[:, 256:512], ALU.subtract)
                            self.tt(ka[:, 0:256], kd[:, 0:256], kd[:, 256:512], ALU.add)
                            self.mm(pny[0:1, seq * 256:(seq + 1) * 256], self.misc[:, 0:1], ka[:, 0:256], start=first, stop=last)
                    for c in range(2):
                        self.recip(rn[:, c, seq:seq + 1], pn[c][:, seq:seq + 1])
                    self.cp(KN[:, seq, :], pny[0:1, seq * 256:(seq + 1) * 256])
                self.release_ps(held)
                tbc = self.sb(es, "tbc8", [128, 2, 2, 256], BF16)
                self.dma(tbc[:].rearrange("p a r c -> p (a r) c"), io["cst_dft8c"].rearrange("a (r p) c -> p (a r) c", p=128))
                for ftile in range(2):
                    p = self.ps()
                    for a in range(2):
                        for jc in range(2):
                            self.mm(p[:, a * 256:(a + 1) * 256], tbc[:, a, jc, ftile * 128:(ftile + 1) * 128], KSD[:, jc, a * 256:(a + 1) * 256],
                                    start=(jc == 0), stop=(jc == 1))
                    self.cp(Kh[:, ftile, :], p[:], eng="act")
                tbs = [self.sb(es, f"tb8{j}", [128, 2, 32, 256], BF16) for j in range(2)]
                for fb in range(16):
                    tb = tbs[fb % 2]
                    for a in range(2):
                        self.dma(tb[:, a, :, :], io["cst_dft8"][fb, :, a, :, :])
                    for fsub in range(2):
                        ftile = fb * 2 + fsub
                        p = self.ps()
                        for a in range(2):
                            for jc in range(32):
                                self.mm(p[:, a * 256:(a + 1) * 256], tb[:, a, jc, fsub * 128:(fsub + 1) * 128], KSD[:, 2 + jc, a * 256:(a + 1) * 256],
                                        start=(jc == 0), stop=(jc == 31))
                        self.cp(Kh[:, 2 + ftile, :], p[:], eng=("act" if ftile % 2 else "dve"))
            self.P.barrier()
            with contextlib.ExitStack() as es:
                cw = self.sb(es, "cw", [128, 6, 3]); cb = self.sb(es, "cb", [128, 6])
                for k in range(3):
                    self.dma(cw[:, :, k], io["hy_conv_w"][l, k:k + 1, :].rearrange("o (c p) -> (o p) c", p=128), allow_slow_non_contiguous=True)
                self.dma(cb[:], io["hy_conv_b"][l:l + 1, :].rearrange("o (c p) -> (o p) c", p=128), allow_slow_non_contiguous=True)
                xin = self.sb(es, "xin", [128, NTOK + 4])
                self.memset(xin[:], 0.0)
                oo = [self.sb(es, f"cvo{j}", [128, NTOK]) for j in range(3)]
                for c in range(2):
                    self.conv_chunk((xin, oo[0]), self.FM[256 + c * 128:256 + (c + 1) * 128, :], cw[:, c, :], cb[:, c:c + 1],
                                    lambda o, c=c: self.dma(self.X1[c * 128:(c + 1) * 128, :], o[:], w=[("X1", c)]))
                    self.conv_chunk((xin, oo[1]), self.FM[256 + (2 + c) * 128:256 + (3 + c) * 128, :], cw[:, 2 + c, :], cb[:, 2 + c:3 + c], lambda o: None)
                    self.conv_chunk((xin, oo[2]), self.FM[256 + (4 + c) * 128:256 + (5 + c) * 128, :], cw[:, 4 + c, :], cb[:, 4 + c:5 + c], lambda o: None)
                    self.tt(oo[1][:], oo[1][:], oo[2][:], ALU.mult)
                    self.dma(self.VV[c * 128:(c + 1) * 128, :], oo[1][:], w=[("VV", c)])
            self.P.barrier()
            with contextlib.ExitStack() as es:
                Pb = self.sb(es, "Pb", [128, NT, 512], BF16)
                PN = self.sb(es, "PN", [1, 2, 256], BF16)
                with contextlib.ExitStack() as es2:
                    vvb = self.sb(es2, "vvb", [128, 2, NTOK], BF16)
                    for c in range(2):
                        self.dma(vvb[:, c, :], self.VV[c * 128:(c + 1) * 128, :], eng="pool")
                    VVt = self.sb(es2, "VVt", [128, NT, 256], BF16)
                    for i in range(NT):
                        pb = self.psb[i % 2]
                        for c in range(2):
                            self.tr(pb[:, c * 128:(c + 1) * 128], vvb[:, c, i * 128:(i + 1) * 128], self.identb[:])
                        self.cp(VVt[:, i, :], pb[:, 0:256], eng=("act" if i % 2 else "dve"))
                    altb = self.sb(es2, "altb", [128, 1], BF16)
                    self.cp(altb[:], self.misc[:, 0:1])
                    vh = self.sb(es2, "vh", [128, 512]); khf = self.sb(es2, "khf", [128, 512])
                    t1 = self.sb(es2, "t1", [128, 256]); t2 = self.sb(es2, "t2", [128, 256])
                    held2 = self.hold_ps(1)
                    pny = held2[0]
                    vn = self.sb(es2, "vn", [1, 256])

                    def spec_product(ti, p, is_f0):
                        self.cp(vh[:], p[:], eng="act")
                        self.cp(khf[:], Kh[:, ti, :], eng="pool")
                        self.tt(t1[:], vh[:, 0:256], khf[:, 0:256], ALU.mult)
                        self.tt(t2[:], vh[:, 256:512], khf[:, 256:512], ALU.mult)
                        self.tt(t1[:], t1[:], t2[:], ALU.subtract)
                        if is_f0:
                            self.ts(t1[:], t1[:], self.misc[:, 2:3], None, ALU.mult)
                        self.cp(Pb[:, ti, 0:256], t1[:], eng="act")
                        self.tt(t1[:], vh[:, 0:256], khf[:, 256:512], ALU.mult, eng="pool")
                        self.tt(t2[:], vh[:, 256:512], khf[:, 0:256], ALU.mult, eng="pool")
                        self.tt(t1[:], t1[:], t2[:], ALU.add, eng="pool")
                        if is_f0:
                            self.ts(t1[:], t1[:], self.misc[:, 2:3], None, ALU.mult)
                        self.cp(Pb[:, ti, 256:512], t1[:], eng="act")

                    tbc = self.sb(es2, "tbc8b", [128, 2, 2, 256], BF16)
                    self.dma(tbc[:].rearrange("p a r c -> p (a r) c"), io["cst_dft8c"].rearrange("a (r p) c -> p (a r) c", p=128))
                    for ftile in range(2):
                        p = self.ps()
                        for a in range(2):
                            for tc in range(2):
                                self.mm(p[:, a * 256:(a + 1) * 256], tbc[:, a, tc, ftile * 128:(ftile + 1) * 128], VVt[:, tc, :], start=(tc == 0), stop=(tc == 1))
                        spec_product(ftile, p, ftile == 0)
                    for tc in range(2):
                        self.mm(pny[0:1, 0:256], altb[:, 0:1], VVt[:, tc, :], start=(tc == 0), stop=(tc == 1))
                    self.tt(vn[:], pny[0:1, 0:256], KN[:, 0, :], ALU.mult)
                    self.ts(PN[:, 0, :], vn[:], 0.5, None, ALU.mult)
                    tbs = [self.sb(es2, f"tb8b{j}", [128, 2, 32, 256], BF16) for j in range(2)]
                    for fb in range(16):
                        tb = tbs[fb % 2]
                        for a in range(2):
                            self.dma(tb[:, a, :, :], io["cst_dft8"][fb, :, a, :, :])
                        for fsub in range(2):
                            ftile = fb * 2 + fsub
                            p = self.ps()
                            for a in range(2):
                                for tc in range(32):
                                    self.mm(p[:, a * 256:(a + 1) * 256], tb[:, a, tc, fsub * 128:(fsub + 1) * 128], VVt[:, 2 + tc, :], start=(tc == 0), stop=(tc == 31))
                            spec_product(2 + ftile, p, ftile == 0)
                    for tc in range(32):
                        self.mm(pny[0:1, 256:512], altb[:, 0:1], VVt[:, 2 + tc, :], start=(tc == 0), stop=(tc == 31))
                    self.tt(vn[:], pny[0:1, 256:512], KN[:, 1, :], ALU.mult)
                    self.ts(PN[:, 1, :], vn[:], 0.5, None, ALU.mult)
                    self.release_ps(held2)
                self.P.barrier()
                with contextlib.ExitStack() as es2:
                    hb_ = self.sb(es2, "hyb", [128, 2])
                    self.dma(hb_[:], io["hy_bias"][l:l + 1, :].rearrange("o (c p) -> (o p) c", p=128), allow_slow_non_contiguous=True)
                    altrow = self.sb(es2, "altrow", [1, 512], BF16)
                    self.dma(altrow[:], io["cst_altrow"][:, 0:512])
                    yv = [self.sb(es2, f"yv{j}", [128, 512]) for j in range(2)]
                    vvt = [self.sb(es2, f"vvt{j}", [128, 512]) for j in range(2)]
                    x1t = [self.sb(es2, f"x1t{j}", [128, 512]) for j in range(2)]
                    yo = [self.sb(es2, f"yho{j}", [128, 512], BF16) for j in range(2)]
                    scl = self.sb(es2, "scl", [128, 2, 2])
                    self.ts(scl[:, :, 0:1], rn[:, :, 0:1], 2.0 / (2 * NCTX), None, ALU.mult)
                    self.ts(scl[:, :, 1:2], rn[:, :, 1:2], 2.0 / (2 * NLAT), None, ALU.mult)
                    n = 0

                    def finish(c, seq, p, t0, tw):
                        nonlocal n
                        b = n % 2
                        n += 1
                        self.dma(vvt[b][:, 0:tw], self.VV[c * 128:(c + 1) * 128, t0:t0 + tw])
                        self.dma(x1t[b][:, 0:tw], self.X1[c * 128:(c + 1) * 128, t0:t0 + tw])
                        self.act(yv[b][:, 0:tw], p[:, 0:tw], AF.Copy, scale=scl[:, c, seq:seq + 1])
                        self.stt(yv[b][:, 0:tw], vvt[b][:, 0:tw], hb_[:, c:c + 1], yv[b][:, 0:tw], ALU.mult, ALU.add)
                        self.tt(yo[b][:, 0:tw], yv[b][:, 0:tw], x1t[b][:, 0:tw], ALU.mult)
                        self.dma(self.YS[256 + c * 128:256 + (c + 1) * 128, t0:t0 + tw], yo[b][:, 0:tw], w=[("YS", 2 + c, t0)])

                    tbc = self.sb(es2, "tbc8c", [128, 2, 2, 256], BF16)
                    self.dma(tbc[:].rearrange("p a r c -> p (a r) c"), io["cst_dft8c"].rearrange("a (r p) c -> p (a r) c", p=128))
                    for c in range(2):
                        p = self.ps()
                        cnt = 0
                        for fc in range(2):
                            for a in range(2):
                                self.mm(p[:, 0:256], Pb[:, fc, a * 256 + c * 128:a * 256 + (c + 1) * 128], tbc[:, a, fc, :], start=(cnt == 0), stop=False)
                                cnt += 1
                        self.mm(p[:, 0:256], PN[:, 0, c * 128:(c + 1) * 128], altrow[:, 0:256], start=False, stop=True)
                        finish(c, 0, p, 0, 256)
                    tbs = [self.sb(es2, f"tb8c{j}", [128, 2, 32, 256], BF16) for j in range(2)]
                    for tbk in range(16):
                        tb = tbs[tbk % 2]
                        for a in range(2):
                            self.dma(tb[:, a, :, :], io["cst_dft8"][tbk, :, a, :, :])
                        for c in range(2):
                            p = self.ps()
                            cnt = 0
                            for fc in range(32):
                                for a in range(2):
                                    self.mm(p[:, 0:256], Pb[:, 2 + fc, a * 256 + c * 128:a * 256 + (c + 1) * 128], tb[:, a, fc, :], start=(cnt == 0), stop=False)
                                    cnt += 1
                            self.mm(p[:, 0:256], PN[:, 1, c * 128:(c + 1) * 128], altrow[:, 0:256], start=False, stop=True)
                            finish(c, 1, p, NCTX + tbk * 256, 256)
        self.P.barrier()

    def phase_pool(self, l):
        io = self.io
        with contextlib.ExitStack() as es:
            pT = self.sb(es, "pinT", [128, 2, NTOK], BF16)
            for c in range(2):
                self.dma(pT[:, c, :], self.FM[2048 + c * 128:2048 + (c + 1) * 128, :], eng="pool")
            bdwf = self.sb(es, "bdwf", [128, 2, 128])
            self.memset(bdwf[:], 0.0)
            for g in range(4):
                pg, gi = g // 2, g % 2
                self.dma(bdwf[gi * 64:(gi + 1) * 64, pg, gi * 64:(gi + 1) * 64], io["pool_w"][l, g])
            bdw = self.sb(es, "bdw", [128, 2, 128], BF16)
            self.cp(bdw[:], bdwf[:])
            psc = self.sb(es, "psc", [128, 2])
            self.dma(psc[:], io["pool_scale"][l:l + 1, :].rearrange("o (c p) -> (o p) c", p=128), allow_slow_non_contiguous=True)
            pm = self.sb(es, "pm", [128, 4, 128], BF16)
            self.dma(pm[:], io["cst_pool"].rearrange("g s t -> s g t"), eng="pool")
            pmc = self.sb(es, "pmc", [128, 4, 2, 256], BF16)
            self.dma(pmc[:].rearrange("p g r t -> p (g r) t"), io["cst_poolc"].rearrange("g (r p) t -> p (g r) t", p=128), eng="pool")
            Az = [self.sb(es, f"Az{j}", [128, 4, 128], BF16) for j in range(2)]
            for j in range(2):
                self.memset(Az[j][:], 0.0)
            po = [self.sb(es, f"po{j}", [128, 2, 128], BF16) for j in range(2)]

            def make_az(i, az):
                p = self.ps()
                for pg in range(2):
                    self.mm(p[:, pg * 128:(pg + 1) * 128], pT[:, pg, i * 128:(i + 1) * 128], bdw[:, pg, :])
                pv = p[:, 0:256].rearrange("p (pg gi d) -> p pg gi d", pg=2, gi=2)
                azv = az[:].rearrange("p (pg gi) (h d) -> p pg gi h d", pg=2, h=2)
                self.cp(azv[:, :, 0, 0, :], pv[:, :, 0, :])
                self.cp(azv[:, :, 1, 1, :], pv[:, :, 1, :], eng="act")

            make_az(0, Az[0])
            make_az(1, Az[1])
            for tt_ in range(2):
                p = self.ps()
                for pg in range(2):
                    cnt = 0
                    for st in range(2):
                        for gi in range(2):
                            g = pg * 2 + gi
                            self.mm(p[:, pg * 128:(pg + 1) * 128], Az[st][:, g, :], pmc[:, g, st, tt_ * 128:(tt_ + 1) * 128], start=(cnt == 0), stop=(cnt == 3))
                            cnt += 1
                o = po[tt_ % 2]
                for pg in range(2):
                    self.act(o[:, pg, :], p[:, pg * 128:(pg + 1) * 128], AF.Copy, scale=psc[:, pg:pg + 1])
                    self.dma(self.YS[512 + pg * 128:512 + (pg + 1) * 128, tt_ * 128:(tt_ + 1) * 128], o[:, pg, :], w=[("YS", 4 + pg, tt_)])
            self.P.barrier()
            for i in range(2, NT):
                az = Az[i % 2]
                make_az(i, az)
                p = self.ps()
                for pg in range(2):
                    for gi in range(2):
                        g = pg * 2 + gi
                        self.mm(p[:, pg * 128:(pg + 1) * 128], az[:, g, :], pm[:, g, :], start=(gi == 0), stop=(gi == 1))
                o = po[i % 2]
                for pg in range(2):
                    self.act(o[:, pg, :], p[:, pg * 128:(pg + 1) * 128], AF.Copy, scale=psc[:, pg:pg + 1])
                    self.dma(self.YS[512 + pg * 128:512 + (pg + 1) * 128, i * 128:(i + 1) * 128], o[:, pg, :], w=[("YS", 4 + pg, i)])
        self.P.barrier()

    def phase_ssd(self, l):
        io = self.io
        with contextlib.ExitStack() as es:
            cw = self.sb(es, "scw", [128, 8, 3]); cb = self.sb(es, "scb", [128, 8])
            for k in range(3):
                self.dma(cw[:, :, k], io["ssm_conv_w"][l, k:k + 1, :].rearrange("o (c p) -> (o p) c", p=128), allow_slow_non_contiguous=True)
            self.dma(cb[:], io["ssm_conv_b"][l:l + 1, :].rearrange("o (c p) -> (o p) c", p=128), allow_slow_non_contiguous=True)
            xin = self.sb(es, "sxin", [128, NTOK + 4])
            self.memset(xin[:], 0.0)
            oo = [self.sb(es, f"scvo{j}", [128, NTOK]) for j in range(2)]
            ob = [self.sb(es, f"scvb{j}", [128, NTOK], BF16) for j in range(2)]
            for c in range(8):
                def fin(o, c=c):
                    self.act(ob[c % 2][:], o[:], AF.Silu)
                    self.dma(self.XBC[c * 128:(c + 1) * 128, :], ob[c % 2][:], w=[("XBC", c)])
                self.conv_chunk((xin, oo[c % 2]), self.FM[1024 + c * 128:1024 + (c + 1) * 128, :], cw[:, c, :], cb[:, c:c + 1], fin)
        self.P.barrier()
        with contextlib.ExitStack() as es:
            dtb = self.sb(es, "dtb", [128, 16]); abc = self.sb(es, "abc", [128, 16]); dsk = self.sb(es, "dsk", [128, 8])
            snw = self.sb(es, "snw", [128, 512])
            self.bc_row(dtb[:], io["ssm_dt_bias"][l:l + 1].rearrange("o a b -> o (a b)"))
            self.bc_row(abc[:], io["ssm_a_log"][l:l + 1].rearrange("o a b -> o (a b)"))
            self.bc_row(dsk[:], io["ssm_d"][l:l + 1, :])
            self.bc_row(snw[:], io["ssm_norm"][l:l + 1, :])
            self.act(abc[:], abc[:], AF.Exp)
            self.ts(abc[:], abc[:], -1.0, None, ALU.mult)
            H = self.sb(es, "Hst", [128, 512])
            Hb = self.sb(es, "Hstb", [128, 512], BF16)
            xb = [self.sb(es, f"xbct{j}", [128, 8, 128], BF16) for j in range(2)]
            tmt = [self.sb(es, f"tmt{j}", [128, 528]) for j in range(2)]
            xs_t = self.sb(es, "xs_t", [128, 512])
            Bt = self.sb(es, "Bt", [128, 256], BF16)
            dt = self.sb(es, "dt", [128, 8]); dta = self.sb(es, "dta", [128, 8]); tq = self.sb(es, "tq", [128, 8])
            dtax = self.sb(es, "dtax", [128, 8, 128])
            acs = self.sb(es, "acs", [128, 8]); tot = self.sb(es, "tot", [128, 8]); eacs = self.sb(es, "eacs", [128, 8])
            tend = self.sb(es, "tend", [128, 8]); dect = self.sb(es, "dect", [128, 8])
            scm = self.sb(es, "scm", [128, 2, 128])
            seg = self.sb(es, "seg", [128, 4, 128]); M = self.sb(es, "Mm", [128, 8, 128], BF16)
            xdt = self.sb(es, "xdt", [128, 512], BF16); xdtw = self.sb(es, "xdtw", [128, 512], BF16)
            yt = self.sb(es, "yt", [128, 512]); yf = self.sb(es, "yf", [128, 512]); zt = self.sb(es, "zt", [128, 512])
            ysq = self.sb(es, "ysq", [128, 512]); yb = self.sb(es, "yb16", [128, 512], BF16)
            ssq = self.sb(es, "ssq", [128, 1])
            yso = [self.sb(es, f"yso{j}", [128, 4, 128], BF16) for j in range(2)]

            def v3(ap, a, b):
                return ap.rearrange("p (a b) -> p a b", a=a)

            for d in range(2):
                self.memset(H[:], 0.0)
                self.memset(Hb[:], 0.0)
                order = list(range(NT)) if d == 0 else [1, 0] + list(range(NT - 1, 1, -1))
                trisel = self.tri[:, d, :]
                for n_, i in enumerate(order):
                    b = n_ % 2
                    self.dma(xb[b][:], self.XBC.rearrange("(c p) n -> p c n", p=128)[:, :, i * 128:(i + 1) * 128])
                    self.dma(tmt[b][:], self.TM[i * 128:(i + 1) * 128, :])
                    pb = self.psb[n_ % 2]
                    for c in range(6):
                        self.tr(pb[:, c * 128:(c + 1) * 128], xb[b][:, c, :], self.identb[:])
                    self.cp(xs_t[:], pb[:, 0:512], eng="act")
                    self.cp(Bt[:], pb[:, 512:768])
                    self.tt(dt[:], tmt[b][:, 512 + d * 8:520 + d * 8], dtb[:, d * 8:(d + 1) * 8], ALU.add)
                    self.act(tq[:], dt[:], AF.Abs)
                    self.act(tq[:], tq[:], AF.Exp, scale=-1.0)
                    self.act(tq[:], tq[:], AF.Ln, bias=1.0, scale=1.0)
                    self.stt(dt[:], dt[:], 0.0, tq[:], ALU.max, ALU.add)
                    self.tt(dta[:], dt[:], abc[:, d * 8:(d + 1) * 8], ALU.mult)
                    self.cp(dtax[:], dta[:].unsqueeze(2).to_broadcast([128, 8, 128]))
                    p = self.ps()
                    self.mm(p[:, 0:8], trisel, dta[:])
                    self.mm(p[:, 8:16], self.tri[:, 3, :], dta[:])
                    self.cp(acs[:], p[:, 0:8])
                    self.cp(tot[:], p[:, 8:16])
                    self.act(eacs[:], acs[:], AF.Exp)
                    self.tt(tend[:], tot[:], acs[:], ALU.subtract)
                    self.act(tend[:], tend[:], AF.Exp)
                    self.act(dect[:], tot[:], AF.Exp)
                    p = self.ps()
                    for g in range(2):
                        self.mm(p[:, g * 128:(g + 1) * 128], xb[b][:, 4 + g, :], xb[b][:, 6 + g, :])
                    self.tt(scm[:], v3(p[:, 0:256], 2, 128), trisel.unsqueeze(1).to_broadcast([128, 2, 128]), ALU.mult)
                    for g in range(2):
                        p = self.ps()
                        for r in range(4):
                            self.mm(p[:, r * 128:(r + 1) * 128], dtax[:, g * 4 + r, :], trisel)
                        self.tt(seg[:], v3(p[:], 4, 128), acs[:, g * 4:(g + 1) * 4].unsqueeze(2).to_broadcast([128, 4, 128]), ALU.subtract)
                        self.ts(seg[:], seg[:], 0.0, None, ALU.min)
                        self.act(seg[:], seg[:], AF.Exp)
                        self.tt(M[:, g * 4:(g + 1) * 4, :], seg[:], scm[:, g, :].unsqueeze(1).to_broadcast([128, 4, 128]), ALU.mult)
                    self.tt(v3(xdt[:], 8, 64), v3(xs_t[:], 8, 64), dt[:].unsqueeze(2).to_broadcast([128, 8, 64]), ALU.mult)
                    self.tt(tq[:], dt[:], tend[:], ALU.mult)
                    self.tt(v3(xdtw[:], 8, 64), v3(xs_t[:], 8, 64), tq[:].unsqueeze(2).to_broadcast([128, 8, 64]), ALU.mult)
                    pd = self.ps()
                    for hh in range(8):
                        self.mm(pd[:, hh * 64:(hh + 1) * 64], M[:, hh, :], xdt[:, hh * 64:(hh + 1) * 64])
                    po_ = self.ps()
                    for g in range(2):
                        self.mm(po_[:, g * 256:(g + 1) * 256], xb[b][:, 6 + g, :], Hb[:, g * 256:(g + 1) * 256])
                    self.tt(v3(yt[:], 8, 64), v3(po_[:], 8, 64), eacs[:].unsqueeze(2).to_broadcast([128, 8, 64]), ALU.mult)
                    self.tt(yt[:], yt[:], pd[:], ALU.add)
                    pst = self.ps()
                    for g in range(2):
                        self.mm(pst[:, g * 256:(g + 1) * 256], Bt[:, g * 128:(g + 1) * 128], xdtw[:, g * 256:(g + 1) * 256])
                    self.tt(v3(H[:], 8, 64), v3(H[:], 8, 64), dect[:].unsqueeze(2).to_broadcast([128, 8, 64]), ALU.mult)
                    self.tt(H[:], H[:], pst[:], ALU.add)
                    self.cp(Hb[:], H[:], eng="act")
                    if d == 0:
                        self.tt(v3(yf[:], 8, 64), v3(xs_t[:], 8, 64), dsk[:].unsqueeze(2).to_broadcast([128, 8, 64]), ALU.mult)
                        self.tt(yf[:], yf[:], yt[:], ALU.add)
                        self.dma(self.YF[i * 128:(i + 1) * 128, :], yf[:], w=[("YF", i)])
                    else:
                        self.dma(yf[:], self.YF[i * 128:(i + 1) * 128, :], r=[("YF", i)])
                        self.tt(yt[:], yt[:], yf[:], ALU.add)
                        self.act(zt[:], tmt[b][:, 0:512], AF.Silu)
                        self.tt(yt[:], yt[:], zt[:], ALU.mult)
                        self.act(ysq[:], yt[:], AF.Square, accum_out=ssq[:])
                        self.ts(ssq[:], ssq[:], 1.0 / 512, EPS, ALU.mult, ALU.add)
                        self.act(ssq[:], ssq[:], AF.Sqrt)
                        self.recip(ssq[:], ssq[:])
                        self.stt(yb[:], yt[:], ssq[:, 0:1], snw[:], ALU.mult, ALU.mult)
                        pb2 = self.psb[(n_ + 1) % 2]
                        for c in range(4):
                            self.tr(pb2[:, c * 128:(c + 1) * 128], yb[:, c * 128:(c + 1) * 128], self.identb[:])
                        o = yso[n_ % 2]
                        self.cp(o[:], pb2[:, 0:512].rearrange("p (c n) -> p c n", c=4), eng="act")
                        self.dma(self.YS.rearrange("(c p) n -> p c n", p=128)[:, 6:10, i * 128:(i + 1) * 128], o[:], w=[("YS", 6, i)])
                self.P.barrier()

    def phase_merge(self, l):
        io = self.io
        with contextlib.ExitStack() as es:
            hT = self.sb(es, "hT2", [128, 8, NTOK], BF16)
            self.phase_norm(l, 1, hT)
            wg = self.sb(es, "wg", [128, 4, 8, 512], BF16)
            wbr = self.sb(es, "wbr", [128, 10, 512], BF16)
            gst = [self.sb(es, f"gst{j}", [128, 4, 512]) for j in range(2)]
            ysb = [self.sb(es, f"ysb{j}", [128, 10, 512], BF16) for j in range(2)]
            gt = [self.sb(es, f"gt{j}", [128, 512]) for j in range(2)]
            acc = self.sb(es, "macc", [128, 512]); tmp = self.sb(es, "mtmp", [128, 512])
            mo = [self.sb(es, f"mo{j}", [128, 512], BF16) for j in range(2)]
            br_k = [(0, 2), (2, 4), (4, 6), (6, 10)]
            ng = 0
            nblk = 0
            for half in range(2):
                hc = slice(half * 512, (half + 1) * 512)
                for k in range(4):
                    for q in range(2):
                        st = gst[ng % 2]
                        ng += 1
                        self.dma(st[:], io["w_gate"][l, k, q * 512:(q + 1) * 512, hc].rearrange("(c p) n -> p c n", p=128))
                        self.cp(wg[:, k, q * 4:(q + 1) * 4, :], st[:], eng=("act" if ng % 2 else "pool"))
                for (q0, qn) in ((0, 4), (4, 4), (8, 2)):
                    st = gst[ng % 2]
                    ng += 1
                    self.dma(st[:, 0:qn, :], io["w_branch"][l, q0 * 128:(q0 + qn) * 128, hc].rearrange("(c p) n -> p c n", p=128))
                    self.cp(wbr[:, q0:q0 + qn, :], st[:, 0:qn, :], eng=("act" if ng % 2 else "pool"))
                for bi, (t0, tw) in enumerate(self.tokblocks()):
                    yb_ = ysb[nblk % 2]
                    nblk += 1
                    self.dma(yb_[:, :, 0:tw], self.YS.rearrange("(c p) n -> p c n", p=128)[:, :, t0:t0 + tw])
                    for o4 in range(4):
                        oc = half * 4 + o4
                        oc_s = slice(o4 * 128, (o4 + 1) * 128)
                        for k in range(4):
                            pg_ = self.ps()
                            for c in range(8):
                                self.mm(pg_[:, 0:tw], wg[:, k, c, oc_s], hT[:, c, t0:t0 + tw], start=(c == 0), stop=(c == 7))
                            g_ = gt[k % 2]
                            self.act(g_[:, 0:tw], pg_[:, 0:tw], AF.Sigmoid)
                            pb_ = self.ps()
                            c0, c1 = br_k[k]
                            for c in range(c0, c1):
                                self.mm(pb_[:, 0:tw], wbr[:, c, oc_s], yb_[:, c, 0:tw], start=(c == c0), stop=(c == c1 - 1))
                            if k == 0:
                                self.tt(acc[:, 0:tw], pb_[:, 0:tw], g_[:, 0:tw], ALU.mult)
                            else:
                                self.tt(tmp[:, 0:tw], pb_[:, 0:tw], g_[:, 0:tw], ALU.mult)
                                self.tt(acc[:, 0:tw], acc[:, 0:tw], tmp[:, 0:tw], ALU.add, eng="pool")
                        o = mo[o4 % 2]
                        self.cp(o[:, 0:tw], acc[:, 0:tw], eng="act")
                        self.dma(self.MG[oc * 128:(oc + 1) * 128, t0:t0 + tw], o[:, 0:tw], w=[("MG", oc, bi)])
        self.P.barrier()
        with contextlib.ExitStack() as es:
            wo = self.sb(es, "wo", [128, 8, D], BF16)
            wost = [self.sb(es, f"wost{j}", [128, D]) for j in range(2)]
            for k in range(8):
                self.dma(wost[k % 2][:], io["w_out"][l, k * 128:(k + 1) * 128, :])
                self.cp(wo[:, k, :], wost[k % 2][:], eng=("act" if k % 2 else "dve"))
            G = [self.sb(es, f"G{j}", [128, D]) for j in range(2)]
            for j in range(2):
                self.bc_row(G[j][:], self.MOD[l, 1 - j:2 - j, 2 * D:3 * D])
            mt = [self.sb(es, f"mt{j}", [128, 8, 128], BF16) for j in range(2)]
            xt = [self.sb(es, f"xo{j}", [128, D]) for j in range(2)]
            yy = self.sb(es, "yy", [128, D])
            for i in range(NT):
                if l == DEPTH - 1 and i < 2:
                    continue
                b = i % 2
                j = 0 if i < 2 else 1
                self.dma(mt[b][:], self.MG.rearrange("(c p) n -> p c n", p=128)[:, :, i * 128:(i + 1) * 128])
                self.dma(xt[b][:], self.XR[i * 128:(i + 1) * 128, :], r=[("XR", i)])
                for hf in range(2):
                    p = self.ps()
                    for k in range(8):
                        self.mm(p[:], mt[b][:, k, :], wo[:, k, hf * 512:(hf + 1) * 512], start=(k == 0), stop=(k == 7))
                    self.tt(yy[:, hf * 512:(hf + 1) * 512], p[:], G[j][:, hf * 512:(hf + 1) * 512], ALU.mult)
                self.tt(xt[b][:], xt[b][:], yy[:], ALU.add, eng="pool")
                self.dma(self.XR[i * 128:(i + 1) * 128, :], xt[b][:], w=[("XR", i)])
        self.P.barrier()

    def phase_moe(self, l):
        io = self.io
        t0 = 2 if l == DEPTH - 1 else 0
        with contextlib.ExitStack() as es_outer:
            SL = self.sb(es_outer, "SL", [128, NT, 4], I32)
            WS = self.sb(es_outer, "WS", [128, NT, 4])
            with contextlib.ExitStack() as es:
                rw = self.sb(es, "rw", [128, 8, NE])
                self.dma(rw[:], io["router_w"][l].rearrange("(k p) n -> p k n", p=128))
                rb = self.sb(es, "rb", [128, NE])
                self.bc_row(rb[:], io["router_b"][l:l + 1, :])
                ebase = self.sb(es, "ebase", [128, NE])
                self.dma(ebase[:], io["cst_ebase"])
                carry = self.sb(es, "carry", [128, NE])
                self.memset(carry[:], 0.0)
                hTf = self.sb(es, "hTf", [128, 8, 128])
                lg = self.sb(es, "lg", [128, NE]); mx = self.sb(es, "mx", [128, 8]); msk = self.sb(es, "msk", [128, NE])
                mskb = self.sb(es, "mskb", [128, NE], BF16)
                ex = self.sb(es, "ex", [128, NE]); den = self.sb(es, "den", [128, 1]); nmx = self.sb(es, "nmx", [128, 1])
                pos = self.sb(es, "pos", [128, NE]); okm = self.sb(es, "okm", [128, NE]); sv = self.sb(es, "sv", [128, NE])
                mx2 = self.sb(es, "mx2", [128, 8]); slf = self.sb(es, "slf", [128, 4]); junk = self.sb(es, "junk", [128, NE])

                sidx = self.p_idx

                def route(i, hf, hb):
                    for hh in range(2):
                        p = self.ps()
                        for k in range(4):
                            self.tr(p[:, k * 128:(k + 1) * 128], hf[:, (hh * 4 + k) * 128:(hh * 4 + k + 1) * 128], self.identf[:])
                        self.cp(hTf[:, hh * 4:(hh + 1) * 4, :], p[:].rearrange("p (k n) -> p k n", k=4), eng=("act" if hh else "dve"))
                    p = self.ps()
                    for k in range(8):
                        self.mm(p[:, 0:NE], hTf[:, k, :], rw[:, k, :], start=(k == 0), stop=(k == 7))
                    self.tt(lg[:], p[:, 0:NE], rb[:], ALU.add)
                    self.vmax(mx[:], lg[:])
                    self.ts(msk[:], lg[:], mx[:, 3:4], None, ALU.is_ge)
                    self.ts(nmx[:], mx[:, 0:1], -1.0, None, ALU.mult)
                    self.act(ex[:], lg[:], AF.Exp, bias=nmx[:, 0:1], scale=1.0)
                    self.tt(ex[:], ex[:], msk[:], ALU.mult)
                    self.rsum(den[:], ex[:])
                    self.recip(den[:], den[:])
                    self.ts(ex[:], ex[:], den[:, 0:1], None, ALU.mult)
                    self.cp(mskb[:], msk[:])
                    p = self.ps()
                    self.mm(p[:, 0:NE], self.trib[:, 2, :], mskb[:])
                    self.mm(p[:, NE:2 * NE], self.trib[:, 3, :], mskb[:])
                    self.tt(pos[:], p[:, 0:NE], carry[:], ALU.add)
                    self.tt(carry[:], carry[:], p[:, NE:2 * NE], ALU.add)
                    self.ts(okm[:], pos[:], float(CAP), None, ALU.is_lt)
                    self.tt(ex[:], ex[:], okm[:], ALU.mult)
                    self.ts(pos[:], pos[:], float(CAP), None, ALU.min)
                    self.tt(pos[:], pos[:], ebase[:], ALU.add)
                    self.ts(sv[:], pos[:], -1.0, BIG, ALU.mult, ALU.add)
                    self.tt(sv[:], sv[:], msk[:], ALU.mult)
                    self.vmax(mx2[:], sv[:])
                    self.ts(slf[:], mx2[:, 0:4], -1.0, BIG, ALU.mult, ALU.add)
                    self.cp(SL[:, i, :], slf[:])
                    for k in range(4):
                        self.stt(junk[:], sv[:], mx2[:, k:k + 1], ex[:], ALU.is_equal, ALU.mult, accum_out=WS[:, i, k:k + 1])
                    for k in range(4):
                        idxt = sidx[(i % 2) * 4 + k]
                        self.cp(idxt[:], SL[:, i, k:k + 1])
                        idx = idxt[:, :]
                        rr, ww = self._rw([], [hb[:], idx], (), ())
                        self.P.op("pool", lambda e, idx=idx, hb=hb: e.indirect_dma_start(
                            out=self.XG[:, :], out_offset=bass.IndirectOffsetOnAxis(ap=idx, axis=0),
                            in_=hb[:], in_offset=None),
                            rr, [("XGs", i, k)], dma=True)

                self.phase_norm(l, 2, None, t0=t0, extra=route)
            self.P.barrier()
            with contextlib.ExitStack() as es:
                wu = [self.sb(es, f"wu{j}", [128, 8, 2048], BF16) for j in range(2)]
                wd = [self.sb(es, f"wd{j}", [128, 8, D], BF16) for j in range(2)]
                bu = [self.sb(es, f"bu{j}", [128, 16]) for j in range(2)]
                bd_ = [self.sb(es, f"bdn{j}", [128, D]) for j in range(2)]
                xg = [self.sb(es, f"xg{j}", [128, D], BF16) for j in range(2)]
                xgT = [self.sb(es, f"xgT{j}", [128, 8, 512], BF16) for j in range(2)]
                actT = self.sb(es, "actT", [128, 8, 512], BF16)
                gq = [self.sb(es, f"gq{j}", [128, 512], BF16) for j in range(3)]
                sg = [self.sb(es, f"sg{j}", [128, 512], BF16) for j in range(3)]
                lq = [self.sb(es, f"lq{j}", [128, 512], BF16) for j in range(3)]
                yo = [self.sb(es, f"yo{j}", [128, D]) for j in range(2)]
                blocks = []
                c0 = 0
                while c0 < CAP:
                    w_ = min(512, CAP - c0)
                    blocks.append((c0, w_))
                    c0 += w_
                nb = 0
                ny = 0
                stg = [self.sb(es, f"stg{j}", [128, 2048]) for j in range(3)]
                nst = [0]

                def loader(e):
                    eb_ = e % 2
                    self.dma(bu[eb_][:], io["exp_b_up"][l, e:e + 1, :].rearrange("o (c p) -> (o p) c", p=128), allow_slow_non_contiguous=True)
                    self.bc_row(bd_[eb_][:], io["exp_b_down"][l, e:e + 1, :])
                    yield
                    for k in range(8):
                        st = stg[nst[0] % 3]
                        nst[0] += 1
                        self.dma(st[:], io["exp_w_up"][l, e, k * 128:(k + 1) * 128, :])
                        self.cp(wu[eb_][:, k, :], st[:], eng="act")
                        yield
                    for k in range(8):
                        st = stg[nst[0] % 3]
                        nst[0] += 1
                        self.dma(st[:, 0:D], io["exp_w_down"][l, e, k * 128:(k + 1) * 128, :])
                        self.cp(wd[eb_][:, k, :], st[:, 0:D], eng="act")
                        yield

                for _ in loader(0):
                    pass
                for e in range(NE):
                    eb = e % 2
                    nxt = loader(e + 1) if e + 1 < NE else iter(())
                    for (c0, w_) in blocks:
                        xT = xgT[nb % 2]
                        nb += 1
                        for s in range(w_ // 128):
                            r0 = e * ESTR + c0 + s * 128
                            xt_ = xg[s % 2]
                            self.dma(xt_[:], self.XG[r0:r0 + 128, :], r=[("XGl", r0)])
                            pb = self.psb[s % 2]
                            for k in range(8):
                                self.tr(pb[:, k * 128:(k + 1) * 128], xt_[:, k * 128:(k + 1) * 128], self.identb[:])
                            self.cp(xT[:, :, s * 128:(s + 1) * 128], pb[:].rearrange("p (k n) -> p k n", k=8), eng=("act" if s % 2 else "dve"))
                        def stage_a(fc):
                            pg_ = self.ps()
                            for k in range(8):
                                self.mm(pg_[:, 0:w_], wu[eb][:, k, fc * 128:(fc + 1) * 128], xT[:, k, 0:w_], start=(k == 0), stop=(k == 7))
                            pl_ = self.ps()
                            for k in range(8):
                                self.mm(pl_[:, 0:w_], wu[eb][:, k, 1024 + fc * 128:1024 + (fc + 1) * 128], xT[:, k, 0:w_], start=(k == 0), stop=(k == 7))
                            g_ = gq[fc % 3]; s_ = sg[fc % 3]; l_ = lq[fc % 3]
                            self.ts(g_[:, 0:w_], pg_[:, 0:w_], bu[eb][:, fc:fc + 1], 7.0, ALU.add, ALU.min)
                            self.act(s_[:, 0:w_], g_[:, 0:w_], AF.Sigmoid, scale=1.702)
                            self.ts(l_[:, 0:w_], pl_[:, 0:w_], bu[eb][:, 8 + fc:9 + fc], 7.0, ALU.add, ALU.min)
                            next(nxt, None)

                        def stage_b(fc):
                            g_ = gq[fc % 3]; s_ = sg[fc % 3]; l_ = lq[fc % 3]
                            self.ts(l_[:, 0:w_], l_[:, 0:w_], -7.0, 1.0, ALU.max, ALU.add)
                            self.tt(g_[:, 0:w_], g_[:, 0:w_], s_[:, 0:w_], ALU.mult)
                            self.tt(actT[:, fc, 0:w_], g_[:, 0:w_], l_[:, 0:w_], ALU.mult)

                        stage_a(0)
                        for fc in range(1, 8):
                            stage_a(fc)
                            stage_b(fc - 1)
                        stage_b(7)
                        for s in range(w_ // 128):
                            r0 = e * ESTR + c0 + s * 128
                            o = yo[ny % 2]
                            ny += 1
                            for hf in range(2):
                                p = self.ps()
                                for fc in range(8):
                                    self.mm(p[:], actT[:, fc, s * 128:(s + 1) * 128], wd[eb][:, fc, hf * 512:(hf + 1) * 512], start=(fc == 0), stop=(fc == 7))
                                self.tt(o[:, hf * 512:(hf + 1) * 512], p[:], bd_[eb][:, hf * 512:(hf + 1) * 512], ALU.add)
                            self.dma(self.YG[r0:r0 + 128, :], o[:], w=[("YGs", r0)])
                    for _ in nxt:
                        pass
            self.P.barrier()
            with contextlib.ExitStack() as es:
                G2 = [self.sb(es, f"G2{j}", [128, D]) for j in range(2)]
                for j in range(2):
                    self.bc_row(G2[j][:], self.MOD[l, 1 - j:2 - j, 5 * D:6 * D])
                gk = self.p_gk
                xt = [self.sb(es, f"xc{j}", [128, D]) for j in range(2)]
                acc = self.sb(es, "cacc", [128, D])
                cidx = self.p_idx
                for i in range(t0, NT):
                    b = i % 2
                    j = 0 if i < 2 else 1
                    self.dma(xt[b][:], self.XR[i * 128:(i + 1) * 128, :], r=[("XR", i)])
                    for k in range(4):
                        idxt = cidx[(i % 2) * 4 + k]
                        self.cp(idxt[:], SL[:, i, k:k + 1])
                        idx = idxt[:, :]
                        gk_ = gk[k]
                        rr, ww = self._rw([gk_[:]], [idx], (), ())
                        self.P.op("pool", lambda e, idx=idx, gk_=gk_: e.indirect_dma_start(
                            out=gk_[:], out_offset=None, in_=self.YG[:, :],
                            in_offset=bass.IndirectOffsetOnAxis(ap=idx, axis=0)), rr, ww, dma=True)
                    self.ts(acc[:], gk[0][:], WS[:, i, 0:1], None, ALU.mult)
                    for k in range(1, 4):
                        self.stt(acc[:], gk[k][:], WS[:, i, k:k + 1], acc[:], ALU.mult, ALU.add)
                    self.tt(acc[:], acc[:], G2[j][:], ALU.mult)
                    self.tt(xt[b][:], xt[b][:], acc[:], ALU.add)
                    self.dma(self.XR[i * 128:(i + 1) * 128, :], xt[b][:], w=[("XR", i)])
        self.P.barrier()

    def phase_final(self):
        io = self.io
        with contextlib.ExitStack() as es:
            g = self.sb(es, "gfin", [128, D])
            self.bc_row(g[:], io["norm_final"])
            xt = [self.sb(es, f"xf{j}", [128, D]) for j in range(2)]
            sq = self.sb(es, "sqf", [128, D])
            ss = [self.sb(es, f"ssf{j}", [128, 1]) for j in range(2)]
            o = [self.sb(es, f"of{j}", [128, D]) for j in range(2)]
            for i in range(2, NT):
                b = i % 2
                self.dma(xt[b][:], self.XR[i * 128:(i + 1) * 128, :], r=[("XR", i)])
                self.act(sq[:], xt[b][:], AF.Square, accum_out=ss[b][:])
                self.ts(ss[b][:], ss[b][:], 1.0 / D, EPS, ALU.mult, ALU.add)
                self.act(ss[b][:], ss[b][:], AF.Sqrt)
                self.recip(ss[b][:], ss[b][:])
                self.stt(o[b][:], xt[b][:], ss[b][:, 0:1], g[:], ALU.mult, ALU.mult)
                self.dma(io["out"][(i - 2) * 128:(i - 1) * 128, :], o[b][:], w=[("out", i)])


W_NAMES = ['w_mod', 'b_mod', 'norm_mix', 'norm_ffn', 'w_in', 'hy_conv_w', 'hy_conv_b', 'hy_ffn_w1', 'hy_ffn_b1',
           'hy_ffn_w2', 'hy_ffn_b2', 'hy_ffn_w3', 'hy_bias', 'pool_w', 'pool_scale', 'ssm_conv_w', 'ssm_conv_b',
           'ssm_dt_bias', 'ssm_a_log', 'ssm_d', 'ssm_norm', 'w_branch', 'w_gate', 'w_out', 'router_w', 'router_b',
           'exp_w_up', 'exp_b_up', 'exp_w_down', 'exp_b_down']

_CONST_CACHE = {}


def make_constants():
    if _CONST_CACHE:
        return _CONST_CACHE
    bf = ml_dtypes.bfloat16
    c = {}
    c["cst_ident"] = np.eye(128, dtype=np.float32)
    t = np.arange(128)
    tri = np.zeros((4, 128, 128), np.float32)
    tri[0] = (t[:, None] <= t[None, :])
    tri[1] = (t[:, None] >= t[None, :])
    tri[2] = (t[:, None] < t[None, :])
    tri[3] = 1.0
    c["cst_tri"] = tri
    misc = np.ones((128, 8), np.float32)
    misc[:, 0] = (-1.0) ** t
    misc[0, 1] = 0.0
    misc[0, 2] = 0.5
    c["cst_misc"] = misc
    c["cst_altrow"] = ((-1.0) ** np.arange(4096)).astype(np.float32).reshape(1, 4096).astype(bf)
    m = np.arange(64)
    a64 = 2 * np.pi * np.outer(m, m) / 64.0
    bd = np.zeros((2, 128, 128), np.float32)
    for g in range(2):
        bd[0, g * 64:(g + 1) * 64, g * 64:(g + 1) * 64] = np.cos(a64)
        bd[1, g * 64:(g + 1) * 64, g * 64:(g + 1) * 64] = np.sin(a64)
    c["cst_bd64"] = bd

    def dft(n, period):
        k = np.arange(n, dtype=np.int64)
        ph = (np.outer(k, k) % period).astype(np.float64) * (2 * np.pi / period)
        out = np.empty((2, n, n), bf)
        out[0] = np.cos(ph).astype(np.float32).astype(bf)
        out[1] = (-np.sin(ph)).astype(np.float32).astype(bf)
        return out

    def tiled(t):
        return np.ascontiguousarray(t.reshape(2, 32, 128, 16, 256).transpose(3, 2, 0, 1, 4))

    c["cst_dft4"] = tiled(dft(NLAT, NLAT))
    c["cst_dft4c"] = dft(NCTX, NCTX)
    c["cst_dft8"] = tiled(dft(NLAT, 2 * NLAT))
    c["cst_dft8c"] = dft(NCTX, 2 * NCTX)

    def poolmat(row_len, nrows):
        n = row_len * nrows
        out = np.zeros((4, n, n), np.float32)
        pos = np.arange(row_len)
        for gi, win in enumerate((2, 4, 8, 16)):
            lo = np.clip(pos - win // 2, 0, row_len)
            hi = np.clip(pos + win // 2, 0, row_len)
            blk = np.zeros((row_len, row_len), np.float32)
            for tt in range(row_len):
                blk[tt, lo[tt]:hi[tt]] = 1.0 / float(hi[tt] - lo[tt])
            blk -= np.eye(row_len, dtype=np.float32)
            for r in range(nrows):
                out[gi, r * row_len:(r + 1) * row_len, r * row_len:(r + 1) * row_len] = blk.T
        return out

    c["cst_pool"] = poolmat(64, 2)
    c["cst_poolc"] = poolmat(256, 1)

    def feats(n):
        pos = np.arange(n, dtype=np.float32)
        tt = pos / np.float32(n - 1)
        ang = (np.float32(2.0 * math.pi) * pos / np.float32(n)).astype(np.float32)
        freqs = np.linspace(1e-4, 15, 16, dtype=np.float32)
        f = np.concatenate([tt[:, None], np.cos(ang[:, None] * freqs), -np.sin(ang[:, None] * freqs)], axis=-1).astype(np.float32)
        return np.ascontiguousarray(f.T), tt

    fl, tl = feats(NLAT)
    fc, tc = feats(NCTX)
    c["cst_feat"] = fl
    c["cst_featc"] = fc
    tneg = np.zeros((128, NT), np.float32)
    tneg[:, 0:2] = -tc.reshape(2, 128).T
    tneg[:, 2:] = -tl.reshape(32, 128).T
    c["cst_tneg"] = tneg
    deltas = np.linspace(HY_MIN, HY_MAX, 256, dtype=np.float32)
    c["cst_delta2"] = np.ascontiguousarray(np.broadcast_to(np.concatenate([deltas, deltas])[None, :], (128, 512))).astype(np.float32)
    c["cst_ebase"] = np.ascontiguousarray(np.broadcast_to((np.arange(NE) * ESTR).astype(np.float32)[None, :], (128, NE)))
    _CONST_CACHE.update(c)
    return c


def build_program(cfg):
    nc = bass.Bass("TRN2", target_bir_lowering=False)
    io = {}

    def inp(name, shape, dt=F32):
        io[name] = nc.dram_tensor(name, list(shape), dt, kind="ExternalInput").ap()

    inp("x", [NLAT, D]); inp("ctx", [NCTX, D]); inp("c", [1, D]); inp("c_ctx", [1, D])
    shapes = cfg["shapes"]
    for n in W_NAMES:
        inp(n, shapes[n])
    inp("norm_final", [1, D])
    consts = make_constants()
    for n, a in consts.items():
        inp(n, a.shape, BF16 if a.dtype == ml_dtypes.bfloat16 else F32)
    io["out"] = nc.dram_tensor("out", [NLAT, D], F32, kind="ExternalOutput").ap()
    k = Kern(nc, io, cfg)
    k.build()
    return nc, k


def kernel(**inputs):
    cfg = {"shapes": {n: list(inputs[n].shape) for n in W_NAMES}}
    env = os.environ
    if env.get("MK_LAYERS"):
        cfg["layers"] = int(env["MK_LAYERS"])
    if env.get("MK_PHASES"):
        cfg["phases"] = env["MK_PHASES"]
    if env.get("MK_DUMP"):
        cfg["dump"] = tuple(env["MK_DUMP"].split(","))
    nc, k = build_program(cfg)
    consts = make_constants()
    shared = {n: np.ascontiguousarray(inputs[n], dtype=np.float32) for n in W_NAMES}
    shared["norm_final"] = np.ascontiguousarray(inputs["norm_final"], dtype=np.float32).reshape(1, D)
    shared["c_ctx"] = np.ascontiguousarray(inputs["c_ctx"], dtype=np.float32).reshape(1, D)
    shared.update(consts)
    ncore = int(env.get("MK_CORES", "8"))
    in_maps = []
    for b in range(ncore):
        m = dict(shared)
        m["x"] = np.ascontiguousarray(inputs["x"][b], dtype=np.float32)
        m["ctx"] = np.ascontiguousarray(inputs["ctx"][b], dtype=np.float32)
        m["c"] = np.ascontiguousarray(inputs["c"][b], dtype=np.float32).reshape(1, D)
        in_maps.append(m)
    res = run_bass_kernel_spmd(nc, in_maps, core_ids=list(range(ncore)))
    out = np.zeros((8, NLAT, D), np.float32)
    for b in range(ncore):
        out[b] = res.results[b]["out"]
    if cfg.get("dump"):
        kernel.last_results = res.results
    return out
```

```python
import os
import math
import contextlib
import numpy as np
import ml_dtypes
import concourse.bass as bass
import concourse.mybir as mybir
from concourse.bass_utils import run_bass_kernel_spmd

F32 = mybir.dt.float32
BF16 = mybir.dt.bfloat16
I32 = mybir.dt.int32
AF = mybir.ActivationFunctionType
ALU = mybir.AluOpType
AX = mybir.AxisListType

SEM_ROT = 30000
DMA_SLOTS = 6

D = 1024
NLAT = 4096
NCTX = 256
NTOK = NLAT + NCTX
NT = NTOK // 128
DEPTH = 4
INW = 2832
NE = 32
CAP = 1280
ESTR = CAP + 8
BIG = 65536.0
EPS = 1e-6
HY_MIN = -math.log(1e-2) / 1.5
HY_MAX = -math.log(1e-2) / 0.3


class Prog:
    ENG = ("pe", "act", "dve", "pool", "sp")

    def __init__(self, nc):
        self.nc = nc
        self.q = {e: [] for e in self.ENG}
        self.cnt = {e: 0 for e in self.ENG}
        self.gen = {e: 0 for e in self.ENG}
        self.clock = {e: {} for e in self.ENG}
        self.res = {}
        self.semnames = []
        self.dma_slots = {e: [[self._newsem(f"dq_{e}_{i}"), 0] for i in range(DMA_SLOTS)]
                          for e in ("sp", "act", "pool")}
        self.dma_i = {e: 0 for e in ("sp", "act", "pool")}
        self.cursem = {e: self._newsem(f"s_{e}_0") for e in self.ENG}
        self.nops = 0
        self.pe_cond = None
        self.pe_snap = None

    def set_pe_cond(self, cond):
        if self.pe_cond is not None:
            self.clock["pe"] = dict(self.pe_snap)
        self.pe_cond = cond
        self.pe_snap = dict(self.clock["pe"]) if cond is not None else None

    def _newsem(self, name):
        self.semnames.append(name)
        return name

    def op(self, eng, fn, reads=(), writes=(), dma=False):
        deps = []
        for k in reads:
            r = self.res.get(k)
            if r is not None and r[0] is not None:
                deps.append(r[0])
        for k in writes:
            r = self.res.get(k)
            if r is not None:
                if r[0] is not None:
                    deps.append(r[0])
                deps.extend(r[1])
        clk = self.clock[eng]
        if dma:
            slots = self.dma_slots[eng]
            slot = slots[self.dma_i[eng] % len(slots)]
            self.dma_i[eng] += 1
            if slot[1] > 0:
                deps.append((slot[0], slot[1], None))
            if slot[1] + 16 > SEM_ROT:
                slot[0] = self._newsem(slot[0] + "r")
                slot[1] = 0
            slot[1] += 16
            ev_sem, ev_val, inc = slot[0], slot[1], 16
        else:
            if self.cnt[eng] + 1 > SEM_ROT:
                self.gen[eng] += 1
                self.cursem[eng] = self._newsem(f"s_{eng}_{self.gen[eng]}")
                self.cnt[eng] = 0
            self.cnt[eng] += 1
            ev_sem, ev_val, inc = self.cursem[eng], self.cnt[eng], 1
        waits = {}
        for (s, v, c) in deps:
            if eng == "pe" and s.startswith("s_pe_"):
                continue
            if clk.get(s, 0) >= v:
                continue
            if waits.get(s, 0) < v:
                waits[s] = v
        for (s, v, c) in deps:
            if s in waits:
                if c:
                    for ks, kv in c.items():
                        if clk.get(ks, 0) < kv:
                            clk[ks] = kv
                if clk.get(s, 0) < v:
                    clk[s] = v
        cond = self.pe_cond if eng == "pe" else None
        evclk = dict(self.pe_snap) if cond is not None else dict(clk)
        evclk[ev_sem] = ev_val
        ev = (ev_sem, ev_val, evclk)
        self.q[eng].append((list(waits.items()), fn, ev_sem, inc, cond))
        self.nops += 1
        for k in writes:
            self.res[k] = [ev, []]
        for k in reads:
            if k in writes:
                continue
            r = self.res.get(k)
            if r is None:
                self.res[k] = [None, [ev]]
            else:
                r[1].append(ev)
                if len(r[1]) > 24:
                    best = {}
                    for e_ in r[1]:
                        if e_[0] not in best or best[e_[0]][1] < e_[1]:
                            best[e_[0]] = e_
                    r[1] = list(best.values())
        return ev

    def barrier(self, engines=None):
        latest = {}
        for r in self.res.values():
            evs = list(r[1])
            if r[0] is not None:
                evs.append(r[0])
            for (s, v, c) in evs:
                if latest.get(s, 0) < v:
                    latest[s] = v
        for e in self.ENG:
            for s, v in self.dma_slots.get(e, []):
                if v > 0 and latest.get(s, 0) < v:
                    latest[s] = v
            if self.cnt[e] > 0 and latest.get(self.cursem[e], 0) < self.cnt[e]:
                latest[self.cursem[e]] = self.cnt[e]
        for e in (engines or self.ENG):
            clk = self.clock[e]
            waits = [(s, v) for s, v in latest.items() if clk.get(s, 0) < v]
            for s, v in waits:
                clk[s] = v
            if waits:
                self.q[e].append((waits, None, None, 0, None))
        self.res = {}

    def emit(self):
        nc = self.nc
        sems = {n: nc.alloc_semaphore(name=n) for n in self.semnames}
        engmap = {"pe": "tensor", "act": "scalar", "dve": "vector", "pool": "gpsimd", "sp": "sync"}
        with nc.Block() as block:
            for e in self.ENG:
                ops = self.q[e]

                def emit_run(eng, run):
                    for waits, fn, ev_sem, inc, cond in run:
                        for s, v in waits:
                            eng.wait_ge(sems[s], v)
                        if fn is not None:
                            ins = fn(eng)
                            ins.then_inc(sems[ev_sem], inc)

                def body(eng, ops=ops, e=e):
                    if e != "pe":
                        emit_run(eng, ops)
                        return
                    running = {}
                    last = None
                    with eng.register("cnd") as reg:
                        i = 0
                        n = len(ops)
                        while i < n:
                            cond = ops[i][4]
                            j = i
                            while j < n and ops[j][4] is cond:
                                j += 1
                            run = ops[i:j]
                            incs = {}
                            for waits, fn, ev_sem, inc, c_ in run:
                                if fn is not None:
                                    incs[ev_sem] = incs.get(ev_sem, 0) + inc
                            if cond is None:
                                emit_run(eng, run)
                            else:
                                ap, thr = cond
                                if last is not None:
                                    eng.wait_ge(sems[last], running[last])
                                eng.reg_load(reg, ap)
                                with eng.If_lt(reg, thr):
                                    for s_, tot in incs.items():
                                        eng.sem_inc(sems[s_], tot)
                                with eng.Else():
                                    emit_run(eng, run)
                            for s_, tot in incs.items():
                                running[s_] = running.get(s_, 0) + tot
                                last = s_
                            i = j

                getattr(block, engmap[e])(body)


def _nm(ap):
    return ap.name


class Kern:
    def __init__(self, nc, io, cfg):
        self.nc = nc
        self.io = io
        self.cfg = cfg
        self.P = Prog(nc)
        self.uid = 0
        self.psi = 0

    def sb(self, es, name, shape, dt=F32):
        self.uid += 1
        return es.enter_context(self.nc.sbuf_tensor(f"{name}_{self.uid}", list(shape), dt))

    def dram(self, name, shape, dt=F32):
        kind = "ExternalOutput" if name in self.cfg.get("dump", ()) else "Internal"
        return self.nc.dram_tensor(name, list(shape), dt, kind=kind).ap()

    def ps(self):
        t = self.psf[self.psi % len(self.psf)]
        self.psi += 1
        return t

    def hold_ps(self, n):
        held = [self.psf.pop() for _ in range(n)]
        return held

    def release_ps(self, held):
        self.psf.extend(held)

    def _rw(self, outs, ins, r, w):
        reads = list(r)
        writes = list(w)
        for a in ins:
            if a is None or isinstance(a, (int, float)):
                continue
            n = _nm(a)
            if n.startswith("ps"):
                writes.append(n)
            else:
                reads.append(n)
        for a in outs:
            writes.append(_nm(a))
        return reads, writes

    def dma(self, out, in_, eng="sp", r=None, w=None, **kw):
        reads = list(r) if r is not None else [_nm(in_)]
        writes = list(w) if w is not None else [_nm(out)]
        self.P.op(eng, lambda e: e.dma_start(out=out, in_=in_, **kw), reads, writes, dma=True)

    def mm(self, out, lhsT, rhs, start=True, stop=True):
        reads, writes = self._rw([out], [lhsT, rhs], (), ())
        self.P.op("pe", lambda e: e.matmul(out, lhsT=lhsT, rhs=rhs, start=start, stop=stop), reads, writes)

    def tr(self, out, in_, ident):
        reads, writes = self._rw([out], [in_, ident], (), ())
        self.P.op("pe", lambda e: e.transpose(out=out, in_=in_, identity=ident), reads, writes)

    def act(self, out, in_, func, bias=None, scale=None, accum_out=None, eng="act"):
        kw = {}
        if bias is not None:
            kw["bias"] = bias
        if scale is not None:
            kw["scale"] = scale
        outs = [out]
        if accum_out is not None:
            kw["accum_out"] = accum_out
            outs.append(accum_out)
        reads, writes = self._rw(outs, [in_, bias, scale], (), ())
        self.P.op(eng, lambda e: e.activation(out=out, in_=in_, func=func, **kw), reads, writes)

    def cp(self, out, in_, eng="dve"):
        reads, writes = self._rw([out], [in_], (), ())
        if eng == "act":
            self.P.op("act", lambda e: e.copy(out=out, in_=in_), reads, writes)
        else:
            self.P.op(eng, lambda e: e.tensor_copy(out=out, in_=in_), reads, writes)

    def tt(self, out, in0, in1, op, eng="dve"):
        reads, writes = self._rw([out], [in0, in1], (), ())
        self.P.op(eng, lambda e: e.tensor_tensor(out=out, in0=in0, in1=in1, op=op), reads, writes)

    def ts(self, out, in0, s1, s2, op0, op1=None, eng="dve"):
        reads, writes = self._rw([out], [in0, s1, s2], (), ())
        if op1 is None:
            self.P.op(eng, lambda e: e.tensor_scalar(out=out, in0=in0, scalar1=s1, scalar2=None, op0=op0), reads, writes)
        else:
            self.P.op(eng, lambda e: e.tensor_scalar(out=out, in0=in0, scalar1=s1, scalar2=s2, op0=op0, op1=op1), reads, writes)

    def stt(self, out, in0, scalar, in1, op0, op1, accum_out=None, eng="dve"):
        outs = [out]
        kw = {}
        if accum_out is not None:
            kw["accum_out"] = accum_out
            outs.append(accum_out)
        reads, writes = self._rw(outs, [in0, scalar, in1], (), ())
        self.P.op(eng, lambda e: e.scalar_tensor_tensor(out=out, in0=in0, scalar=scalar, in1=in1, op0=op0, op1=op1, **kw), reads, writes)

    def memset(self, ap, val, eng="dve"):
        reads, writes = self._rw([ap], [], (), ())
        self.P.op(eng, lambda e: e.memset(ap, val), reads, writes)

    def vmax(self, out, in_):
        reads, writes = self._rw([out], [in_], (), ())
        self.P.op("dve", lambda e: e.max(out=out, in_=in_), reads, writes)

    def recip(self, out, in_):
        reads, writes = self._rw([out], [in_], (), ())
        self.P.op("dve", lambda e: e.reciprocal(out=out, in_=in_), reads, writes)

    def rsum(self, out, in_):
        reads, writes = self._rw([out], [in_], (), ())
        self.P.op("dve", lambda e: e.reduce_sum(out=out, in_=in_, axis=AX.X), reads, writes)

    def build(self):
        nc, io, cfg = self.nc, self.io, self.cfg
        layers = cfg.get("layers", DEPTH)
        with contextlib.ExitStack() as es:
            self.psf = [es.enter_context(nc.psum_tensor(f"psF{i}", [128, 512], F32)) for i in range(6)]
            self.psb = [es.enter_context(nc.psum_tensor(f"psB{i}", [128, 1024], BF16)) for i in range(2)]
            self.identf = self.sb(es, "identf", [128, 128])
            self.identb = self.sb(es, "identb", [128, 128], BF16)
            self.tri = self.sb(es, "tri", [128, 4, 128])
            self.trib = self.sb(es, "trib", [128, 4, 128], BF16)
            self.misc = self.sb(es, "misc", [128, 8])
            self.dma(self.identf[:], io["cst_ident"])
            self.dma(self.tri[:], io["cst_tri"].rearrange("a p n -> p a n"))
            self.dma(self.misc[:], io["cst_misc"])
            self.p_hb = [self.sb(es, f"p_hb{j}", [128, D], BF16) for j in range(2)]
            self.p_idx = [self.sb(es, f"p_idx{j}", [128, 1], I32) for j in range(8)]
            self.p_gk = [self.sb(es, f"p_gk{j}", [128, D]) for j in range(4)]
            self.cp(self.identb[:], self.identf[:])
            self.cp(self.trib[:], self.tri[:])
            self.XR = self.dram("XR", [NTOK, D])
            self.MOD = self.dram("MOD", [DEPTH, 2, 6 * D])
            self.FM = self.dram("FM", [2048 + 256, NTOK])
            self.TM = self.dram("TM", [NTOK, 528])
            self.YS = self.dram("YS", [1280, NTOK], BF16)
            self.MG = self.dram("MG", [D, NTOK], BF16)
            self.X1 = self.dram("X1", [256, NTOK])
            self.VV = self.dram("VV", [256, NTOK])
            self.XBC = self.dram("XBC", [1024, NTOK], BF16)
            self.YF = self.dram("YF", [NTOK, 512])
            self.XG = self.dram("XG", [NE * ESTR, D], BF16)
            self.YG = self.dram("YG", [NE * ESTR, D])
            self.dma(self.XR[0:NCTX, :], io["ctx"], w=[("XR", 0), ("XR", 1)])
            for q in range(4):
                self.dma(self.XR[NCTX + q * 1024:NCTX + (q + 1) * 1024, :], io["x"][q * 1024:(q + 1) * 1024, :], w=[("XRi", q)])
            with contextlib.ExitStack() as es0:
                z = self.sb(es0, "zrow", [8, D])
                self.memset(z[:], 0.0)
                for e in range(NE):
                    self.dma(self.YG[e * ESTR + CAP:(e + 1) * ESTR, :], z[:], w=[("YGz", e)])
                self.P.barrier()
            self.phase_mods()
            for l in range(layers):
                self.layer(l)
            self.phase_final()
            self.P.barrier(["sp"])
            self.P.emit()

    def phase_mods(self):
        io = self.io
        with contextlib.ExitStack() as es:
            cc = self.sb(es, "cc", [128, 8, 2])
            ccs = self.sb(es, "ccs", [128, 8, 2])
            self.dma(cc[:, :, 0], io["c"].rearrange("o (k p) -> (o p) k", p=128), allow_slow_non_contiguous=True)
            self.dma(cc[:, :, 1], io["c_ctx"].rearrange("o (k p) -> (o p) k", p=128), allow_slow_non_contiguous=True)
            self.act(ccs[:], cc[:], AF.Silu)
            wm = [self.sb(es, f"wm{i}", [128, 8, 512]) for i in range(2)]
            bm = self.sb(es, "bm", [2, 6 * D])
            mo = self.sb(es, "mo", [2, 6 * D])
            for l in range(DEPTH):
                self.dma(bm[:], io["b_mod"][l:l + 1, :].partition_broadcast(2))
                for nb in range(12):
                    w = wm[nb % 2]
                    for k in range(8):
                        self.dma(w[:, k, :], io["w_mod"][l, k * 128:(k + 1) * 128, nb * 512:(nb + 1) * 512])
                    p = self.ps()
                    for k in range(8):
                        self.mm(p[0:2, :], ccs[:, k, :], w[:, k, :], start=(k == 0), stop=(k == 7))
                    self.tt(mo[:, nb * 512:(nb + 1) * 512], p[0:2, :], bm[:, nb * 512:(nb + 1) * 512], ALU.add)
                self.dma(self.MOD[l], mo[:], w=[("MOD", l)])
        self.P.barrier()

    def bc_row(self, dst, src_row):
        self.dma(dst, src_row.partition_broadcast(128))

    def phase_norm(self, l, which, hT, t0=0, extra=None, after=None):
        io = self.io
        gname = "norm_mix" if which == 1 else "norm_ffn"
        o_sh, o_sc = (0, 1) if which == 1 else (3, 4)
        with contextlib.ExitStack() as es:
            A = [self.sb(es, f"A{j}", [128, D]) for j in range(2)]
            B = [self.sb(es, f"Bv{j}", [128, D]) for j in range(2)]
            g = self.sb(es, "g", [128, D])
            self.bc_row(g[:], io[gname][l:l + 1, :])
            for j in range(2):
                row = 1 - j
                self.bc_row(A[j][:], self.MOD[l, row:row + 1, o_sc * D:(o_sc + 1) * D])
                self.bc_row(B[j][:], self.MOD[l, row:row + 1, o_sh * D:(o_sh + 1) * D])
                self.stt(A[j][:], A[j][:], 1.0, g[:], ALU.add, ALU.mult)
            xt = [self.sb(es, f"xt{j}", [128, D]) for j in range(2)]
            sq = self.sb(es, "sq", [128, D])
            hf = [self.sb(es, f"hf{j}", [128, D]) for j in range(2)]
            hb = self.p_hb
            ss = [self.sb(es, f"ss{j}", [128, 1]) for j in range(2)]
            for i in range(t0, NT):
                j = 0 if i < 2 else 1
                b = i % 2
                self.dma(xt[b][:], self.XR[i * 128:(i + 1) * 128, :], r=[("XR", i)])
                self.act(sq[:], xt[b][:], AF.Square, accum_out=ss[b][:])
                self.ts(ss[b][:], ss[b][:], 1.0 / D, EPS, ALU.mult, ALU.add)
                self.act(ss[b][:], ss[b][:], AF.Sqrt)
                self.recip(ss[b][:], ss[b][:])
                self.stt(hf[b][:], xt[b][:], ss[b][:, 0:1], A[j][:], ALU.mult, ALU.mult)
                self.tt(hf[b][:], hf[b][:], B[j][:], ALU.add)
                self.cp(hb[b][:], hf[b][:], eng="act")
                if hT is not None:
                    pb = self.psb[i % 2]
                    for k in range(8):
                        self.tr(pb[:, k * 128:(k + 1) * 128], hb[b][:, k * 128:(k + 1) * 128], self.identb[:])
                    self.cp(hT[:, :, i * 128:(i + 1) * 128], pb[:].rearrange("p (k n) -> p k n", k=8))
                if extra is not None:
                    extra(i, hf[b], hb[b])
            if after is not None:
                after()
            self.P.barrier()

    def layer(self, l):
        cfg = self.cfg
        ph = cfg.get("phases", "WFHPSGM")
        if "W" in ph:
            self.phase_proj(l)
        if "F" in ph:
            self.phase_fourier(l)
        if "H" in ph:
            self.phase_hyena(l)
        if "P" in ph:
            self.phase_pool(l)
        if "S" in ph:
            self.phase_ssd(l)
        if "G" in ph:
            self.phase_merge(l)
        if "M" in ph:
            self.phase_moe(l)

    def tokblocks(self):
        return [(0, 256)] + [(NCTX + 512 * j, 512) for j in range(8)]

    def phase_proj(self, l):
        io = self.io
        with contextlib.ExitStack() as es:
            hT = self.sb(es, "hT", [128, 8, NTOK], BF16)
            wi = self.sb(es, "wi", [128, 8, INW], BF16)
            wst = [self.sb(es, f"wst{j}", [128, INW]) for j in range(2)]
            for k in range(8):
                self.dma(wst[k % 2][:], io["w_in"][l, k * 128:(k + 1) * 128, :])
                self.cp(wi[:, k, :], wst[k % 2][:], eng=("act" if k % 2 else "pool"))
            self.phase_norm(l, 1, hT)
            ob = [self.sb(es, f"ob{j}", [128, 512]) for j in range(3)]
            fm_cols = [(0, 256), (256, 768), (1792, 1024), (1024, 256)]
            fm_chunks = []
            for c0, n in fm_cols:
                for j in range(n // 128):
                    fm_chunks.append(c0 + j * 128)
            n = 0
            for (t0, tw) in self.tokblocks():
                for oc, c0 in enumerate(fm_chunks):
                    p = self.ps()
                    for k in range(8):
                        self.mm(p[:, 0:tw], wi[:, k, c0:c0 + 128], hT[:, k, t0:t0 + tw], start=(k == 0), stop=(k == 7))
                    o = ob[n % 3]
                    n += 1
                    if n % 2:
                        self.cp(o[:, 0:tw], p[:, 0:tw], eng="act")
                    else:
                        self.cp(o[:, 0:tw], p[:, 0:tw])
                    self.dma(self.FM[oc * 128:(oc + 1) * 128, t0:t0 + tw], o[:, 0:tw], w=[("FM", oc, t0)])
            for i in range(NT):
                p = self.ps()
                for k in range(8):
                    self.mm(p[:, 0:512], hT[:, k, i * 128:(i + 1) * 128], wi[:, k, 1280:1792], start=(k == 0), stop=(k == 7))
                p2 = self.ps()
                for k in range(8):
                    self.mm(p2[:, 0:16], hT[:, k, i * 128:(i + 1) * 128], wi[:, k, 2816:2832], start=(k == 0), stop=(k == 7))
                o = ob[n % 3]
                n += 1
                self.cp(o[:, 0:512], p[:, 0:512], eng="act")
                self.dma(self.TM[i * 128:(i + 1) * 128, 0:512], o[:, 0:512], w=[("TM", i)])
                o = ob[n % 3]
                n += 1
                self.cp(o[:, 0:16], p2[:, 0:16])
                self.dma(self.TM[i * 128:(i + 1) * 128, 512:528], o[:, 0:16], w=[("TMd", i)])
        self.P.barrier()

    def phase_fourier(self, l):
        io = self.io
        with contextlib.ExitStack() as es:
            fT = self.sb(es, "fT", [128, 2, NTOK], BF16)
            for c in range(2):
                self.dma(fT[:, c, :], self.FM[c * 128:(c + 1) * 128, :], eng="pool")
            bd = self.sb(es, "bd", [128, 2, 128], BF16)
            self.dma(bd[:], io["cst_bd64"].rearrange("a p n -> p a n"), eng="pool")
            U = self.sb(es, "U", [128, NT, 512], BF16)
            for i in range(NT):
                p = self.ps()
                for a in range(2):
                    for c in range(2):
                        self.mm(p[:, a * 256 + c * 128:a * 256 + (c + 1) * 128], fT[:, c, i * 128:(i + 1) * 128], bd[:, a, :])
                self.cp(U[:, i, :], p[:], eng=("act" if i % 2 else "dve"))
            ys = [self.sb(es, f"ysf{j}", [128, 512], BF16) for j in range(2)]
            n = 0
            tbc = self.sb(es, "tbc", [128, 2, 2, 256], BF16)
            self.dma(tbc[:].rearrange("p a r c -> p (a r) c"), io["cst_dft4c"].rearrange("a (r p) c -> p (a r) c", p=128))
            sc_c = 1.0 / math.sqrt(NCTX * 64.0)
            for c in range(2):
                p = self.ps()
                cnt = 0
                for tc in range(2):
                    for a in range(2):
                        self.mm(p[:, 0:256], U[:, tc, a * 256 + c * 128:a * 256 + (c + 1) * 128], tbc[:, a, tc, :], start=(cnt == 0), stop=(cnt == 3))
                        cnt += 1
                o = ys[n % 2]
                n += 1
                self.act(o[:, 0:256], p[:, 0:256], AF.Copy, scale=sc_c)
                self.dma(self.YS[c * 128:(c + 1) * 128, 0:256], o[:, 0:256], w=[("YS", c, 0)])
            tbs = [self.sb(es, f"tb{j}", [128, 2, 32, 256], BF16) for j in range(2)]
            sc_l = 1.0 / math.sqrt(NLAT * 64.0)
            for kb in range(16):
                tb = tbs[kb % 2]
                for a in range(2):
                    self.dma(tb[:, a, :, :], io["cst_dft4"][kb, :, a, :, :])
                for c in range(2):
                    p = self.ps()
                    cnt = 0
                    for tc in range(32):
                        for a in range(2):
                            self.mm(p[:, 0:256], U[:, 2 + tc, a * 256 + c * 128:a * 256 + (c + 1) * 128], tb[:, a, tc, :], start=(cnt == 0), stop=(cnt == 63))
                            cnt += 1
                    o = ys[n % 2]
                    n += 1
                    self.act(o[:, 0:256], p[:, 0:256], AF.Copy, scale=sc_l)
                    self.dma(self.YS[c * 128:(c + 1) * 128, NCTX + kb * 256:NCTX + (kb + 1) * 256], o[:, 0:256], w=[("YS", c, kb + 1)])
        self.P.barrier()

    def conv_chunk(self, es_bufs, src_rows, wcol, bcol, out_ap_fn):
        xin, o = es_bufs
        self.dma(xin[:, 1:1 + NCTX], src_rows[:, 0:NCTX])
        self.dma(xin[:, 259:259 + NLAT], src_rows[:, NCTX:NTOK])
        for (b0, n, o0) in ((0, NCTX, 0), (258, NLAT, NCTX)):
            self.ts(o[:, o0:o0 + n], xin[:, b0:b0 + n], wcol[:, 0:1], bcol, ALU.mult, ALU.add)
            self.stt(o[:, o0:o0 + n], xin[:, b0 + 1:b0 + 1 + n], wcol[:, 1:2], o[:, o0:o0 + n], ALU.mult, ALU.add)
            self.stt(o[:, o0:o0 + n], xin[:, b0 + 2:b0 + 2 + n], wcol[:, 2:3], o[:, o0:o0 + n], ALU.mult, ALU.add)
        out_ap_fn(o)

    def phase_hyena(self, l):
        io = self.io
        with contextlib.ExitStack() as es_outer:
            Kh = self.sb(es_outer, "Kh", [128, NT, 512], BF16)
            KN = self.sb(es_outer, "KN", [1, 2, 256])
            rn = self.sb(es_outer, "rn", [128, 2, 2])
            with contextlib.ExitStack() as es:
                w1 = self.sb(es, "w1", [33, 64]); w2 = self.sb(es, "w2", [64, 64]); w3 = self.sb(es, "w3", [64, 512])
                b1 = self.sb(es, "b1", [64, 1]); b2 = self.sb(es, "b2", [64, 1])
                self.dma(w1[:], io["hy_ffn_w1"][l]); self.dma(w2[:], io["hy_ffn_w2"][l]); self.dma(w3[:], io["hy_ffn_w3"][l])
                self.dma(b1[:], io["hy_ffn_b1"][l:l + 1, :].rearrange("o n -> n o"), allow_slow_non_contiguous=True)
                self.dma(b2[:], io["hy_ffn_b2"][l:l + 1, :].rearrange("o n -> n o"), allow_slow_non_contiguous=True)
                d2 = self.sb(es, "d2", [128, 512])
                self.dma(d2[:], io["cst_delta2"])
                tneg = self.sb(es, "tneg", [128, NT])
                self.dma(tneg[:], io["cst_tneg"])
                KSD = self.sb(es, "KSD", [128, NT, 512], BF16)
                ft = self.sb(es, "ft", [33, 512])
                h1 = self.sb(es, "h1", [64, 512]); h2 = self.sb(es, "h2", [64, 512])
                rrf = self.sb(es, "rrf", [64, 512]); rri = self.sb(es, "rri", [64, 512], I32)
                kd = self.sb(es, "kd", [128, 512]); ka = self.sb(es, "ka", [128, 512]); dec = self.sb(es, "dec", [128, 512])
                held = self.hold_ps(3)
                pn = held[0:2]
                pny = held[2]
                for seq, (nseq, tile0, fkey) in enumerate(((NCTX, 0, "cst_featc"), (NLAT, 2, "cst_feat"))):
                    ntile = nseq // 128
                    for jb in range(0, nseq, 512):
                        bw = min(512, nseq - jb)
                        self.dma(ft[:, 0:bw], io[fkey][:, jb:jb + bw])
                        for (src, wgt, bias, dst, kk) in ((ft, w1, b1, h1, 33), (h1, w2, b2, h2, 64)):
                            p = self.ps()
                            self.mm(p[0:64, 0:bw], wgt[0:kk, :], src[0:kk, 0:bw])
                            self.ts(dst[:, 0:bw], p[0:64, 0:bw], bias[:, 0:1], None, ALU.add)
                            self.ts(rrf[:, 0:bw], dst[:, 0:bw], 1.0 / (2 * math.pi), None, ALU.mult)
                            self.cp(rri[:, 0:bw], rrf[:, 0:bw])
                            self.cp(rrf[:, 0:bw], rri[:, 0:bw])
                            self.stt(dst[:, 0:bw], rrf[:, 0:bw], -2 * math.pi, dst[:, 0:bw], ALU.mult, ALU.add)
                            self.act(dst[:, 0:bw], dst[:, 0:bw], AF.Sin)
                        for jt in range(bw // 128):
                            ti = tile0 + (jb // 128) + jt
                            p = self.ps()
                            self.mm(p[:], h2[:, jt * 128:(jt + 1) * 128], w3[:])
                            self.act(dec[:], d2[:], AF.Exp, scale=tneg[:, ti:ti + 1])
                            self.tt(kd[:], p[:], dec[:], ALU.mult)
                            if jb == 0 and jt == 0:
                                self.ts(kd[:, 256:512], kd[:, 256:512], self.misc[:, 1:2], None, ALU.mult)
                            self.act(ka[:], kd[:], AF.Abs)
                            first = (jb == 0 and jt == 0)
                            last = (jb + jt * 128 + 128 == nseq)
                            for c in range(2):
                                for hlf in range(2):
                                    self.mm(pn[c][:, seq:seq + 1], ka[:, hlf * 256 + c * 128:hlf * 256 + (c + 1) * 128], self.tri[:, 3, 0:1],
                                            start=(first and hlf == 0), stop=(last and hlf == 1))
                            self.tt(KSD[:, ti, 0:256], kd[:, 0:256], kd[:, 256:512], ALU.add)
                            self.tt(KSD[:, ti, 256:512], kd[:, 0:256], kd[:, 256:512], ALU.subtract)
                            self.tt(ka[:, 0:256], kd[:, 0:256], kd[:, 256:512], ALU.add)
                            self.mm(pny[0:1, seq * 256:(seq + 1) * 256], self.misc[:, 0:1], ka[:, 0:256], start=first, stop=last)
                    for c in range(2):
                        self.recip(rn[:, c, seq:seq + 1], pn[c][:, seq:seq + 1])
                    self.cp(KN[:, seq, :], pny[0:1, seq * 256:(seq + 1) * 256])
                self.release_ps(held)
                tbc = self.sb(es, "tbc8", [128, 2, 2, 256], BF16)
                self.dma(tbc[:].rearrange("p a r c -> p (a r) c"), io["cst_dft8c"].rearrange("a (r p) c -> p (a r) c", p=128))
                for ftile in range(2):
                    p = self.ps()
                    for a in range(2):
                        for jc in range(2):
                            self.mm(p[:, a * 256:(a + 1) * 256], tbc[:, a, jc, ftile * 128:(ftile + 1) * 128], KSD[:, jc, a * 256:(a + 1) * 256],
                                    start=(jc == 0), stop=(jc == 1))
                    self.cp(Kh[:, ftile, :], p[:], eng="act")
                tbs = [self.sb(es, f"tb8{j}", [128, 2, 32, 256], BF16) for j in range(2)]
                for fb in range(16):
                    tb = tbs[fb % 2]
                    for a in range(2):
                        self.dma(tb[:, a, :, :], io["cst_dft8"][fb, :, a, :, :])
                    for fsub in range(2):
                        ftile = fb * 2 + fsub
                        p = self.ps()
                        for a in range(2):
                            for jc in range(32):
                                self.mm(p[:, a * 256:(a + 1) * 256], tb[:, a, jc, fsub * 128:(fsub + 1) * 128], KSD[:, 2 + jc, a * 256:(a + 1) * 256],
                                        start=(jc == 0), stop=(jc == 31))
                        self.cp(Kh[:, 2 + ftile, :], p[:], eng=("act" if ftile % 2 else "dve"))
            self.P.barrier()
            with contextlib.ExitStack() as es:
                cw = self.sb(es, "cw", [128, 6, 3]); cb = self.sb(es, "cb", [128, 6])
                for k in range(3):
                    self.dma(cw[:, :, k], io["hy_conv_w"][l, k:k + 1, :].rearrange("o (c p) -> (o p) c", p=128), allow_slow_non_contiguous=True)
                self.dma(cb[:], io["hy_conv_b"][l:l + 1, :].rearrange("o (c p) -> (o p) c", p=128), allow_slow_non_contiguous=True)
                xin = self.sb(es, "xin", [128, NTOK + 4])
                self.memset(xin[:], 0.0)
                oo = [self.sb(es, f"cvo{j}", [128, NTOK]) for j in range(3)]
                for c in range(2):
                    self.conv_chunk((xin, oo[0]), self.FM[256 + c * 128:256 + (c + 1) * 128, :], cw[:, c, :], cb[:, c:c + 1],
                                    lambda o, c=c: self.dma(self.X1[c * 128:(c + 1) * 128, :], o[:], w=[("X1", c)]))
                    self.conv_chunk((xin, oo[1]), self.FM[256 + (2 + c) * 128:256 + (3 + c) * 128, :], cw[:, 2 + c, :], cb[:, 2 + c:3 + c], lambda o: None)
                    self.conv_chunk((xin, oo[2]), self.FM[256 + (4 + c) * 128:256 + (5 + c) * 128, :], cw[:, 4 + c, :], cb[:, 4 + c:5 + c], lambda o: None)
                    self.tt(oo[1][:], oo[1][:], oo[2][:], ALU.mult)
                    self.dma(self.VV[c * 128:(c + 1) * 128, :], oo[1][:], w=[("VV", c)])
            self.P.barrier()
            with contextlib.ExitStack() as es:
                Pb = self.sb(es, "Pb", [128, NT, 512], BF16)
                PN = self.sb(es, "PN", [1, 2, 256], BF16)
                with contextlib.ExitStack() as es2:
                    vvb = self.sb(es2, "vvb", [128, 2, NTOK], BF16)
                    for c in range(2):
                        self.dma(vvb[:, c, :], self.VV[c * 128:(c + 1) * 128, :], eng="pool")
                    VVt = self.sb(es2, "VVt", [128, NT, 256], BF16)
                    for i in range(NT):
                        pb = self.psb[i % 2]
                        for c in range(2):
                            self.tr(pb[:, c * 128:(c + 1) * 128], vvb[:, c, i * 128:(i + 1) * 128], self.identb[:])
                        self.cp(VVt[:, i, :], pb[:, 0:256], eng=("act" if i % 2 else "dve"))
                    altb = self.sb(es2, "altb", [128, 1], BF16)
                    self.cp(altb[:], self.misc[:, 0:1])
                    vh = self.sb(es2, "vh", [128, 512]); khf = self.sb(es2, "khf", [128, 512])
                    t1 = self.sb(es2, "t1", [128, 256]); t2 = self.sb(es2, "t2", [128, 256])
                    held2 = self.hold_ps(1)
                    pny = held2[0]
                    vn = self.sb(es2, "vn", [1, 256])

                    def spec_product(ti, p, is_f0):
                        self.cp(vh[:], p[:], eng="act")
                        self.cp(khf[:], Kh[:, ti, :], eng="pool")
                        self.tt(t1[:], vh[:, 0:256], khf[:, 0:256], ALU.mult)
                        self.tt(t2[:], vh[:, 256:512], khf[:, 256:512], ALU.mult)
                        self.tt(t1[:], t1[:], t2[:], ALU.subtract)
                        if is_f0:
                            self.ts(t1[:], t1[:], self.misc[:, 2:3], None, ALU.mult)
                        self.cp(Pb[:, ti, 0:256], t1[:], eng="act")
                        self.tt(t1[:], vh[:, 0:256], khf[:, 256:512], ALU.mult, eng="pool")
                        self.tt(t2[:], vh[:, 256:512], khf[:, 0:256], ALU.mult, eng="pool")
                        self.tt(t1[:], t1[:], t2[:], ALU.add, eng="pool")
                        if is_f0:
                            self.ts(t1[:], t1[:], self.misc[:, 2:3], None, ALU.mult)
                        self.cp(Pb[:, ti, 256:512], t1[:], eng="act")

                    tbc = self.sb(es2, "tbc8b", [128, 2, 2, 256], BF16)
                    self.dma(tbc[:].rearrange("p a r c -> p (a r) c"), io["cst_dft8c"].rearrange("a (r p) c -> p (a r) c", p=128))
                    for ftile in range(2):
                        p = self.ps()
                        for a in range(2):
                            for tc in range(2):
                                self.mm(p[:, a * 256:(a + 1) * 256], tbc[:, a, tc, ftile * 128:(ftile + 1) * 128], VVt[:, tc, :], start=(tc == 0), stop=(tc == 1))
                        spec_product(ftile, p, ftile == 0)
                    for tc in range(2):
                        self.mm(pny[0:1, 0:256], altb[:, 0:1], VVt[:, tc, :], start=(tc == 0), stop=(tc == 1))
                    self.tt(vn[:], pny[0:1, 0:256], KN[:, 0, :], ALU.mult)
                    self.ts(PN[:, 0, :], vn[:], 0.5, None, ALU.mult)
                    tbs = [self.sb(es2, f"tb8b{j}", [128, 2, 32, 256], BF16) for j in range(2)]
                    for fb in range(16):
                        tb = tbs[fb % 2]
                        for a in range(2):
                            self.dma(tb[:, a, :, :], io["cst_dft8"][fb, :, a, :, :])
                        for fsub in range(2):
                            ftile = fb * 2 + fsub
                            p = self.ps()
                            for a in range(2):
                                for tc in range(32):
                                    self.mm(p[:, a * 256:(a + 1) * 256], tb[:, a, tc, fsub * 128:(fsub + 1) * 128], VVt[:, 2 + tc, :], start=(tc == 0), stop=(tc == 31))
                            spec_product(2 + ftile, p, ftile == 0)
                    for tc in range(32):
                        self.mm(pny[0:1, 256:512], altb[:, 0:1], VVt[:, 2 + tc, :], start=(tc == 0), stop=(tc == 31))
                    self.tt(vn[:], pny[0:1, 256:512], KN[:, 1, :], ALU.mult)
                    self.ts(PN[:, 1, :], vn[:], 0.5, None, ALU.mult)
                    self.release_ps(held2)
                self.P.barrier()
                with contextlib.ExitStack() as es2:
                    hb_ = self.sb(es2, "hyb", [128, 2])
                    self.dma(hb_[:], io["hy_bias"][l:l + 1, :].rearrange("o (c p) -> (o p) c", p=128), allow_slow_non_contiguous=True)
                    altrow = self.sb(es2, "altrow", [1, 512], BF16)
                    self.dma(altrow[:], io["cst_altrow"][:, 0:512])
                    yv = [self.sb(es2, f"yv{j}", [128, 512]) for j in range(2)]
                    vvt = [self.sb(es2, f"vvt{j}", [128, 512]) for j in range(2)]
                    x1t = [self.sb(es2, f"x1t{j}", [128, 512]) for j in range(2)]
                    yo = [self.sb(es2, f"yho{j}", [128, 512], BF16) for j in range(2)]
                    scl = self.sb(es2, "scl", [128, 2, 2])
                    self.ts(scl[:, :, 0:1], rn[:, :, 0:1], 2.0 / (2 * NCTX), None, ALU.mult)
                    self.ts(scl[:, :, 1:2], rn[:, :, 1:2], 2.0 / (2 * NLAT), None, ALU.mult)
                    n = 0

                    def finish(c, seq, p, t0, tw):
                        nonlocal n
                        b = n % 2
                        n += 1
                        self.dma(vvt[b][:, 0:tw], self.VV[c * 128:(c + 1) * 128, t0:t0 + tw])
                        self.dma(x1t[b][:, 0:tw], self.X1[c * 128:(c + 1) * 128, t0:t0 + tw])
                        self.act(yv[b][:, 0:tw], p[:, 0:tw], AF.Copy, scale=scl[:, c, seq:seq + 1])
                        self.stt(yv[b][:, 0:tw], vvt[b][:, 0:tw], hb_[:, c:c + 1], yv[b][:, 0:tw], ALU.mult, ALU.add)
                        self.tt(yo[b][:, 0:tw], yv[b][:, 0:tw], x1t[b][:, 0:tw], ALU.mult)
                        self.dma(self.YS[256 + c * 128:256 + (c + 1) * 128, t0:t0 + tw], yo[b][:, 0:tw], w=[("YS", 2 + c, t0)])

                    tbc = self.sb(es2, "tbc8c", [128, 2, 2, 256], BF16)
                    self.dma(tbc[:].rearrange("p a r c -> p (a r) c"), io["cst_dft8c"].rearrange("a (r p) c -> p (a r) c", p=128))
                    for c in range(2):
                        p = self.ps()
                        cnt = 0
                        for fc in range(2):
                            for a in range(2):
                                self.mm(p[:, 0:256], Pb[:, fc, a * 256 + c * 128:a * 256 + (c + 1) * 128], tbc[:, a, fc, :], start=(cnt == 0), stop=False)
                                cnt += 1
                        self.mm(p[:, 0:256], PN[:, 0, c * 128:(c + 1) * 128], altrow[:, 0:256], start=False, stop=True)
                        finish(c, 0, p, 0, 256)
                    tbs = [self.sb(es2, f"tb8c{j}", [128, 2, 32, 256], BF16) for j in range(2)]
                    for tbk in range(16):
                        tb = tbs[tbk % 2]
                        for a in range(2):
                            self.dma(tb[:, a, :, :], io["cst_dft8"][tbk, :, a, :, :])
                        for c in range(2):
                            p = self.ps()
                            cnt = 0
                            for fc in range(32):
                                for a in range(2):
                                    self.mm(p[:, 0:256], Pb[:, 2 + fc, a * 256 + c * 128:a * 256 + (c + 1) * 128], tb[:, a, fc, :], start=(cnt == 0), stop=False)
                                    cnt += 1
                            self.mm(p[:, 0:256], PN[:, 1, c * 128:(c + 1) * 128], altrow[:, 0:256], start=False, stop=True)
                            finish(c, 1, p, NCTX + tbk * 256, 256)
        self.P.barrier()

    def phase_pool(self, l):
        io = self.io
        with contextlib.ExitStack() as es:
            pT = self.sb(es, "pinT", [128, 2, NTOK], BF16)
            for c in range(2):
                self.dma(pT[:, c, :], self.FM[2048 + c * 128:2048 + (c + 1) * 128, :], eng="pool")
            bdwf = self.sb(es, "bdwf", [128, 2, 128])
            self.memset(bdwf[:], 0.0)
            for g in range(4):
                pg, gi = g // 2, g % 2
                self.dma(bdwf[gi * 64:(gi + 1) * 64, pg, gi * 64:(gi + 1) * 64], io["pool_w"][l, g])
            bdw = self.sb(es, "bdw", [128, 2, 128], BF16)
            self.cp(bdw[:], bdwf[:])
            psc = self.sb(es, "psc", [128, 2])
            self.dma(psc[:], io["pool_scale"][l:l + 1, :].rearrange("o (c p) -> (o p) c", p=128), allow_slow_non_contiguous=True)
            pm = self.sb(es, "pm", [128, 4, 128], BF16)
            self.dma(pm[:], io["cst_pool"].rearrange("g s t -> s g t"), eng="pool")
            pmc = self.sb(es, "pmc", [128, 4, 2, 256], BF16)
            self.dma(pmc[:].rearrange("p g r t -> p (g r) t"), io["cst_poolc"].rearrange("g (r p) t -> p (g r) t", p=128), eng="pool")
            Az = [self.sb(es, f"Az{j}", [128, 4, 128], BF16) for j in range(2)]
            for j in range(2):
                self.memset(Az[j][:], 0.0)
            po = [self.sb(es, f"po{j}", [128, 2, 128], BF16) for j in range(2)]

            def make_az(i, az):
                p = self.ps()
                for pg in range(2):
                    self.mm(p[:, pg * 128:(pg + 1) * 128], pT[:, pg, i * 128:(i + 1) * 128], bdw[:, pg, :])
                pv = p[:, 0:256].rearrange("p (pg gi d) -> p pg gi d", pg=2, gi=2)
                azv = az[:].rearrange("p (pg gi) (h d) -> p pg gi h d", pg=2, h=2)
                self.cp(azv[:, :, 0, 0, :], pv[:, :, 0, :])
                self.cp(azv[:, :, 1, 1, :], pv[:, :, 1, :], eng="act")

            make_az(0, Az[0])
            make_az(1, Az[1])
            for tt_ in range(2):
                p = self.ps()
                for pg in range(2):
                    cnt = 0
                    for st in range(2):
                        for gi in range(2):
                            g = pg * 2 + gi
                            self.mm(p[:, pg * 128:(pg + 1) * 128], Az[st][:, g, :], pmc[:, g, st, tt_ * 128:(tt_ + 1) * 128], start=(cnt == 0), stop=(cnt == 3))
                            cnt += 1
                o = po[tt_ % 2]
                for pg in range(2):
                    self.act(o[:, pg, :], p[:, pg * 128:(pg + 1) * 128], AF.Copy, scale=psc[:, pg:pg + 1])
                    self.dma(self.YS[512 + pg * 128:512 + (pg + 1) * 128, tt_ * 128:(tt_ + 1) * 128], o[:, pg, :], w=[("YS", 4 + pg, tt_)])
            self.P.barrier()
            for i in range(2, NT):
                az = Az[i % 2]
                make_az(i, az)
                p = self.ps()
                for pg in range(2):
                    for gi in range(2):
                        g = pg * 2 + gi
                        self.mm(p[:, pg * 128:(pg + 1) * 128], az[:, g, :], pm[:, g, :], start=(gi == 0), stop=(gi == 1))
                o = po[i % 2]
                for pg in range(2):
                    self.act(o[:, pg, :], p[:, pg * 128:(pg + 1) * 128], AF.Copy, scale=psc[:, pg:pg + 1])
                    self.dma(self.YS[512 + pg * 128:512 + (pg + 1) * 128, i * 128:(i + 1) * 128], o[:, pg, :], w=[("YS", 4 + pg, i)])
        self.P.barrier()

    def phase_ssd(self, l):
        io = self.io
        with contextlib.ExitStack() as es:
            cw = self.sb(es, "scw", [128, 8, 3]); cb = self.sb(es, "scb", [128, 8])
            for k in range(3):
                self.dma(cw[:, :, k], io["ssm_conv_w"][l, k:k + 1, :].rearrange("o (c p) -> (o p) c", p=128), allow_slow_non_contiguous=True)
            self.dma(cb[:], io["ssm_conv_b"][l:l + 1, :].rearrange("o (c p) -> (o p) c", p=128), allow_slow_non_contiguous=True)
            xin = self.sb(es, "sxin", [128, NTOK + 4])
            self.memset(xin[:], 0.0)
            oo = [self.sb(es, f"scvo{j}", [128, NTOK]) for j in range(2)]
            ob = [self.sb(es, f"scvb{j}", [128, NTOK], BF16) for j in range(2)]
            for c in range(8):
                def fin(o, c=c):
                    self.act(ob[c % 2][:], o[:], AF.Silu)
                    self.dma(self.XBC[c * 128:(c + 1) * 128, :], ob[c % 2][:], w=[("XBC", c)])
                self.conv_chunk((xin, oo[c % 2]), self.FM[1024 + c * 128:1024 + (c + 1) * 128, :], cw[:, c, :], cb[:, c:c + 1], fin)
        self.P.barrier()
        with contextlib.ExitStack() as es:
            dtb = self.sb(es, "dtb", [128, 16]); abc = self.sb(es, "abc", [128, 16]); dsk = self.sb(es, "dsk", [128, 8])
            snw = self.sb(es, "snw", [128, 512])
            self.bc_row(dtb[:], io["ssm_dt_bias"][l:l + 1].rearrange("o a b -> o (a b)"))
            self.bc_row(abc[:], io["ssm_a_log"][l:l + 1].rearrange("o a b -> o (a b)"))
            self.bc_row(dsk[:], io["ssm_d"][l:l + 1, :])
            self.bc_row(snw[:], io["ssm_norm"][l:l + 1, :])
            self.act(abc[:], abc[:], AF.Exp)
            self.ts(abc[:], abc[:], -1.0, None, ALU.mult)
            H = self.sb(es, "Hst", [128, 512])
            Hb = self.sb(es, "Hstb", [128, 512], BF16)
            xb = [self.sb(es, f"xbct{j}", [128, 8, 128], BF16) for j in range(2)]
            tmt = [self.sb(es, f"tmt{j}", [128, 528]) for j in range(2)]
            xs_t = self.sb(es, "xs_t", [128, 512])
            Bt = self.sb(es, "Bt", [128, 256], BF16)
            dt = self.sb(es, "dt", [128, 8]); dta = self.sb(es, "dta", [128, 8]); tq = self.sb(es, "tq", [128, 8])
            dtax = self.sb(es, "dtax", [128, 8, 128])
            acs = self.sb(es, "acs", [128, 8]); tot = self.sb(es, "tot", [128, 8]); eacs = self.sb(es, "eacs", [128, 8])
            tend = self.sb(es, "tend", [128, 8]); dect = self.sb(es, "dect", [128, 8])
            scm = self.sb(es, "scm", [128, 2, 128])
            seg = self.sb(es, "seg", [128, 4, 128]); M = self.sb(es, "Mm", [128, 8, 128], BF16)
            xdt = self.sb(es, "xdt", [128, 512], BF16); xdtw = self.sb(es, "xdtw", [128, 512], BF16)
            yt = self.sb(es, "yt", [128, 512]); yf = self.sb(es, "yf", [128, 512]); zt = self.sb(es, "zt", [128, 512])
            ysq = self.sb(es, "ysq", [128, 512]); yb = self.sb(es, "yb16", [128, 512], BF16)
            ssq = self.sb(es, "ssq", [128, 1])
            yso = [self.sb(es, f"yso{j}", [128, 4, 128], BF16) for j in range(2)]

            def v3(ap, a, b):
                return ap.rearrange("p (a b) -> p a b", a=a)

            for d in range(2):
                self.memset(H[:], 0.0)
                self.memset(Hb[:], 0.0)
                order = list(range(NT)) if d == 0 else [1, 0] + list(range(NT - 1, 1, -1))
                trisel = self.tri[:, d, :]
                for n_, i in enumerate(order):
                    b = n_ % 2
                    self.dma(xb[b][:], self.XBC.rearrange("(c p) n -> p c n", p=128)[:, :, i * 128:(i + 1) * 128])
                    self.dma(tmt[b][:], self.TM[i * 128:(i + 1) * 128, :])
                    pb = self.psb[n_ % 2]
                    for c in range(6):
                        self.tr(pb[:, c * 128:(c + 1) * 128], xb[b][:, c, :], self.identb[:])
                    self.cp(xs_t[:], pb[:, 0:512], eng="act")
                    self.cp(Bt[:], pb[:, 512:768])
                    self.tt(dt[:], tmt[b][:, 512 + d * 8:520 + d * 8], dtb[:, d * 8:(d + 1) * 8], ALU.add)
                    self.act(tq[:], dt[:], AF.Abs)
                    self.act(tq[:], tq[:], AF.Exp, scale=-1.0)
                    self.act(tq[:], tq[:], AF.Ln, bias=1.0, scale=1.0)
                    self.stt(dt[:], dt[:], 0.0, tq[:], ALU.max, ALU.add)
                    self.tt(dta[:], dt[:], abc[:, d * 8:(d + 1) * 8], ALU.mult)
                    self.cp(dtax[:], dta[:].unsqueeze(2).to_broadcast([128, 8, 128]))
                    p = self.ps()
                    self.mm(p[:, 0:8], trisel, dta[:])
                    self.mm(p[:, 8:16], self.tri[:, 3, :], dta[:])
                    self.cp(acs[:], p[:, 0:8])
                    self.cp(tot[:], p[:, 8:16])
                    self.act(eacs[:], acs[:], AF.Exp)
                    self.tt(tend[:], tot[:], acs[:], ALU.subtract)
                    self.act(tend[:], tend[:], AF.Exp)
                    self.act(dect[:], tot[:], AF.Exp)
                    p = self.ps()
                    for g in range(2):
                        self.mm(p[:, g * 128:(g + 1) * 128], xb[b][:, 4 + g, :], xb[b][:, 6 + g, :])
                    self.tt(scm[:], v3(p[:, 0:256], 2, 128), trisel.unsqueeze(1).to_broadcast([128, 2, 128]), ALU.mult)
                    for g in range(2):
                        p = self.ps()
                        for r in range(4):
                            self.mm(p[:, r * 128:(r + 1) * 128], dtax[:, g * 4 + r, :], trisel)
                        self.tt(seg[:], v3(p[:], 4, 128), acs[:, g * 4:(g + 1) * 4].unsqueeze(2).to_broadcast([128, 4, 128]), ALU.subtract)
                        self.ts(seg[:], seg[:], 0.0, None, ALU.min)
                        self.act(seg[:], seg[:], AF.Exp)
                        self.tt(M[:, g * 4:(g + 1) * 4, :], seg[:], scm[:, g, :].unsqueeze(1).to_broadcast([128, 4, 128]), ALU.mult)
                    self.tt(v3(xdt[:], 8, 64), v3(xs_t[:], 8, 64), dt[:].unsqueeze(2).to_broadcast([128, 8, 64]), ALU.mult)
                    self.tt(tq[:], dt[:], tend[:], ALU.mult)
                    self.tt(v3(xdtw[:], 8, 64), v3(xs_t[:], 8, 64), tq[:].unsqueeze(2).to_broadcast([128, 8, 64]), ALU.mult)
                    pd = self.ps()
                    for hh in range(8):
                        self.mm(pd[:, hh * 64:(hh + 1) * 64], M[:, hh, :], xdt[:, hh * 64:(hh + 1) * 64])
                    po_ = self.ps()
                    for g in range(2):
                        self.mm(po_[:, g * 256:(g + 1) * 256], xb[b][:, 6 + g, :], Hb[:, g * 256:(g + 1) * 256])
                    self.tt(v3(yt[:], 8, 64), v3(po_[:], 8, 64), eacs[:].unsqueeze(2).to_broadcast([128, 8, 64]), ALU.mult)
                    self.tt(yt[:], yt[:], pd[:], ALU.add)
                    pst = self.ps()
                    for g in range(2):
                        self.mm(pst[:, g * 256:(g + 1) * 256], Bt[:, g * 128:(g + 1) * 128], xdtw[:, g * 256:(g + 1) * 256])
                    self.tt(v3(H[:], 8, 64), v3(H[:], 8, 64), dect[:].unsqueeze(2).to_broadcast([128, 8, 64]), ALU.mult)
                    self.tt(H[:], H[:], pst[:], ALU.add)
                    self.cp(Hb[:], H[:], eng="act")
                    if d == 0:
                        self.tt(v3(yf[:], 8, 64), v3(xs_t[:], 8, 64), dsk[:].unsqueeze(2).to_broadcast([128, 8, 64]), ALU.mult)
                        self.tt(yf[:], yf[:], yt[:], ALU.add)
                        self.dma(self.YF[i * 128:(i + 1) * 128, :], yf[:], w=[("YF", i)])
                    else:
                        self.dma(yf[:], self.YF[i * 128:(i + 1) * 128, :], r=[("YF", i)])
                        self.tt(yt[:], yt[:], yf[:], ALU.add)
                        self.act(zt[:], tmt[b][:, 0:512], AF.Silu)
                        self.tt(yt[:], yt[:], zt[:], ALU.mult)
                        self.act(ysq[:], yt[:], AF.Square, accum_out=ssq[:])
                        self.ts(ssq[:], ssq[:], 1.0 / 512, EPS, ALU.mult, ALU.add)
                        self.act(ssq[:], ssq[:], AF.Sqrt)
                        self.recip(ssq[:], ssq[:])
                        self.stt(yb[:], yt[:], ssq[:, 0:1], snw[:], ALU.mult, ALU.mult)
                        pb2 = self.psb[(n_ + 1) % 2]
                        for c in range(4):
                            self.tr(pb2[:, c * 128:(c + 1) * 128], yb[:, c * 128:(c + 1) * 128], self.identb[:])
                        o = yso[n_ % 2]
                        self.cp(o[:], pb2[:, 0:512].rearrange("p (c n) -> p c n", c=4), eng="act")
                        self.dma(self.YS.rearrange("(c p) n -> p c n", p=128)[:, 6:10, i * 128:(i + 1) * 128], o[:], w=[("YS", 6, i)])
                self.P.barrier()

    def phase_merge(self, l):
        io = self.io
        with contextlib.ExitStack() as es:
            hT = self.sb(es, "hT2", [128, 8, NTOK], BF16)
            self.phase_norm(l, 1, hT)
            wg = self.sb(es, "wg", [128, 4, 8, 512], BF16)
            wbr = self.sb(es, "wbr", [128, 10, 512], BF16)
            gst = [self.sb(es, f"gst{j}", [128, 4, 512]) for j in range(2)]
            ysb = [self.sb(es, f"ysb{j}", [128, 10, 512], BF16) for j in range(2)]
            gt = [self.sb(es, f"gt{j}", [128, 512]) for j in range(2)]
            acc = self.sb(es, "macc", [128, 512]); tmp = self.sb(es, "mtmp", [128, 512])
            mo = [self.sb(es, f"mo{j}", [128, 512], BF16) for j in range(2)]
            br_k = [(0, 2), (2, 4), (4, 6), (6, 10)]
            ng = 0
            nblk = 0
            for half in range(2):
                hc = slice(half * 512, (half + 1) * 512)
                for k in range(4):
                    for q in range(2):
                        st = gst[ng % 2]
                        ng += 1
                        self.dma(st[:], io["w_gate"][l, k, q * 512:(q + 1) * 512, hc].rearrange("(c p) n -> p c n", p=128))
                        self.cp(wg[:, k, q * 4:(q + 1) * 4, :], st[:], eng=("act" if ng % 2 else "pool"))
                for (q0, qn) in ((0, 4), (4, 4), (8, 2)):
                    st = gst[ng % 2]
                    ng += 1
                    self.dma(st[:, 0:qn, :], io["w_branch"][l, q0 * 128:(q0 + qn) * 128, hc].rearrange("(c p) n -> p c n", p=128))
                    self.cp(wbr[:, q0:q0 + qn, :], st[:, 0:qn, :], eng=("act" if ng % 2 else "pool"))
                for bi, (t0, tw) in enumerate(self.tokblocks()):
                    yb_ = ysb[nblk % 2]
                    nblk += 1
                    self.dma(yb_[:, :, 0:tw], self.YS.rearrange("(c p) n -> p c n", p=128)[:, :, t0:t0 + tw])
                    for o4 in range(4):
                        oc = half * 4 + o4
                        oc_s = slice(o4 * 128, (o4 + 1) * 128)
                        for k in range(4):
                            pg_ = self.ps()
                            for c in range(8):
                                self.mm(pg_[:, 0:tw], wg[:, k, c, oc_s], hT[:, c, t0:t0 + tw], start=(c == 0), stop=(c == 7))
                            g_ = gt[k % 2]
                            self.act(g_[:, 0:tw], pg_[:, 0:tw], AF.Sigmoid)
                            pb_ = self.ps()
                            c0, c1 = br_k[k]
                            for c in range(c0, c1):
                                self.mm(pb_[:, 0:tw], wbr[:, c, oc_s], yb_[:, c, 0:tw], start=(c == c0), stop=(c == c1 - 1))
                            if k == 0:
                                self.tt(acc[:, 0:tw], pb_[:, 0:tw], g_[:, 0:tw], ALU.mult)
                            else:
                                self.tt(tmp[:, 0:tw], pb_[:, 0:tw], g_[:, 0:tw], ALU.mult)
                                self.tt(acc[:, 0:tw], acc[:, 0:tw], tmp[:, 0:tw], ALU.add, eng="pool")
                        o = mo[o4 % 2]
                        self.cp(o[:, 0:tw], acc[:, 0:tw], eng="act")
                        self.dma(self.MG[oc * 128:(oc + 1) * 128, t0:t0 + tw], o[:, 0:tw], w=[("MG", oc, bi)])
        self.P.barrier()
        with contextlib.ExitStack() as es:
            wo = self.sb(es, "wo", [128, 8, D], BF16)
            wost = [self.sb(es, f"wost{j}", [128, D]) for j in range(2)]
            for k in range(8):
                self.dma(wost[k % 2][:], io["w_out"][l, k * 128:(k + 1) * 128, :])
                self.cp(wo[:, k, :], wost[k % 2][:], eng=("act" if k % 2 else "dve"))
            G = [self.sb(es, f"G{j}", [128, D]) for j in range(2)]
            for j in range(2):
                self.bc_row(G[j][:], self.MOD[l, 1 - j:2 - j, 2 * D:3 * D])
            mt = [self.sb(es, f"mt{j}", [128, 8, 128], BF16) for j in range(2)]
            xt = [self.sb(es, f"xo{j}", [128, D]) for j in range(2)]
            yy = self.sb(es, "yy", [128, D])
            for i in range(NT):
                if l == DEPTH - 1 and i < 2:
                    continue
                b = i % 2
                j = 0 if i < 2 else 1
                self.dma(mt[b][:], self.MG.rearrange("(c p) n -> p c n", p=128)[:, :, i * 128:(i + 1) * 128])
                self.dma(xt[b][:], self.XR[i * 128:(i + 1) * 128, :], r=[("XR", i)])
                for hf in range(2):
                    p = self.ps()
                    for k in range(8):
                        self.mm(p[:], mt[b][:, k, :], wo[:, k, hf * 512:(hf + 1) * 512], start=(k == 0), stop=(k == 7))
                    self.tt(yy[:, hf * 512:(hf + 1) * 512], p[:], G[j][:, hf * 512:(hf + 1) * 512], ALU.mult)
                self.tt(xt[b][:], xt[b][:], yy[:], ALU.add, eng="pool")
                self.dma(self.XR[i * 128:(i + 1) * 128, :], xt[b][:], w=[("XR", i)])
        self.P.barrier()

    def phase_moe(self, l):
        io = self.io
        t0 = 2 if l == DEPTH - 1 else 0
        with contextlib.ExitStack() as es_outer:
            SL = self.sb(es_outer, "SL", [128, NT, 4], I32)
            WS = self.sb(es_outer, "WS", [128, NT, 4])
            CNT = self.sb(es_outer, "CNT", [128, NE], I32)
            with contextlib.ExitStack() as es:
                rw = self.sb(es, "rw", [128, 8, NE])
                self.dma(rw[:], io["router_w"][l].rearrange("(k p) n -> p k n", p=128))
                rb = self.sb(es, "rb", [128, NE])
                self.bc_row(rb[:], io["router_b"][l:l + 1, :])
                ebase = self.sb(es, "ebase", [128, NE])
                self.dma(ebase[:], io["cst_ebase"])
                carry = self.sb(es, "carry", [128, NE])
                self.memset(carry[:], 0.0)
                hTf = self.sb(es, "hTf", [128, 8, 128])
                lg = self.sb(es, "lg", [128, NE]); mx = self.sb(es, "mx", [128, 8]); msk = self.sb(es, "msk", [128, NE])
                mskb = self.sb(es, "mskb", [128, NE], BF16)
                ex = self.sb(es, "ex", [128, NE]); den = self.sb(es, "den", [128, 1]); nmx = self.sb(es, "nmx", [128, 1])
                pos = self.sb(es, "pos", [128, NE]); okm = self.sb(es, "okm", [128, NE]); sv = self.sb(es, "sv", [128, NE])
                mx2 = self.sb(es, "mx2", [128, 8]); slf = self.sb(es, "slf", [128, 4]); junk = self.sb(es, "junk", [128, NE])

                sidx = self.p_idx

                def route(i, hf, hb):
                    for hh in range(2):
                        p = self.ps()
                        for k in range(4):
                            self.tr(p[:, k * 128:(k + 1) * 128], hf[:, (hh * 4 + k) * 128:(hh * 4 + k + 1) * 128], self.identf[:])
                        self.cp(hTf[:, hh * 4:(hh + 1) * 4, :], p[:].rearrange("p (k n) -> p k n", k=4), eng=("act" if hh else "dve"))
                    p = self.ps()
                    for k in range(8):
                        self.mm(p[:, 0:NE], hTf[:, k, :], rw[:, k, :], start=(k == 0), stop=(k == 7))
                    self.tt(lg[:], p[:, 0:NE], rb[:], ALU.add)
                    self.vmax(mx[:], lg[:])
                    self.ts(msk[:], lg[:], mx[:, 3:4], None, ALU.is_ge)
                    self.ts(nmx[:], mx[:, 0:1], -1.0, None, ALU.mult)
                    self.act(ex[:], lg[:], AF.Exp, bias=nmx[:, 0:1], scale=1.0)
                    self.tt(ex[:], ex[:], msk[:], ALU.mult)
                    self.rsum(den[:], ex[:])
                    self.recip(den[:], den[:])
                    self.ts(ex[:], ex[:], den[:, 0:1], None, ALU.mult)
                    self.cp(mskb[:], msk[:])
                    p = self.ps()
                    self.mm(p[:, 0:NE], self.trib[:, 2, :], mskb[:])
                    self.mm(p[:, NE:2 * NE], self.trib[:, 3, :], mskb[:])
                    self.tt(pos[:], p[:, 0:NE], carry[:], ALU.add)
                    self.tt(carry[:], carry[:], p[:, NE:2 * NE], ALU.add)
                    self.ts(okm[:], pos[:], float(CAP), None, ALU.is_lt)
                    self.tt(ex[:], ex[:], okm[:], ALU.mult)
                    self.ts(pos[:], pos[:], float(CAP), None, ALU.min)
                    self.tt(pos[:], pos[:], ebase[:], ALU.add)
                    self.ts(sv[:], pos[:], -1.0, BIG, ALU.mult, ALU.add)
                    self.tt(sv[:], sv[:], msk[:], ALU.mult)
                    self.vmax(mx2[:], sv[:])
                    self.ts(slf[:], mx2[:, 0:4], -1.0, BIG, ALU.mult, ALU.add)
                    self.cp(SL[:, i, :], slf[:])
                    for k in range(4):
                        self.stt(junk[:], sv[:], mx2[:, k:k + 1], ex[:], ALU.is_equal, ALU.mult, accum_out=WS[:, i, k:k + 1])
                    for k in range(4):
                        idxt = sidx[(i % 2) * 4 + k]
                        self.cp(idxt[:], SL[:, i, k:k + 1])
                        idx = idxt[:, :]
                        rr, ww = self._rw([], [hb[:], idx], (), ())
                        self.P.op("pool", lambda e, idx=idx, hb=hb: e.indirect_dma_start(
                            out=self.XG[:, :], out_offset=bass.IndirectOffsetOnAxis(ap=idx, axis=0),
                            in_=hb[:], in_offset=None),
                            rr, [("XGs", i, k)], dma=True)

                self.phase_norm(l, 2, None, t0=t0, extra=route, after=lambda: self.cp(CNT[:], carry[:]))
            self.P.barrier()
            with contextlib.ExitStack() as es:
                wu = [self.sb(es, f"wu{j}", [128, 8, 2048], BF16) for j in range(2)]
                wd = [self.sb(es, f"wd{j}", [128, 8, D], BF16) for j in range(2)]
                bu = [self.sb(es, f"bu{j}", [128, 16]) for j in range(2)]
                bd_ = [self.sb(es, f"bdn{j}", [128, D]) for j in range(2)]
                xg = [self.sb(es, f"xg{j}", [128, D], BF16) for j in range(2)]
                xgT = [self.sb(es, f"xgT{j}", [128, 8, 512], BF16) for j in range(2)]
                actT = self.sb(es, "actT", [128, 8, 512], BF16)
                gq = [self.sb(es, f"gq{j}", [128, 512], BF16) for j in range(3)]
                sg = [self.sb(es, f"sg{j}", [128, 512], BF16) for j in range(3)]
                lq = [self.sb(es, f"lq{j}", [128, 512], BF16) for j in range(3)]
                yo = [self.sb(es, f"yo{j}", [128, D]) for j in range(2)]
                blocks = []
                c0 = 0
                while c0 < CAP:
                    w_ = min(512, CAP - c0)
                    blocks.append((c0, w_))
                    c0 += w_
                nb = 0
                ny = 0
                stg = [self.sb(es, f"stg{j}", [128, 2048]) for j in range(3)]
                nst = [0]

                def loader(e):
                    eb_ = e % 2
                    self.dma(bu[eb_][:], io["exp_b_up"][l, e:e + 1, :].rearrange("o (c p) -> (o p) c", p=128), allow_slow_non_contiguous=True)
                    self.bc_row(bd_[eb_][:], io["exp_b_down"][l, e:e + 1, :])
                    yield
                    for k in range(8):
                        st = stg[nst[0] % 3]
                        nst[0] += 1
                        self.dma(st[:], io["exp_w_up"][l, e, k * 128:(k + 1) * 128, :])
                        self.cp(wu[eb_][:, k, :], st[:], eng="act")
                        yield
                    for k in range(8):
                        st = stg[nst[0] % 3]
                        nst[0] += 1
                        self.dma(st[:, 0:D], io["exp_w_down"][l, e, k * 128:(k + 1) * 128, :])
                        self.cp(wd[eb_][:, k, :], st[:, 0:D], eng="act")
                        yield

                for _ in loader(0):
                    pass
                for e in range(NE):
                    eb = e % 2
                    nxt = loader(e + 1) if e + 1 < NE else iter(())
                    for (c0, w_) in blocks:
                        xT = xgT[nb % 2]
                        nb += 1
                        cnt_ap = CNT[0:1, e:e + 1]
                        for s in range(w_ // 128):
                            r0 = e * ESTR + c0 + s * 128
                            xt_ = xg[s % 2]
                            self.dma(xt_[:], self.XG[r0:r0 + 128, :], r=[("XGl", r0)])
                            pb = self.psb[s % 2]
                            self.P.set_pe_cond((cnt_ap, c0 + s * 128 + 1))
                            for k in range(8):
                                self.tr(pb[:, k * 128:(k + 1) * 128], xt_[:, k * 128:(k + 1) * 128], self.identb[:])
                            self.P.set_pe_cond(None)
                            self.cp(xT[:, :, s * 128:(s + 1) * 128], pb[:].rearrange("p (k n) -> p k n", k=8), eng=("act" if s % 2 else "dve"))
                        def stage_a(fc):
                            self.P.set_pe_cond((cnt_ap, c0 + 1))
                            pg_ = self.ps()
                            for k in range(8):
                                self.mm(pg_[:, 0:w_], wu[eb][:, k, fc * 128:(fc + 1) * 128], xT[:, k, 0:w_], start=(k == 0), stop=(k == 7))
                            pl_ = self.ps()
                            for k in range(8):
                                self.mm(pl_[:, 0:w_], wu[eb][:, k, 1024 + fc * 128:1024 + (fc + 1) * 128], xT[:, k, 0:w_], start=(k == 0), stop=(k == 7))
                            self.P.set_pe_cond(None)
                            g_ = gq[fc % 3]; s_ = sg[fc % 3]; l_ = lq[fc % 3]
                            self.ts(g_[:, 0:w_], pg_[:, 0:w_], bu[eb][:, fc:fc + 1], 7.0, ALU.add, ALU.min)
                            self.act(s_[:, 0:w_], g_[:, 0:w_], AF.Sigmoid, scale=1.702)
                            self.ts(l_[:, 0:w_], pl_[:, 0:w_], bu[eb][:, 8 + fc:9 + fc], 7.0, ALU.add, ALU.min)
                            next(nxt, None)

                        def stage_b(fc):
                            g_ = gq[fc % 3]; s_ = sg[fc % 3]; l_ = lq[fc % 3]
                            self.ts(l_[:, 0:w_], l_[:, 0:w_], -7.0, 1.0, ALU.max, ALU.add)
                            self.tt(g_[:, 0:w_], g_[:, 0:w_], s_[:, 0:w_], ALU.mult)
                            self.tt(actT[:, fc, 0:w_], g_[:, 0:w_], l_[:, 0:w_], ALU.mult)

                        stage_a(0)
                        for fc in range(1, 8):
                            stage_a(fc)
                            stage_b(fc - 1)
                        stage_b(7)
                        for s in range(w_ // 128):
                            r0 = e * ESTR + c0 + s * 128
                            o = yo[ny % 2]
                            ny += 1
                            cond_s = (cnt_ap, c0 + s * 128 + 1)
                            for hf in range(2):
                                p = self.ps()
                                self.P.set_pe_cond(cond_s)
                                for fc in range(8):
                                    self.mm(p[:], actT[:, fc, s * 128:(s + 1) * 128], wd[eb][:, fc, hf * 512:(hf + 1) * 512], start=(fc == 0), stop=(fc == 7))
                                self.P.set_pe_cond(None)
                                self.tt(o[:, hf * 512:(hf + 1) * 512], p[:], bd_[eb][:, hf * 512:(hf + 1) * 512], ALU.add)
                            self.dma(self.YG[r0:r0 + 128, :], o[:], w=[("YGs", r0)])
                    for _ in nxt:
                        pass
            self.P.barrier()
            with contextlib.ExitStack() as es:
                G2 = [self.sb(es, f"G2{j}", [128, D]) for j in range(2)]
                for j in range(2):
                    self.bc_row(G2[j][:], self.MOD[l, 1 - j:2 - j, 5 * D:6 * D])
                gk = self.p_gk
                xt = [self.sb(es, f"xc{j}", [128, D]) for j in range(2)]
                acc = self.sb(es, "cacc", [128, D])
                cidx = self.p_idx
                for i in range(t0, NT):
                    b = i % 2
                    j = 0 if i < 2 else 1
                    self.dma(xt[b][:], self.XR[i * 128:(i + 1) * 128, :], r=[("XR", i)])
                    for k in range(4):
                        idxt = cidx[(i % 2) * 4 + k]
                        self.cp(idxt[:], SL[:, i, k:k + 1])
                        idx = idxt[:, :]
                        gk_ = gk[k]
                        rr, ww = self._rw([gk_[:]], [idx], (), ())
                        self.P.op("pool", lambda e, idx=idx, gk_=gk_: e.indirect_dma_start(
                            out=gk_[:], out_offset=None, in_=self.YG[:, :],
                            in_offset=bass.IndirectOffsetOnAxis(ap=idx, axis=0)), rr, ww, dma=True)
                    self.ts(acc[:], gk[0][:], WS[:, i, 0:1], None, ALU.mult)
                    for k in range(1, 4):
                        self.stt(acc[:], gk[k][:], WS[:, i, k:k + 1], acc[:], ALU.mult, ALU.add)
                    self.tt(acc[:], acc[:], G2[j][:], ALU.mult)
                    self.tt(xt[b][:], xt[b][:], acc[:], ALU.add)
                    self.dma(self.XR[i * 128:(i + 1) * 128, :], xt[b][:], w=[("XR", i)])
        self.P.barrier()

    def phase_final(self):
        io = self.io
        with contextlib.ExitStack() as es:
            g = self.sb(es, "gfin", [128, D])
            self.bc_row(g[:], io["norm_final"])
            xt = [self.sb(es, f"xf{j}", [128, D]) for j in range(2)]
            sq = self.sb(es, "sqf", [128, D])
            ss = [self.sb(es, f"ssf{j}", [128, 1]) for j in range(2)]
            o = [self.sb(es, f"of{j}", [128, D]) for j in range(2)]
            for i in range(2, NT):
                b = i % 2
                self.dma(xt[b][:], self.XR[i * 128:(i + 1) * 128, :], r=[("XR", i)])
                self.act(sq[:], xt[b][:], AF.Square, accum_out=ss[b][:])
                self.ts(ss[b][:], ss[b][:], 1.0 / D, EPS, ALU.mult, ALU.add)
                self.act(ss[b][:], ss[b][:], AF.Sqrt)
                self.recip(ss[b][:], ss[b][:])
                self.stt(o[b][:], xt[b][:], ss[b][:, 0:1], g[:], ALU.mult, ALU.mult)
                self.dma(io["out"][(i - 2) * 128:(i - 1) * 128, :], o[b][:], w=[("out", i)])


W_NAMES = ['w_mod', 'b_mod', 'norm_mix', 'norm_ffn', 'w_in', 'hy_conv_w', 'hy_conv_b', 'hy_ffn_w1', 'hy_ffn_b1',
           'hy_ffn_w2', 'hy_ffn_b2', 'hy_ffn_w3', 'hy_bias', 'pool_w', 'pool_scale', 'ssm_conv_w', 'ssm_conv_b',
           'ssm_dt_bias', 'ssm_a_log', 'ssm_d', 'ssm_norm', 'w_branch', 'w_gate', 'w_out', 'router_w', 'router_b',
           'exp_w_up', 'exp_b_up', 'exp_w_down', 'exp_b_down']

_CONST_CACHE = {}


def make_constants():
    if _CONST_CACHE:
        return _CONST_CACHE
    bf = ml_dtypes.bfloat16
    c = {}
    c["cst_ident"] = np.eye(128, dtype=np.float32)
    t = np.arange(128)
    tri = np.zeros((4, 128, 128), np.float32)
    tri[0] = (t[:, None] <= t[None, :])
    tri[1] = (t[:, None] >= t[None, :])
    tri[2] = (t[:, None] < t[None, :])
    tri[3] = 1.0
    c["cst_tri"] = tri
    misc = np.ones((128, 8), np.float32)
    misc[:, 0] = (-1.0) ** t
    misc[0, 1] = 0.0
    misc[0, 2] = 0.5
    c["cst_misc"] = misc
    c["cst_altrow"] = ((-1.0) ** np.arange(4096)).astype(np.float32).reshape(1, 4096).astype(bf)
    m = np.arange(64)
    a64 = 2 * np.pi * np.outer(m, m) / 64.0
    bd = np.zeros((2, 128, 128), np.float32)
    for g in range(2):
        bd[0, g * 64:(g + 1) * 64, g * 64:(g + 1) * 64] = np.cos(a64)
        bd[1, g * 64:(g + 1) * 64, g * 64:(g + 1) * 64] = np.sin(a64)
    c["cst_bd64"] = bd

    def dft(n, period):
        k = np.arange(n, dtype=np.int64)
        ph = (np.outer(k, k) % period).astype(np.float64) * (2 * np.pi / period)
        out = np.empty((2, n, n), bf)
        out[0] = np.cos(ph).astype(np.float32).astype(bf)
        out[1] = (-np.sin(ph)).astype(np.float32).astype(bf)
        return out

    def tiled(t):
        return np.ascontiguousarray(t.reshape(2, 32, 128, 16, 256).transpose(3, 2, 0, 1, 4))

    c["cst_dft4"] = tiled(dft(NLAT, NLAT))
    c["cst_dft4c"] = dft(NCTX, NCTX)
    c["cst_dft8"] = tiled(dft(NLAT, 2 * NLAT))
    c["cst_dft8c"] = dft(NCTX, 2 * NCTX)

    def poolmat(row_len, nrows):
        n = row_len * nrows
        out = np.zeros((4, n, n), np.float32)
        pos = np.arange(row_len)
        for gi, win in enumerate((2, 4, 8, 16)):
            lo = np.clip(pos - win // 2, 0, row_len)
            hi = np.clip(pos + win // 2, 0, row_len)
            blk = np.zeros((row_len, row_len), np.float32)
            for tt in range(row_len):
                blk[tt, lo[tt]:hi[tt]] = 1.0 / float(hi[tt] - lo[tt])
            blk -= np.eye(row_len, dtype=np.float32)
            for r in range(nrows):
                out[gi, r * row_len:(r + 1) * row_len, r * row_len:(r + 1) * row_len] = blk.T
        return out

    c["cst_pool"] = poolmat(64, 2)
    c["cst_poolc"] = poolmat(256, 1)

    def feats(n):
        pos = np.arange(n, dtype=np.float32)
        tt = pos / np.float32(n - 1)
        ang = (np.float32(2.0 * math.pi) * pos / np.float32(n)).astype(np.float32)
        freqs = np.linspace(1e-4, 15, 16, dtype=np.float32)
        f = np.concatenate([tt[:, None], np.cos(ang[:, None] * freqs), -np.sin(ang[:, None] * freqs)], axis=-1).astype(np.float32)
        return np.ascontiguousarray(f.T), tt

    fl, tl = feats(NLAT)
    fc, tc = feats(NCTX)
    c["cst_feat"] = fl
    c["cst_featc"] = fc
    tneg = np.zeros((128, NT), np.float32)
    tneg[:, 0:2] = -tc.reshape(2, 128).T
    tneg[:, 2:] = -tl.reshape(32, 128).T
    c["cst_tneg"] = tneg
    deltas = np.linspace(HY_MIN, HY_MAX, 256, dtype=np.float32)
    c["cst_delta2"] = np.ascontiguousarray(np.broadcast_to(np.concatenate([deltas, deltas])[None, :], (128, 512))).astype(np.float32)
    c["cst_ebase"] = np.ascontiguousarray(np.broadcast_to((np.arange(NE) * ESTR).astype(np.float32)[None, :], (128, NE)))
    _CONST_CACHE.update(c)
    return c


def build_program(cfg):
    nc = bass.Bass("TRN2", target_bir_lowering=False)
    io = {}

    def inp(name, shape, dt=F32):
        io[name] = nc.dram_tensor(name, list(shape), dt, kind="ExternalInput").ap()

    inp("x", [NLAT, D]); inp("ctx", [NCTX, D]); inp("c", [1, D]); inp("c_ctx", [1, D])
    shapes = cfg["shapes"]
    for n in W_NAMES:
        inp(n, shapes[n])
    inp("norm_final", [1, D])
    consts = make_constants()
    for n, a in consts.items():
        inp(n, a.shape, BF16 if a.dtype == ml_dtypes.bfloat16 else F32)
    io["out"] = nc.dram_tensor("out", [NLAT, D], F32, kind="ExternalOutput").ap()
    k = Kern(nc, io, cfg)
    k.build()
    return nc, k


def kernel(**inputs):
    cfg = {"shapes": {n: list(inputs[n].shape) for n in W_NAMES}}
    env = os.environ
    if env.get("MK_LAYERS"):
        cfg["layers"] = int(env["MK_LAYERS"])
    if env.get("MK_PHASES"):
        cfg["phases"] = env["MK_PHASES"]
    if env.get("MK_DUMP"):
        cfg["dump"] = tuple(env["MK_DUMP"].split(","))
    nc, k = build_program(cfg)
    consts = make_constants()
    shared = {n: np.ascontiguousarray(inputs[n], dtype=np.float32) for n in W_NAMES}
    shared["norm_final"] = np.ascontiguousarray(inputs["norm_final"], dtype=np.float32).reshape(1, D)
    shared["c_ctx"] = np.ascontiguousarray(inputs["c_ctx"], dtype=np.float32).reshape(1, D)
    shared.update(consts)
    ncore = int(env.get("MK_CORES", "8"))
    in_maps = []
    for b in range(ncore):
        m = dict(shared)
        m["x"] = np.ascontiguousarray(inputs["x"][b], dtype=np.float32)
        m["ctx"] = np.ascontiguousarray(inputs["ctx"][b], dtype=np.float32)
        m["c"] = np.ascontiguousarray(inputs["c"][b], dtype=np.float32).reshape(1, D)
        in_maps.append(m)
    res = run_bass_kernel_spmd(nc, in_maps, core_ids=list(range(ncore)))
    out = np.zeros((8, NLAT, D), np.float32)
    for b in range(ncore):
        out[b] = res.results[b]["out"]
    if cfg.get("dump"):
        kernel.last_results = res.results
    return out
```

```python
import os
import math
import contextlib
import numpy as np
import ml_dtypes
import concourse.bass as bass
import concourse.mybir as mybir
from concourse.bass_utils import run_bass_kernel_spmd

F32 = mybir.dt.float32
BF16 = mybir.dt.bfloat16
I32 = mybir.dt.int32
AF = mybir.ActivationFunctionType
ALU = mybir.AluOpType
AX = mybir.AxisListType

SEM_ROT = 30000
DMA_SLOTS = 6

D = 1024
NLAT = 4096
NCTX = 256
NTOK = NLAT + NCTX
NT = NTOK // 128
DEPTH = 4
INW = 2832
NE = 32
CAP = 1280
ESTR = CAP + 8
BIG = 65536.0
EPS = 1e-6
HY_MIN = -math.log(1e-2) / 1.5
HY_MAX = -math.log(1e-2) / 0.3


class Prog:
    ENG = ("pe", "act", "dve", "pool", "sp")

    def __init__(self, nc):
        self.nc = nc
        self.q = {e: [] for e in self.ENG}
        self.cnt = {e: 0 for e in self.ENG}
        self.gen = {e: 0 for e in self.ENG}
        self.clock = {e: {} for e in self.ENG}
        self.res = {}
        self.semnames = []
        self.dma_slots = {e: [[self._newsem(f"dq_{e}_{i}"), 0] for i in range(DMA_SLOTS)]
                          for e in ("sp", "act", "pool")}
        self.dma_i = {e: 0 for e in ("sp", "act", "pool")}
        self.cursem = {e: self._newsem(f"s_{e}_0") for e in self.ENG}
        self.nops = 0
        self.pe_cond = None
        self.pe_snap = None

    def set_pe_cond(self, cond):
        if self.pe_cond is not None:
            self.clock["pe"] = dict(self.pe_snap)
        self.pe_cond = cond
        self.pe_snap = dict(self.clock["pe"]) if cond is not None else None

    def _newsem(self, name):
        self.semnames.append(name)
        return name

    def op(self, eng, fn, reads=(), writes=(), dma=False):
        deps = []
        for k in reads:
            r = self.res.get(k)
            if r is not None and r[0] is not None:
                deps.append(r[0])
        for k in writes:
            r = self.res.get(k)
            if r is not None:
                if r[0] is not None:
                    deps.append(r[0])
                deps.extend(r[1])
        clk = self.clock[eng]
        if dma:
            slots = self.dma_slots[eng]
            slot = slots[self.dma_i[eng] % len(slots)]
            self.dma_i[eng] += 1
            if slot[1] > 0:
                deps.append((slot[0], slot[1], None))
            if slot[1] + 16 > SEM_ROT:
                slot[0] = self._newsem(slot[0] + "r")
                slot[1] = 0
            slot[1] += 16
            ev_sem, ev_val, inc = slot[0], slot[1], 16
        else:
            if self.cnt[eng] + 1 > SEM_ROT:
                self.gen[eng] += 1
                self.cursem[eng] = self._newsem(f"s_{eng}_{self.gen[eng]}")
                self.cnt[eng] = 0
            self.cnt[eng] += 1
            ev_sem, ev_val, inc = self.cursem[eng], self.cnt[eng], 1
        waits = {}
        for (s, v, c) in deps:
            if eng == "pe" and s.startswith("s_pe_"):
                continue
            if clk.get(s, 0) >= v:
                continue
            if waits.get(s, 0) < v:
                waits[s] = v
        for (s, v, c) in deps:
            if s in waits:
                if c:
                    for ks, kv in c.items():
                        if clk.get(ks, 0) < kv:
                            clk[ks] = kv
                if clk.get(s, 0) < v:
                    clk[s] = v
        cond = self.pe_cond if eng == "pe" else None
        evclk = dict(self.pe_snap) if cond is not None else dict(clk)
        evclk[ev_sem] = ev_val
        ev = (ev_sem, ev_val, evclk)
        self.q[eng].append((list(waits.items()), fn, ev_sem, inc, cond))
        self.nops += 1
        for k in writes:
            self.res[k] = [ev, []]
        for k in reads:
            if k in writes:
                continue
            r = self.res.get(k)
            if r is None:
                self.res[k] = [None, [ev]]
            else:
                r[1].append(ev)
                if len(r[1]) > 24:
                    best = {}
                    for e_ in r[1]:
                        if e_[0] not in best or best[e_[0]][1] < e_[1]:
                            best[e_[0]] = e_
                    r[1] = list(best.values())
        return ev

    def barrier(self, engines=None):
        latest = {}
        for r in self.res.values():
            evs = list(r[1])
            if r[0] is not None:
                evs.append(r[0])
            for (s, v, c) in evs:
                if latest.get(s, 0) < v:
                    latest[s] = v
        for e in self.ENG:
            for s, v in self.dma_slots.get(e, []):
                if v > 0 and latest.get(s, 0) < v:
                    latest[s] = v
            if self.cnt[e] > 0 and latest.get(self.cursem[e], 0) < self.cnt[e]:
                latest[self.cursem[e]] = self.cnt[e]
        for e in (engines or self.ENG):
            clk = self.clock[e]
            waits = [(s, v) for s, v in latest.items() if clk.get(s, 0) < v]
            for s, v in waits:
                clk[s] = v
            if waits:
                self.q[e].append((waits, None, None, 0, None))
        self.res = {}

    def emit(self):
        nc = self.nc
        sems = {n: nc.alloc_semaphore(name=n) for n in self.semnames}
        engmap = {"pe": "tensor", "act": "scalar", "dve": "vector", "pool": "gpsimd", "sp": "sync"}
        with nc.Block() as block:
            for e in self.ENG:
                ops = self.q[e]

                def emit_run(eng, run):
                    for waits, fn, ev_sem, inc, cond in run:
                        for s, v in waits:
                            eng.wait_ge(sems[s], v)
                        if fn is not None:
                            ins = fn(eng)
                            ins.then_inc(sems[ev_sem], inc)

                def body(eng, ops=ops, e=e):
                    if e != "pe":
                        emit_run(eng, ops)
                        return
                    running = {}
                    last = None
                    with eng.register("cnd") as reg:
                        i = 0
                        n = len(ops)
                        while i < n:
                            cond = ops[i][4]
                            j = i
                            while j < n and ops[j][4] is cond:
                                j += 1
                            run = ops[i:j]
                            incs = {}
                            for waits, fn, ev_sem, inc, c_ in run:
                                if fn is not None:
                                    incs[ev_sem] = incs.get(ev_sem, 0) + inc
                            if cond is None:
                                emit_run(eng, run)
                            else:
                                ap, thr = cond
                                if last is not None:
                                    eng.wait_ge(sems[last], running[last])
                                eng.reg_load(reg, ap)
                                with eng.If_lt(reg, thr):
                                    for s_, tot in incs.items():
                                        eng.sem_inc(sems[s_], tot)
                                with eng.Else():
                                    emit_run(eng, run)
                            for s_, tot in incs.items():
                                running[s_] = running.get(s_, 0) + tot
                                last = s_
                            i = j

                getattr(block, engmap[e])(body)


def _nm(ap):
    return ap.name


class Kern:
    def __init__(self, nc, io, cfg):
        self.nc = nc
        self.io = io
        self.cfg = cfg
        self.P = Prog(nc)
        self.uid = 0
        self.psi = 0

    def sb(self, es, name, shape, dt=F32):
        self.uid += 1
        return es.enter_context(self.nc.sbuf_tensor(f"{name}_{self.uid}", list(shape), dt))

    def dram(self, name, shape, dt=F32):
        kind = "ExternalOutput" if name in self.cfg.get("dump", ()) else "Internal"
        return self.nc.dram_tensor(name, list(shape), dt, kind=kind).ap()

    def ps(self):
        t = self.psf[self.psi % len(self.psf)]
        self.psi += 1
        return t

    def hold_ps(self, n):
        held = [self.psf.pop() for _ in range(n)]
        return held

    def release_ps(self, held):
        self.psf.extend(held)

    def _rw(self, outs, ins, r, w):
        reads = list(r)
        writes = list(w)
        for a in ins:
            if a is None or isinstance(a, (int, float)):
                continue
            n = _nm(a)
            if n.startswith("ps"):
                writes.append(n)
            else:
                reads.append(n)
        for a in outs:
            writes.append(_nm(a))
        return reads, writes

    def dma(self, out, in_, eng="sp", r=None, w=None, **kw):
        reads = list(r) if r is not None else [_nm(in_)]
        writes = list(w) if w is not None else [_nm(out)]
        self.P.op(eng, lambda e: e.dma_start(out=out, in_=in_, **kw), reads, writes, dma=True)

    def mm(self, out, lhsT, rhs, start=True, stop=True):
        reads, writes = self._rw([out], [lhsT, rhs], (), ())
        self.P.op("pe", lambda e: e.matmul(out, lhsT=lhsT, rhs=rhs, start=start, stop=stop), reads, writes)

    def tr(self, out, in_, ident):
        reads, writes = self._rw([out], [in_, ident], (), ())
        self.P.op("pe", lambda e: e.transpose(out=out, in_=in_, identity=ident), reads, writes)

    def act(self, out, in_, func, bias=None, scale=None, accum_out=None, eng="act"):
        kw = {}
        if bias is not None:
            kw["bias"] = bias
        if scale is not None:
            kw["scale"] = scale
        outs = [out]
        if accum_out is not None:
            kw["accum_out"] = accum_out
            outs.append(accum_out)
        reads, writes = self._rw(outs, [in_, bias, scale], (), ())
        self.P.op(eng, lambda e: e.activation(out=out, in_=in_, func=func, **kw), reads, writes)

    def cp(self, out, in_, eng="dve"):
        reads, writes = self._rw([out], [in_], (), ())
        if eng == "act":
            self.P.op("act", lambda e: e.copy(out=out, in_=in_), reads, writes)
        else:
            self.P.op(eng, lambda e: e.tensor_copy(out=out, in_=in_), reads, writes)

    def tt(self, out, in0, in1, op, eng="dve"):
        reads, writes = self._rw([out], [in0, in1], (), ())
        self.P.op(eng, lambda e: e.tensor_tensor(out=out, in0=in0, in1=in1, op=op), reads, writes)

    def ts(self, out, in0, s1, s2, op0, op1=None, eng="dve"):
        reads, writes = self._rw([out], [in0, s1, s2], (), ())
        if op1 is None:
            self.P.op(eng, lambda e: e.tensor_scalar(out=out, in0=in0, scalar1=s1, scalar2=None, op0=op0), reads, writes)
        else:
            self.P.op(eng, lambda e: e.tensor_scalar(out=out, in0=in0, scalar1=s1, scalar2=s2, op0=op0, op1=op1), reads, writes)

    def stt(self, out, in0, scalar, in1, op0, op1, accum_out=None, eng="dve"):
        outs = [out]
        kw = {}
        if accum_out is not None:
            kw["accum_out"] = accum_out
            outs.append(accum_out)
        reads, writes = self._rw(outs, [in0, scalar, in1], (), ())
        self.P.op(eng, lambda e: e.scalar_tensor_tensor(out=out, in0=in0, scalar=scalar, in1=in1, op0=op0, op1=op1, **kw), reads, writes)

    def memset(self, ap, val, eng="dve"):
        reads, writes = self._rw([ap], [], (), ())
        self.P.op(eng, lambda e: e.memset(ap, val), reads, writes)

    def vmax(self, out, in_):
        reads, writes = self._rw([out], [in_], (), ())
        self.P.op("dve", lambda e: e.max(out=out, in_=in_), reads, writes)

    def recip(self, out, in_):
        reads, writes = self._rw([out], [in_], (), ())
        self.P.op("dve", lambda e: e.reciprocal(out=out, in_=in_), reads, writes)

    def rsum(self, out, in_):
        reads, writes = self._rw([out], [in_], (), ())
        self.P.op("dve", lambda e: e.reduce_sum(out=out, in_=in_, axis=AX.X), reads, writes)

    def build(self):
        nc, io, cfg = self.nc, self.io, self.cfg
        layers = cfg.get("layers", DEPTH)
        with contextlib.ExitStack() as es:
            self.psf = [es.enter_context(nc.psum_tensor(f"psF{i}", [128, 512], F32)) for i in range(6)]
            self.psb = [es.enter_context(nc.psum_tensor(f"psB{i}", [128, 1024], BF16)) for i in range(2)]
            self.identf = self.sb(es, "identf", [128, 128])
            self.identb = self.sb(es, "identb", [128, 128], BF16)
            self.tri = self.sb(es, "tri", [128, 4, 128])
            self.trib = self.sb(es, "trib", [128, 4, 128], BF16)
            self.misc = self.sb(es, "misc", [128, 8])
            self.dma(self.identf[:], io["cst_ident"])
            self.dma(self.tri[:], io["cst_tri"].rearrange("a p n -> p a n"))
            self.dma(self.misc[:], io["cst_misc"])
            self.p_hb = [self.sb(es, f"p_hb{j}", [128, D], BF16) for j in range(2)]
            self.p_idx = [self.sb(es, f"p_idx{j}", [128, 1], I32) for j in range(8)]
            self.cp(self.identb[:], self.identf[:])
            self.cp(self.trib[:], self.tri[:])
            self.XR = self.dram("XR", [NTOK, D])
            self.MOD = self.dram("MOD", [DEPTH, 2, 6 * D])
            self.FM = self.dram("FM", [2048 + 256, NTOK])
            self.TM = self.dram("TM", [NTOK, 528])
            self.YS = self.dram("YS", [1280, NTOK], BF16)
            self.MG = self.dram("MG", [D, NTOK], BF16)
            self.X1 = self.dram("X1", [256, NTOK])
            self.VV = self.dram("VV", [256, NTOK])
            self.XBC = self.dram("XBC", [1024, NTOK], BF16)
            self.YF = self.dram("YF", [NTOK, 512])
            self.YB = self.dram("YB", [NTOK, 512])
            self.XG = self.dram("XG", [NE * ESTR, D], BF16)
            self.YG = self.dram("YG", [NE * ESTR, D])
            self.dma(self.XR[0:NCTX, :], io["ctx"], w=[("XR", 0), ("XR", 1)])
            for q in range(4):
                self.dma(self.XR[NCTX + q * 1024:NCTX + (q + 1) * 1024, :], io["x"][q * 1024:(q + 1) * 1024, :], w=[("XRi", q)])
            with contextlib.ExitStack() as es0:
                z = self.sb(es0, "zrow", [8, D])
                self.memset(z[:], 0.0)
                for e in range(NE):
                    self.dma(self.YG[e * ESTR + CAP:(e + 1) * ESTR, :], z[:], w=[("YGz", e)])
                self.P.barrier()
            self.phase_mods()
            for l in range(layers):
                self.layer(l)
            self.phase_final()
            self.P.barrier(["sp"])
            self.P.emit()

    def phase_mods(self):
        io = self.io
        with contextlib.ExitStack() as es:
            cc = self.sb(es, "cc", [128, 8, 2])
            ccs = self.sb(es, "ccs", [128, 8, 2])
            self.dma(cc[:, :, 0], io["c"].rearrange("o (k p) -> (o p) k", p=128), allow_slow_non_contiguous=True)
            self.dma(cc[:, :, 1], io["c_ctx"].rearrange("o (k p) -> (o p) k", p=128), allow_slow_non_contiguous=True)
            self.act(ccs[:], cc[:], AF.Silu)
            wm = [self.sb(es, f"wm{i}", [128, 8, 512]) for i in range(2)]
            bm = self.sb(es, "bm", [2, 6 * D])
            mo = self.sb(es, "mo", [2, 6 * D])
            for l in range(DEPTH):
                self.dma(bm[:], io["b_mod"][l:l + 1, :].partition_broadcast(2))
                for nb in range(12):
                    w = wm[nb % 2]
                    for k in range(8):
                        self.dma(w[:, k, :], io["w_mod"][l, k * 128:(k + 1) * 128, nb * 512:(nb + 1) * 512])
                    p = self.ps()
                    for k in range(8):
                        self.mm(p[0:2, :], ccs[:, k, :], w[:, k, :], start=(k == 0), stop=(k == 7))
                    self.tt(mo[:, nb * 512:(nb + 1) * 512], p[0:2, :], bm[:, nb * 512:(nb + 1) * 512], ALU.add)
                self.dma(self.MOD[l], mo[:], w=[("MOD", l)])
        self.P.barrier()

    def bc_row(self, dst, src_row):
        self.dma(dst, src_row.partition_broadcast(128))

    def phase_norm(self, l, which, hT, t0=0, extra=None, after=None):
        io = self.io
        gname = "norm_mix" if which == 1 else "norm_ffn"
        o_sh, o_sc = (0, 1) if which == 1 else (3, 4)
        with contextlib.ExitStack() as es:
            A = [self.sb(es, f"A{j}", [128, D]) for j in range(2)]
            B = [self.sb(es, f"Bv{j}", [128, D]) for j in range(2)]
            g = self.sb(es, "g", [128, D])
            self.bc_row(g[:], io[gname][l:l + 1, :])
            for j in range(2):
                row = 1 - j
                self.bc_row(A[j][:], self.MOD[l, row:row + 1, o_sc * D:(o_sc + 1) * D])
                self.bc_row(B[j][:], self.MOD[l, row:row + 1, o_sh * D:(o_sh + 1) * D])
                self.stt(A[j][:], A[j][:], 1.0, g[:], ALU.add, ALU.mult)
            xt = [self.sb(es, f"xt{j}", [128, D]) for j in range(2)]
            sq = self.sb(es, "sq", [128, D])
            hf = [self.sb(es, f"hf{j}", [128, D]) for j in range(2)]
            hb = self.p_hb
            ss = [self.sb(es, f"ss{j}", [128, 1]) for j in range(2)]
            for i in range(t0, NT):
                j = 0 if i < 2 else 1
                b = i % 2
                self.dma(xt[b][:], self.XR[i * 128:(i + 1) * 128, :], r=[("XR", i)])
                self.act(sq[:], xt[b][:], AF.Square, accum_out=ss[b][:])
                self.ts(ss[b][:], ss[b][:], 1.0 / D, EPS, ALU.mult, ALU.add)
                self.act(ss[b][:], ss[b][:], AF.Sqrt)
                self.recip(ss[b][:], ss[b][:])
                self.stt(hf[b][:], xt[b][:], ss[b][:, 0:1], A[j][:], ALU.mult, ALU.mult)
                self.tt(hf[b][:], hf[b][:], B[j][:], ALU.add)
                self.cp(hb[b][:], hf[b][:], eng="act")
                if hT is not None:
                    pb = self.psb[i % 2]
                    for k in range(8):
                        self.tr(pb[:, k * 128:(k + 1) * 128], hb[b][:, k * 128:(k + 1) * 128], self.identb[:])
                    self.cp(hT[:, :, i * 128:(i + 1) * 128], pb[:].rearrange("p (k n) -> p k n", k=8))
                if extra is not None:
                    extra(i, hf[b], hb[b])
            if after is not None:
                after()
            self.P.barrier()

    def layer(self, l):
        cfg = self.cfg
        ph = cfg.get("phases", "WFHPSGM")
        if "W" in ph:
            self.phase_proj(l)
        if "F" in ph:
            self.phase_fourier(l)
        if "H" in ph:
            self.phase_hyena(l)
        if "P" in ph:
            self.phase_pool(l)
        if "S" in ph:
            self.phase_ssd(l)
        if "G" in ph:
            self.phase_merge(l)
        if "M" in ph:
            self.phase_moe(l)

    def tokblocks(self):
        return [(0, 256)] + [(NCTX + 512 * j, 512) for j in range(8)]

    def phase_proj(self, l):
        io = self.io
        with contextlib.ExitStack() as es:
            hT = self.sb(es, "hT", [128, 8, NTOK], BF16)
            wi = self.sb(es, "wi", [128, 8, INW], BF16)
            wst = [self.sb(es, f"wst{j}", [128, INW]) for j in range(2)]
            for k in range(8):
                self.dma(wst[k % 2][:], io["w_in"][l, k * 128:(k + 1) * 128, :])
                self.cp(wi[:, k, :], wst[k % 2][:], eng=("act" if k % 2 else "pool"))
            self.phase_norm(l, 1, hT)
            ob = [self.sb(es, f"ob{j}", [128, 512]) for j in range(3)]
            fm_cols = [(0, 256), (256, 768), (1792, 1024), (1024, 256)]
            fm_chunks = []
            for c0, n in fm_cols:
                for j in range(n // 128):
                    fm_chunks.append(c0 + j * 128)
            n = 0
            for (t0, tw) in self.tokblocks():
                for oc, c0 in enumerate(fm_chunks):
                    p = self.ps()
                    for k in range(8):
                        self.mm(p[:, 0:tw], wi[:, k, c0:c0 + 128], hT[:, k, t0:t0 + tw], start=(k == 0), stop=(k == 7))
                    o = ob[n % 3]
                    n += 1
                    if n % 2:
                        self.cp(o[:, 0:tw], p[:, 0:tw], eng="act")
                    else:
                        self.cp(o[:, 0:tw], p[:, 0:tw])
                    self.dma(self.FM[oc * 128:(oc + 1) * 128, t0:t0 + tw], o[:, 0:tw], w=[("FM", oc, t0)])
            for i in range(NT):
                p = self.ps()
                for k in range(8):
                    self.mm(p[:, 0:512], hT[:, k, i * 128:(i + 1) * 128], wi[:, k, 1280:1792], start=(k == 0), stop=(k == 7))
                p2 = self.ps()
                for k in range(8):
                    self.mm(p2[:, 0:16], hT[:, k, i * 128:(i + 1) * 128], wi[:, k, 2816:2832], start=(k == 0), stop=(k == 7))
                o = ob[n % 3]
                n += 1
                self.cp(o[:, 0:512], p[:, 0:512], eng="act")
                self.dma(self.TM[i * 128:(i + 1) * 128, 0:512], o[:, 0:512], w=[("TM", i)])
                o = ob[n % 3]
                n += 1
                self.cp(o[:, 0:16], p2[:, 0:16])
                self.dma(self.TM[i * 128:(i + 1) * 128, 512:528], o[:, 0:16], w=[("TMd", i)])
        self.P.barrier()

    def phase_fourier(self, l):
        io = self.io
        with contextlib.ExitStack() as es:
            fT = self.sb(es, "fT", [128, 2, NTOK], BF16)
            for c in range(2):
                self.dma(fT[:, c, :], self.FM[c * 128:(c + 1) * 128, :], eng="pool")
            bd = self.sb(es, "bd", [128, 2, 128], BF16)
            self.dma(bd[:], io["cst_bd64"].rearrange("a p n -> p a n"), eng="pool")
            U = self.sb(es, "U", [128, NT, 512], BF16)
            for i in range(NT):
                p = self.ps()
                for a in range(2):
                    for c in range(2):
                        self.mm(p[:, a * 256 + c * 128:a * 256 + (c + 1) * 128], fT[:, c, i * 128:(i + 1) * 128], bd[:, a, :])
                self.cp(U[:, i, :], p[:], eng=("act" if i % 2 else "dve"))
            ys = [self.sb(es, f"ysf{j}", [128, 512], BF16) for j in range(2)]
            n = 0
            tbc = self.sb(es, "tbc", [128, 2, 2, 256], BF16)
            self.dma(tbc[:].rearrange("p a r c -> p (a r) c"), io["cst_dft4c"].rearrange("a (r p) c -> p (a r) c", p=128))
            sc_c = 1.0 / math.sqrt(NCTX * 64.0)
            for c in range(2):
                p = self.ps()
                cnt = 0
                for tc in range(2):
                    for a in range(2):
                        self.mm(p[:, 0:256], U[:, tc, a * 256 + c * 128:a * 256 + (c + 1) * 128], tbc[:, a, tc, :], start=(cnt == 0), stop=(cnt == 3))
                        cnt += 1
                o = ys[n % 2]
                n += 1
                self.act(o[:, 0:256], p[:, 0:256], AF.Copy, scale=sc_c)
                self.dma(self.YS[c * 128:(c + 1) * 128, 0:256], o[:, 0:256], w=[("YS", c, 0)])
            tbs = [self.sb(es, f"tb{j}", [128, 2, 32, 256], BF16) for j in range(2)]
            sc_l = 1.0 / math.sqrt(NLAT * 64.0)
            for kb in range(16):
                tb = tbs[kb % 2]
                for a in range(2):
                    self.dma(tb[:, a, :, :], io["cst_dft4"][kb, :, a, :, :])
                for c in range(2):
                    p = self.ps()
                    cnt = 0
                    for tc in range(32):
                        for a in range(2):
                            self.mm(p[:, 0:256], U[:, 2 + tc, a * 256 + c * 128:a * 256 + (c + 1) * 128], tb[:, a, tc, :], start=(cnt == 0), stop=(cnt == 63))
                            cnt += 1
                    o = ys[n % 2]
                    n += 1
                    self.act(o[:, 0:256], p[:, 0:256], AF.Copy, scale=sc_l)
                    self.dma(self.YS[c * 128:(c + 1) * 128, NCTX + kb * 256:NCTX + (kb + 1) * 256], o[:, 0:256], w=[("YS", c, kb + 1)])
        self.P.barrier()

    def conv_chunk(self, es_bufs, src_rows, wcol, bcol, out_ap_fn):
        xin, o = es_bufs
        self.dma(xin[:, 1:1 + NCTX], src_rows[:, 0:NCTX])
        self.dma(xin[:, 259:259 + NLAT], src_rows[:, NCTX:NTOK])
        for (b0, n, o0) in ((0, NCTX, 0), (258, NLAT, NCTX)):
            self.ts(o[:, o0:o0 + n], xin[:, b0:b0 + n], wcol[:, 0:1], bcol, ALU.mult, ALU.add)
            self.stt(o[:, o0:o0 + n], xin[:, b0 + 1:b0 + 1 + n], wcol[:, 1:2], o[:, o0:o0 + n], ALU.mult, ALU.add)
            self.stt(o[:, o0:o0 + n], xin[:, b0 + 2:b0 + 2 + n], wcol[:, 2:3], o[:, o0:o0 + n], ALU.mult, ALU.add)
        out_ap_fn(o)

    def phase_hyena(self, l):
        io = self.io
        with contextlib.ExitStack() as es_outer:
            Kh = self.sb(es_outer, "Kh", [128, NT, 512], BF16)
            KN = self.sb(es_outer, "KN", [1, 2, 256])
            rn = self.sb(es_outer, "rn", [128, 2, 2])
            with contextlib.ExitStack() as es:
                w1 = self.sb(es, "w1", [33, 64]); w2 = self.sb(es, "w2", [64, 64]); w3 = self.sb(es, "w3", [64, 512])
                b1 = self.sb(es, "b1", [64, 1]); b2 = self.sb(es, "b2", [64, 1])
                self.dma(w1[:], io["hy_ffn_w1"][l]); self.dma(w2[:], io["hy_ffn_w2"][l]); self.dma(w3[:], io["hy_ffn_w3"][l])
                self.dma(b1[:], io["hy_ffn_b1"][l:l + 1, :].rearrange("o n -> n o"), allow_slow_non_contiguous=True)
                self.dma(b2[:], io["hy_ffn_b2"][l:l + 1, :].rearrange("o n -> n o"), allow_slow_non_contiguous=True)
                d2 = self.sb(es, "d2", [128, 512])
                self.dma(d2[:], io["cst_delta2"])
                tneg = self.sb(es, "tneg", [128, NT])
                self.dma(tneg[:], io["cst_tneg"])
                KSD = self.sb(es, "KSD", [128, NT, 512], BF16)
                ft = self.sb(es, "ft", [33, 512])
                h1 = self.sb(es, "h1", [64, 512]); h2 = self.sb(es, "h2", [64, 512])
                rrf = self.sb(es, "rrf", [64, 512]); rri = self.sb(es, "rri", [64, 512], I32)
                kd = self.sb(es, "kd", [128, 512]); ka = self.sb(es, "ka", [128, 512]); dec = self.sb(es, "dec", [128, 512])
                held = self.hold_ps(3)
                pn = held[0:2]
                pny = held[2]
                for seq, (nseq, tile0, fkey) in enumerate(((NCTX, 0, "cst_featc"), (NLAT, 2, "cst_feat"))):
                    ntile = nseq // 128
                    for jb in range(0, nseq, 512):
                        bw = min(512, nseq - jb)
                        self.dma(ft[:, 0:bw], io[fkey][:, jb:jb + bw])
                        for (src, wgt, bias, dst, kk) in ((ft, w1, b1, h1, 33), (h1, w2, b2, h2, 64)):
                            p = self.ps()
                            self.mm(p[0:64, 0:bw], wgt[0:kk, :], src[0:kk, 0:bw])
                            self.ts(dst[:, 0:bw], p[0:64, 0:bw], bias[:, 0:1], None, ALU.add)
                            self.ts(rrf[:, 0:bw], dst[:, 0:bw], 1.0 / (2 * math.pi), None, ALU.mult)
                            self.cp(rri[:, 0:bw], rrf[:, 0:bw])
                            self.cp(rrf[:, 0:bw], rri[:, 0:bw])
                            self.stt(dst[:, 0:bw], rrf[:, 0:bw], -2 * math.pi, dst[:, 0:bw], ALU.mult, ALU.add)
                            self.act(dst[:, 0:bw], dst[:, 0:bw], AF.Sin)
                        for jt in range(bw // 128):
                            ti = tile0 + (jb // 128) + jt
                            p = self.ps()
                            self.mm(p[:], h2[:, jt * 128:(jt + 1) * 128], w3[:])
                            self.act(dec[:], d2[:], AF.Exp, scale=tneg[:, ti:ti + 1])
                            self.tt(kd[:], p[:], dec[:], ALU.mult)
                            if jb == 0 and jt == 0:
                                self.ts(kd[:, 256:512], kd[:, 256:512], self.misc[:, 1:2], None, ALU.mult)
                            self.act(ka[:], kd[:], AF.Abs)
                            first = (jb == 0 and jt == 0)
                            last = (jb + jt * 128 + 128 == nseq)
                            for c in range(2):
                                for hlf in range(2):
                                    self.mm(pn[c][:, seq:seq + 1], ka[:, hlf * 256 + c * 128:hlf * 256 + (c + 1) * 128], self.tri[:, 3, 0:1],
                                            start=(first and hlf == 0), stop=(last and hlf == 1))
                            self.tt(KSD[:, ti, 0:256], kd[:, 0:256], kd[:, 256:512], ALU.add)
                            self.tt(KSD[:, ti, 256:512], kd[:, 0:256], kd[:, 256:512], ALU.subtract)
                            self.tt(ka[:, 0:256], kd[:, 0:256], kd[:, 256:512], ALU.add)
                            self.mm(pny[0:1, seq * 256:(seq + 1) * 256], self.misc[:, 0:1], ka[:, 0:256], start=first, stop=last)
                    for c in range(2):
                        self.recip(rn[:, c, seq:seq + 1], pn[c][:, seq:seq + 1])
                    self.cp(KN[:, seq, :], pny[0:1, seq * 256:(seq + 1) * 256])
                self.release_ps(held)
                tbc = self.sb(es, "tbc8", [128, 2, 2, 256], BF16)
                self.dma(tbc[:].rearrange("p a r c -> p (a r) c"), io["cst_dft8c"].rearrange("a (r p) c -> p (a r) c", p=128))
                for ftile in range(2):
                    p = self.ps()
                    for a in range(2):
                        for jc in range(2):
                            self.mm(p[:, a * 256:(a + 1) * 256], tbc[:, a, jc, ftile * 128:(ftile + 1) * 128], KSD[:, jc, a * 256:(a + 1) * 256],
                                    start=(jc == 0), stop=(jc == 1))
                    self.cp(Kh[:, ftile, :], p[:], eng="act")
                tbs = [self.sb(es, f"tb8{j}", [128, 2, 32, 256], BF16) for j in range(2)]
                for fb in range(16):
                    tb = tbs[fb % 2]
                    for a in range(2):
                        self.dma(tb[:, a, :, :], io["cst_dft8"][fb, :, a, :, :])
                    for fsub in range(2):
                        ftile = fb * 2 + fsub
                        p = self.ps()
                        for a in range(2):
                            for jc in range(32):
                                self.mm(p[:, a * 256:(a + 1) * 256], tb[:, a, jc, fsub * 128:(fsub + 1) * 128], KSD[:, 2 + jc, a * 256:(a + 1) * 256],
                                        start=(jc == 0), stop=(jc == 31))
                        self.cp(Kh[:, 2 + ftile, :], p[:], eng=("act" if ftile % 2 else "dve"))
            self.P.barrier()
            with contextlib.ExitStack() as es:
                cw = self.sb(es, "cw", [128, 6, 3]); cb = self.sb(es, "cb", [128, 6])
                for k in range(3):
                    self.dma(cw[:, :, k], io["hy_conv_w"][l, k:k + 1, :].rearrange("o (c p) -> (o p) c", p=128), allow_slow_non_contiguous=True)
                self.dma(cb[:], io["hy_conv_b"][l:l + 1, :].rearrange("o (c p) -> (o p) c", p=128), allow_slow_non_contiguous=True)
                xin = self.sb(es, "xin", [128, NTOK + 4])
                self.memset(xin[:], 0.0)
                oo = [self.sb(es, f"cvo{j}", [128, NTOK]) for j in range(3)]
                for c in range(2):
                    self.conv_chunk((xin, oo[0]), self.FM[256 + c * 128:256 + (c + 1) * 128, :], cw[:, c, :], cb[:, c:c + 1],
                                    lambda o, c=c: self.dma(self.X1[c * 128:(c + 1) * 128, :], o[:], w=[("X1", c)]))
                    self.conv_chunk((xin, oo[1]), self.FM[256 + (2 + c) * 128:256 + (3 + c) * 128, :], cw[:, 2 + c, :], cb[:, 2 + c:3 + c], lambda o: None)
                    self.conv_chunk((xin, oo[2]), self.FM[256 + (4 + c) * 128:256 + (5 + c) * 128, :], cw[:, 4 + c, :], cb[:, 4 + c:5 + c], lambda o: None)
                    self.tt(oo[1][:], oo[1][:], oo[2][:], ALU.mult)
                    self.dma(self.VV[c * 128:(c + 1) * 128, :], oo[1][:], w=[("VV", c)])
            self.P.barrier()
            with contextlib.ExitStack() as es:
                Pb = self.sb(es, "Pb", [128, NT, 512], BF16)
                PN = self.sb(es, "PN", [1, 2, 256], BF16)
                with contextlib.ExitStack() as es2:
                    vvb = self.sb(es2, "vvb", [128, 2, NTOK], BF16)
                    for c in range(2):
                        self.dma(vvb[:, c, :], self.VV[c * 128:(c + 1) * 128, :], eng="pool")
                    VVt = self.sb(es2, "VVt", [128, NT, 256], BF16)
                    for i in range(NT):
                        pb = self.psb[i % 2]
                        for c in range(2):
                            self.tr(pb[:, c * 128:(c + 1) * 128], vvb[:, c, i * 128:(i + 1) * 128], self.identb[:])
                        self.cp(VVt[:, i, :], pb[:, 0:256], eng=("act" if i % 2 else "dve"))
                    altb = self.sb(es2, "altb", [128, 1], BF16)
                    self.cp(altb[:], self.misc[:, 0:1])
                    vh = self.sb(es2, "vh", [128, 512]); khf = self.sb(es2, "khf", [128, 512])
                    t1 = self.sb(es2, "t1", [128, 256]); t2 = self.sb(es2, "t2", [128, 256])
                    held2 = self.hold_ps(1)
                    pny = held2[0]
                    vn = self.sb(es2, "vn", [1, 256])

                    def spec_product(ti, p, is_f0):
                        self.cp(vh[:], p[:], eng="act")
                        self.cp(khf[:], Kh[:, ti, :], eng="pool")
                        self.tt(t1[:], vh[:, 0:256], khf[:, 0:256], ALU.mult)
                        self.tt(t2[:], vh[:, 256:512], khf[:, 256:512], ALU.mult)
                        self.tt(t1[:], t1[:], t2[:], ALU.subtract)
                        if is_f0:
                            self.ts(t1[:], t1[:], self.misc[:, 2:3], None, ALU.mult)
                        self.cp(Pb[:, ti, 0:256], t1[:], eng="act")
                        self.tt(t1[:], vh[:, 0:256], khf[:, 256:512], ALU.mult, eng="pool")
                        self.tt(t2[:], vh[:, 256:512], khf[:, 0:256], ALU.mult, eng="pool")
                        self.tt(t1[:], t1[:], t2[:], ALU.add, eng="pool")
                        if is_f0:
                            self.ts(t1[:], t1[:], self.misc[:, 2:3], None, ALU.mult)
                        self.cp(Pb[:, ti, 256:512], t1[:], eng="act")

                    tbc = self.sb(es2, "tbc8b", [128, 2, 2, 256], BF16)
                    self.dma(tbc[:].rearrange("p a r c -> p (a r) c"), io["cst_dft8c"].rearrange("a (r p) c -> p (a r) c", p=128))
                    for ftile in range(2):
                        p = self.ps()
                        for a in range(2):
                            for tc in range(2):
                                self.mm(p[:, a * 256:(a + 1) * 256], tbc[:, a, tc, ftile * 128:(ftile + 1) * 128], VVt[:, tc, :], start=(tc == 0), stop=(tc == 1))
                        spec_product(ftile, p, ftile == 0)
                    for tc in range(2):
                        self.mm(pny[0:1, 0:256], altb[:, 0:1], VVt[:, tc, :], start=(tc == 0), stop=(tc == 1))
                    self.tt(vn[:], pny[0:1, 0:256], KN[:, 0, :], ALU.mult)
                    self.ts(PN[:, 0, :], vn[:], 0.5, None, ALU.mult)
                    tbs = [self.sb(es2, f"tb8b{j}", [128, 2, 32, 256], BF16) for j in range(2)]
                    for fb in range(16):
                        tb = tbs[fb % 2]
                        for a in range(2):
                            self.dma(tb[:, a, :, :], io["cst_dft8"][fb, :, a, :, :])
                        for fsub in range(2):
                            ftile = fb * 2 + fsub
                            p = self.ps()
                            for a in range(2):
                                for tc in range(32):
                                    self.mm(p[:, a * 256:(a + 1) * 256], tb[:, a, tc, fsub * 128:(fsub + 1) * 128], VVt[:, 2 + tc, :], start=(tc == 0), stop=(tc == 31))
                            spec_product(2 + ftile, p, ftile == 0)
                    for tc in range(32):
                        self.mm(pny[0:1, 256:512], altb[:, 0:1], VVt[:, 2 + tc, :], start=(tc == 0), stop=(tc == 31))
                    self.tt(vn[:], pny[0:1, 256:512], KN[:, 1, :], ALU.mult)
                    self.ts(PN[:, 1, :], vn[:], 0.5, None, ALU.mult)
                    self.release_ps(held2)
                self.P.barrier()
                with contextlib.ExitStack() as es2:
                    hb_ = self.sb(es2, "hyb", [128, 2])
                    self.dma(hb_[:], io["hy_bias"][l:l + 1, :].rearrange("o (c p) -> (o p) c", p=128), allow_slow_non_contiguous=True)
                    altrow = self.sb(es2, "altrow", [1, 512], BF16)
                    self.dma(altrow[:], io["cst_altrow"][:, 0:512])
                    yv = [self.sb(es2, f"yv{j}", [128, 512]) for j in range(2)]
                    vvt = [self.sb(es2, f"vvt{j}", [128, 512]) for j in range(2)]
                    x1t = [self.sb(es2, f"x1t{j}", [128, 512]) for j in range(2)]
                    yo = [self.sb(es2, f"yho{j}", [128, 512], BF16) for j in range(2)]
                    scl = self.sb(es2, "scl", [128, 2, 2])
                    self.ts(scl[:, :, 0:1], rn[:, :, 0:1], 2.0 / (2 * NCTX), None, ALU.mult)
                    self.ts(scl[:, :, 1:2], rn[:, :, 1:2], 2.0 / (2 * NLAT), None, ALU.mult)
                    n = 0

                    def finish(c, seq, p, t0, tw):
                        nonlocal n
                        b = n % 2
                        n += 1
                        self.dma(vvt[b][:, 0:tw], self.VV[c * 128:(c + 1) * 128, t0:t0 + tw])
                        self.dma(x1t[b][:, 0:tw], self.X1[c * 128:(c + 1) * 128, t0:t0 + tw])
                        self.act(yv[b][:, 0:tw], p[:, 0:tw], AF.Copy, scale=scl[:, c, seq:seq + 1])
                        self.stt(yv[b][:, 0:tw], vvt[b][:, 0:tw], hb_[:, c:c + 1], yv[b][:, 0:tw], ALU.mult, ALU.add)
                        self.tt(yo[b][:, 0:tw], yv[b][:, 0:tw], x1t[b][:, 0:tw], ALU.mult)
                        self.dma(self.YS[256 + c * 128:256 + (c + 1) * 128, t0:t0 + tw], yo[b][:, 0:tw], w=[("YS", 2 + c, t0)])

                    tbc = self.sb(es2, "tbc8c", [128, 2, 2, 256], BF16)
                    self.dma(tbc[:].rearrange("p a r c -> p (a r) c"), io["cst_dft8c"].rearrange("a (r p) c -> p (a r) c", p=128))
                    for c in range(2):
                        p = self.ps()
                        cnt = 0
                        for fc in range(2):
                            for a in range(2):
                                self.mm(p[:, 0:256], Pb[:, fc, a * 256 + c * 128:a * 256 + (c + 1) * 128], tbc[:, a, fc, :], start=(cnt == 0), stop=False)
                                cnt += 1
                        self.mm(p[:, 0:256], PN[:, 0, c * 128:(c + 1) * 128], altrow[:, 0:256], start=False, stop=True)
                        finish(c, 0, p, 0, 256)
                    tbs = [self.sb(es2, f"tb8c{j}", [128, 2, 32, 256], BF16) for j in range(2)]
                    for tbk in range(16):
                        tb = tbs[tbk % 2]
                        for a in range(2):
                            self.dma(tb[:, a, :, :], io["cst_dft8"][tbk, :, a, :, :])
                        for c in range(2):
                            p = self.ps()
                            cnt = 0
                            for fc in range(32):
                                for a in range(2):
                                    self.mm(p[:, 0:256], Pb[:, 2 + fc, a * 256 + c * 128:a * 256 + (c + 1) * 128], tb[:, a, fc, :], start=(cnt == 0), stop=False)
                                    cnt += 1
                            self.mm(p[:, 0:256], PN[:, 1, c * 128:(c + 1) * 128], altrow[:, 0:256], start=False, stop=True)
                            finish(c, 1, p, NCTX + tbk * 256, 256)
        self.P.barrier()

    def phase_pool(self, l):
        io = self.io
        with contextlib.ExitStack() as es:
            pT = self.sb(es, "pinT", [128, 2, NTOK], BF16)
            for c in range(2):
                self.dma(pT[:, c, :], self.FM[2048 + c * 128:2048 + (c + 1) * 128, :], eng="pool")
            bdwf = self.sb(es, "bdwf", [128, 2, 128])
            self.memset(bdwf[:], 0.0)
            for g in range(4):
                pg, gi = g // 2, g % 2
                self.dma(bdwf[gi * 64:(gi + 1) * 64, pg, gi * 64:(gi + 1) * 64], io["pool_w"][l, g])
            bdw = self.sb(es, "bdw", [128, 2, 128], BF16)
            self.cp(bdw[:], bdwf[:])
            psc = self.sb(es, "psc", [128, 2])
            self.dma(psc[:], io["pool_scale"][l:l + 1, :].rearrange("o (c p) -> (o p) c", p=128), allow_slow_non_contiguous=True)
            pm = self.sb(es, "pm", [128, 4, 128], BF16)
            self.dma(pm[:], io["cst_pool"].rearrange("g s t -> s g t"), eng="pool")
            pmc = self.sb(es, "pmc", [128, 4, 2, 256], BF16)
            self.dma(pmc[:].rearrange("p g r t -> p (g r) t"), io["cst_poolc"].rearrange("g (r p) t -> p (g r) t", p=128), eng="pool")
            Az = [self.sb(es, f"Az{j}", [128, 4, 128], BF16) for j in range(2)]
            for j in range(2):
                self.memset(Az[j][:], 0.0)
            po = [self.sb(es, f"po{j}", [128, 2, 128], BF16) for j in range(2)]

            def make_az(i, az):
                p = self.ps()
                for pg in range(2):
                    self.mm(p[:, pg * 128:(pg + 1) * 128], pT[:, pg, i * 128:(i + 1) * 128], bdw[:, pg, :])
                pv = p[:, 0:256].rearrange("p (pg gi d) -> p pg gi d", pg=2, gi=2)
                azv = az[:].rearrange("p (pg gi) (h d) -> p pg gi h d", pg=2, h=2)
                self.cp(azv[:, :, 0, 0, :], pv[:, :, 0, :])
                self.cp(azv[:, :, 1, 1, :], pv[:, :, 1, :], eng="act")

            make_az(0, Az[0])
            make_az(1, Az[1])
            for tt_ in range(2):
                p = self.ps()
                for pg in range(2):
                    cnt = 0
                    for st in range(2):
                        for gi in range(2):
                            g = pg * 2 + gi
                            self.mm(p[:, pg * 128:(pg + 1) * 128], Az[st][:, g, :], pmc[:, g, st, tt_ * 128:(tt_ + 1) * 128], start=(cnt == 0), stop=(cnt == 3))
                            cnt += 1
                o = po[tt_ % 2]
                for pg in range(2):
                    self.act(o[:, pg, :], p[:, pg * 128:(pg + 1) * 128], AF.Copy, scale=psc[:, pg:pg + 1])
                    self.dma(self.YS[512 + pg * 128:512 + (pg + 1) * 128, tt_ * 128:(tt_ + 1) * 128], o[:, pg, :], w=[("YS", 4 + pg, tt_)])
            self.P.barrier()
            for i in range(2, NT):
                az = Az[i % 2]
                make_az(i, az)
                p = self.ps()
                for pg in range(2):
                    for gi in range(2):
                        g = pg * 2 + gi
                        self.mm(p[:, pg * 128:(pg + 1) * 128], az[:, g, :], pm[:, g, :], start=(gi == 0), stop=(gi == 1))
                o = po[i % 2]
                for pg in range(2):
                    self.act(o[:, pg, :], p[:, pg * 128:(pg + 1) * 128], AF.Copy, scale=psc[:, pg:pg + 1])
                    self.dma(self.YS[512 + pg * 128:512 + (pg + 1) * 128, i * 128:(i + 1) * 128], o[:, pg, :], w=[("YS", 4 + pg, i)])
        self.P.barrier()

    def phase_ssd(self, l):
        io = self.io
        with contextlib.ExitStack() as es:
            cw = self.sb(es, "scw", [128, 8, 3]); cb = self.sb(es, "scb", [128, 8])
            for k in range(3):
                self.dma(cw[:, :, k], io["ssm_conv_w"][l, k:k + 1, :].rearrange("o (c p) -> (o p) c", p=128), allow_slow_non_contiguous=True)
            self.dma(cb[:], io["ssm_conv_b"][l:l + 1, :].rearrange("o (c p) -> (o p) c", p=128), allow_slow_non_contiguous=True)
            xin = self.sb(es, "sxin", [128, NTOK + 4])
            self.memset(xin[:], 0.0)
            oo = [self.sb(es, f"scvo{j}", [128, NTOK]) for j in range(2)]
            ob = [self.sb(es, f"scvb{j}", [128, NTOK], BF16) for j in range(2)]
            for c in range(8):
                def fin(o, c=c):
                    self.act(ob[c % 2][:], o[:], AF.Silu)
                    self.dma(self.XBC[c * 128:(c + 1) * 128, :], ob[c % 2][:], w=[("XBC", c)])
                self.conv_chunk((xin, oo[c % 2]), self.FM[1024 + c * 128:1024 + (c + 1) * 128, :], cw[:, c, :], cb[:, c:c + 1], fin)
        self.P.barrier()
        with contextlib.ExitStack() as es:
            dtb = self.sb(es, "dtb", [128, 16]); abc = self.sb(es, "abc", [128, 16]); dsk = self.sb(es, "dsk", [128, 8])
            self.bc_row(dtb[:], io["ssm_dt_bias"][l:l + 1].rearrange("o a b -> o (a b)"))
            self.bc_row(abc[:], io["ssm_a_log"][l:l + 1].rearrange("o a b -> o (a b)"))
            self.bc_row(dsk[:], io["ssm_d"][l:l + 1, :])
            self.act(abc[:], abc[:], AF.Exp)
            self.ts(abc[:], abc[:], -1.0, None, ALU.mult)

            def v3(ap, a, b):
                return ap.rearrange("p (a b) -> p a b", a=a)

            def bufs(d):
                B = {}
                n = f"d{d}"
                B["H"] = self.sb(es, "Hst" + n, [128, 512]); B["Hb"] = self.sb(es, "Hstb" + n, [128, 512], BF16)
                B["xb"] = [self.sb(es, f"xbct{j}" + n, [128, 8, 128], BF16) for j in range(2)]
                B["tmt"] = [self.sb(es, f"tmt{j}" + n, [128, 16]) for j in range(2)]
                B["xs_t"] = self.sb(es, "xs_t" + n, [128, 512]); B["Bt"] = self.sb(es, "Bt" + n, [128, 256], BF16)
                for nm in ("dt", "dta", "tq", "acs", "tot", "eacs", "tend", "dect"):
                    B[nm] = self.sb(es, nm + n, [128, 8])
                B["dtax"] = self.sb(es, "dtax" + n, [128, 8, 128])
                B["scm"] = self.sb(es, "scm" + n, [128, 2, 128])
                B["seg"] = self.sb(es, "seg" + n, [128, 4, 128]); B["M"] = self.sb(es, "Mm" + n, [128, 8, 128], BF16)
                B["xdt"] = self.sb(es, "xdt" + n, [128, 512], BF16); B["xdtw"] = self.sb(es, "xdtw" + n, [128, 512], BF16)
                B["yt"] = self.sb(es, "yt" + n, [128, 512]); B["yf"] = self.sb(es, "yf" + n, [128, 512])
                return B

            def gen(d, B):
                H, Hb, xs_t, Bt = B["H"], B["Hb"], B["xs_t"], B["Bt"]
                dt, dta, tq, acs, tot, eacs, tend, dect = (B[k] for k in ("dt", "dta", "tq", "acs", "tot", "eacs", "tend", "dect"))
                dtax, scm, seg, M, xdt, xdtw, yt, yf = (B[k] for k in ("dtax", "scm", "seg", "M", "xdt", "xdtw", "yt", "yf"))
                self.memset(H[:], 0.0)
                self.memset(Hb[:], 0.0)
                banks = self.psf[d * 3:(d + 1) * 3]
                cnt_ = [0]

                def lps():
                    t = banks[cnt_[0] % 3]
                    cnt_[0] += 1
                    return t

                order = list(range(NT)) if d == 0 else [1, 0] + list(range(NT - 1, 1, -1))
                trisel = self.tri[:, d, :]
                for n_, i in enumerate(order):
                    b = n_ % 2
                    xb = B["xb"][b]; tmt = B["tmt"][b]
                    self.dma(xb[:], self.XBC.rearrange("(c p) n -> p c n", p=128)[:, :, i * 128:(i + 1) * 128])
                    self.dma(tmt[:], self.TM[i * 128:(i + 1) * 128, 512:528])
                    pb = self.psb[d]
                    for c in range(6):
                        self.tr(pb[:, c * 128:(c + 1) * 128], xb[:, c, :], self.identb[:])
                    self.cp(xs_t[:], pb[:, 0:512], eng="act")
                    self.cp(Bt[:], pb[:, 512:768])
                    yield
                    self.tt(dt[:], tmt[:, d * 8:8 + d * 8], dtb[:, d * 8:(d + 1) * 8], ALU.add)
                    self.act(tq[:], dt[:], AF.Abs)
                    yield
                    self.act(tq[:], tq[:], AF.Exp, scale=-1.0)
                    yield
                    self.act(tq[:], tq[:], AF.Ln, bias=1.0, scale=1.0)
                    yield
                    self.stt(dt[:], dt[:], 0.0, tq[:], ALU.max, ALU.add)
                    yield
                    self.tt(dta[:], dt[:], abc[:, d * 8:(d + 1) * 8], ALU.mult)
                    yield
                    self.cp(dtax[:], dta[:].unsqueeze(2).to_broadcast([128, 8, 128]))
                    p = lps()
                    self.mm(p[:, 0:8], trisel, dta[:])
                    self.mm(p[:, 8:16], self.tri[:, 3, :], dta[:])
                    yield
                    self.cp(acs[:], p[:, 0:8])
                    self.cp(tot[:], p[:, 8:16], eng="act")
                    yield
                    self.act(eacs[:], acs[:], AF.Exp)
                    self.tt(tend[:], tot[:], acs[:], ALU.subtract)
                    yield
                    self.act(tend[:], tend[:], AF.Exp)
                    self.act(dect[:], tot[:], AF.Exp)
                    p = lps()
                    for g in range(2):
                        self.mm(p[:, g * 128:(g + 1) * 128], xb[:, 4 + g, :], xb[:, 6 + g, :])
                    yield
                    self.tt(scm[:], v3(p[:, 0:256], 2, 128), trisel.unsqueeze(1).to_broadcast([128, 2, 128]), ALU.mult)
                    yield
                    for g in range(2):
                        p = lps()
                        for r in range(4):
                            self.mm(p[:, r * 128:(r + 1) * 128], dtax[:, g * 4 + r, :], trisel)
                        yield
                        self.tt(seg[:], v3(p[:], 4, 128), acs[:, g * 4:(g + 1) * 4].unsqueeze(2).to_broadcast([128, 4, 128]), ALU.subtract)
                        yield
                        self.ts(seg[:], seg[:], 0.0, None, ALU.min)
                        yield
                        self.act(seg[:], seg[:], AF.Exp)
                        yield
                        self.tt(M[:, g * 4:(g + 1) * 4, :], seg[:], scm[:, g, :].unsqueeze(1).to_broadcast([128, 4, 128]), ALU.mult)
                        yield
                    self.tt(v3(xdt[:], 8, 64), v3(xs_t[:], 8, 64), dt[:].unsqueeze(2).to_broadcast([128, 8, 64]), ALU.mult)
                    self.tt(tq[:], dt[:], tend[:], ALU.mult)
                    yield
                    self.tt(v3(xdtw[:], 8, 64), v3(xs_t[:], 8, 64), tq[:].unsqueeze(2).to_broadcast([128, 8, 64]), ALU.mult)
                    pd = lps()
                    for hh in range(8):
                        self.mm(pd[:, hh * 64:(hh + 1) * 64], M[:, hh, :], xdt[:, hh * 64:(hh + 1) * 64])
                    po_ = lps()
                    for g in range(2):
                        self.mm(po_[:, g * 256:(g + 1) * 256], xb[:, 6 + g, :], Hb[:, g * 256:(g + 1) * 256])
                    yield
                    self.tt(v3(yt[:], 8, 64), v3(po_[:], 8, 64), eacs[:].unsqueeze(2).to_broadcast([128, 8, 64]), ALU.mult)
                    pst = lps()
                    for g in range(2):
                        self.mm(pst[:, g * 256:(g + 1) * 256], Bt[:, g * 128:(g + 1) * 128], xdtw[:, g * 256:(g + 1) * 256])
                    yield
                    self.tt(yt[:], yt[:], pd[:], ALU.add)
                    self.tt(v3(H[:], 8, 64), v3(H[:], 8, 64), dect[:].unsqueeze(2).to_broadcast([128, 8, 64]), ALU.mult)
                    yield
                    self.tt(H[:], H[:], pst[:], ALU.add)
                    yield
                    self.cp(Hb[:], H[:], eng="act")
                    if d == 0:
                        self.tt(v3(yf[:], 8, 64), v3(xs_t[:], 8, 64), dsk[:].unsqueeze(2).to_broadcast([128, 8, 64]), ALU.mult)
                        yield
                        self.tt(yf[:], yf[:], yt[:], ALU.add)
                        self.dma(self.YF[i * 128:(i + 1) * 128, :], yf[:], w=[("YF", i)])
                    else:
                        self.cp(yf[:], yt[:], eng="pool")
                        self.dma(self.YB[i * 128:(i + 1) * 128, :], yf[:], w=[("YB", i)])
                    yield

            gens = [gen(0, bufs(0)), gen(1, bufs(1))]
            alive = [True, True]
            while any(alive):
                for gi in range(2):
                    if alive[gi]:
                        try:
                            next(gens[gi])
                        except StopIteration:
                            alive[gi] = False
        self.P.barrier()
        with contextlib.ExitStack() as es:
            snw = self.sb(es, "snw", [128, 512])
            self.bc_row(snw[:], io["ssm_norm"][l:l + 1, :])
            yfs = [self.sb(es, f"cyf{j}", [128, 512]) for j in range(2)]
            ybs = [self.sb(es, f"cyb{j}", [128, 512]) for j in range(2)]
            zts = [self.sb(es, f"czt{j}", [128, 512]) for j in range(2)]
            ysq = [self.sb(es, f"cysq{j}", [128, 512]) for j in range(2)]
            ybf = [self.sb(es, f"cybf{j}", [128, 512], BF16) for j in range(2)]
            ssq = [self.sb(es, f"cssq{j}", [128, 1]) for j in range(2)]
            yso = [self.sb(es, f"yso{j}", [128, 4, 128], BF16) for j in range(2)]

            def cgen(i):
                b = i % 2
                yf, yb_, zt, sq, ybb, ss, o = yfs[b], ybs[b], zts[b], ysq[b], ybf[b], ssq[b], yso[b]
                self.dma(yf[:], self.YF[i * 128:(i + 1) * 128, :])
                self.dma(yb_[:], self.YB[i * 128:(i + 1) * 128, :])
                self.dma(zt[:], self.TM[i * 128:(i + 1) * 128, 0:512])
                yield
                self.tt(yf[:], yf[:], yb_[:], ALU.add)
                self.act(zt[:], zt[:], AF.Silu)
                yield
                self.tt(yf[:], yf[:], zt[:], ALU.mult)
                yield
                self.act(sq[:], yf[:], AF.Square, accum_out=ss[:])
                yield
                self.ts(ss[:], ss[:], 1.0 / 512, EPS, ALU.mult, ALU.add)
                yield
                self.act(ss[:], ss[:], AF.Sqrt)
                yield
                self.recip(ss[:], ss[:])
                yield
                self.stt(ybb[:], yf[:], ss[:, 0:1], snw[:], ALU.mult, ALU.mult)
                yield
                pb2 = self.psb[b]
                for c in range(4):
                    self.tr(pb2[:, c * 128:(c + 1) * 128], ybb[:, c * 128:(c + 1) * 128], self.identb[:])
                yield
                self.cp(o[:], pb2[:, 0:512].rearrange("p (c n) -> p c n", c=4), eng="act")
                self.dma(self.YS.rearrange("(c p) n -> p c n", p=128)[:, 6:10, i * 128:(i + 1) * 128], o[:], w=[("YS", 6, i)])
                yield

            for i0 in range(0, NT, 2):
                g2 = [cgen(i0), cgen(i0 + 1)]
                alive = [True, True]
                while any(alive):
                    for gi in range(2):
                        if alive[gi]:
                            try:
                                next(g2[gi])
                            except StopIteration:
                                alive[gi] = False
        self.P.barrier()

    def phase_merge(self, l):
        io = self.io
        with contextlib.ExitStack() as es:
            hT = self.sb(es, "hT2", [128, 8, NTOK], BF16)
            self.phase_norm(l, 1, hT)
            wg = self.sb(es, "wg", [128, 4, 8, 512], BF16)
            wbr = self.sb(es, "wbr", [128, 10, 512], BF16)
            gst = [self.sb(es, f"gst{j}", [128, 4, 512]) for j in range(2)]
            ysb = [self.sb(es, f"ysb{j}", [128, 10, 512], BF16) for j in range(2)]
            gt = [self.sb(es, f"gt{j}", [128, 512]) for j in range(2)]
            acc = self.sb(es, "macc", [128, 512]); tmp = self.sb(es, "mtmp", [128, 512])
            mo = [self.sb(es, f"mo{j}", [128, 512], BF16) for j in range(2)]
            br_k = [(0, 2), (2, 4), (4, 6), (6, 10)]
            ng = 0
            nblk = 0
            for half in range(2):
                hc = slice(half * 512, (half + 1) * 512)
                for k in range(4):
                    for q in range(2):
                        st = gst[ng % 2]
                        ng += 1
                        self.dma(st[:], io["w_gate"][l, k, q * 512:(q + 1) * 512, hc].rearrange("(c p) n -> p c n", p=128))
                        self.cp(wg[:, k, q * 4:(q + 1) * 4, :], st[:], eng=("act" if ng % 2 else "pool"))
                for (q0, qn) in ((0, 4), (4, 4), (8, 2)):
                    st = gst[ng % 2]
                    ng += 1
                    self.dma(st[:, 0:qn, :], io["w_branch"][l, q0 * 128:(q0 + qn) * 128, hc].rearrange("(c p) n -> p c n", p=128))
                    self.cp(wbr[:, q0:q0 + qn, :], st[:, 0:qn, :], eng=("act" if ng % 2 else "pool"))
                for bi, (t0, tw) in enumerate(self.tokblocks()):
                    yb_ = ysb[nblk % 2]
                    nblk += 1
                    self.dma(yb_[:, :, 0:tw], self.YS.rearrange("(c p) n -> p c n", p=128)[:, :, t0:t0 + tw])
                    for o4 in range(4):
                        oc = half * 4 + o4
                        oc_s = slice(o4 * 128, (o4 + 1) * 128)
                        for k in range(4):
                            pg_ = self.ps()
                            for c in range(8):
                                self.mm(pg_[:, 0:tw], wg[:, k, c, oc_s], hT[:, c, t0:t0 + tw], start=(c == 0), stop=(c == 7))
                            g_ = gt[k % 2]
                            self.act(g_[:, 0:tw], pg_[:, 0:tw], AF.Sigmoid)
                            pb_ = self.ps()
                            c0, c1 = br_k[k]
                            for c in range(c0, c1):
                                self.mm(pb_[:, 0:tw], wbr[:, c, oc_s], yb_[:, c, 0:tw], start=(c == c0), stop=(c == c1 - 1))
                            if k == 0:
                                self.tt(acc[:, 0:tw], pb_[:, 0:tw], g_[:, 0:tw], ALU.mult)
                            else:
                                self.tt(tmp[:, 0:tw], pb_[:, 0:tw], g_[:, 0:tw], ALU.mult)
                                self.tt(acc[:, 0:tw], acc[:, 0:tw], tmp[:, 0:tw], ALU.add, eng="pool")
                        o = mo[o4 % 2]
                        self.cp(o[:, 0:tw], acc[:, 0:tw], eng="act")
                        self.dma(self.MG[oc * 128:(oc + 1) * 128, t0:t0 + tw], o[:, 0:tw], w=[("MG", oc, bi)])
        self.P.barrier()
        with contextlib.ExitStack() as es:
            wo = self.sb(es, "wo", [128, 8, D], BF16)
            wost = [self.sb(es, f"wost{j}", [128, D]) for j in range(2)]
            for k in range(8):
                self.dma(wost[k % 2][:], io["w_out"][l, k * 128:(k + 1) * 128, :])
                self.cp(wo[:, k, :], wost[k % 2][:], eng=("act" if k % 2 else "dve"))
            G = [self.sb(es, f"G{j}", [128, D]) for j in range(2)]
            for j in range(2):
                self.bc_row(G[j][:], self.MOD[l, 1 - j:2 - j, 2 * D:3 * D])
            mt = [self.sb(es, f"mt{j}", [128, 8, 128], BF16) for j in range(2)]
            xt = [self.sb(es, f"xo{j}", [128, D]) for j in range(2)]
            yy = self.sb(es, "yy", [128, D])
            for i in range(NT):
                if l == DEPTH - 1 and i < 2:
                    continue
                b = i % 2
                j = 0 if i < 2 else 1
                self.dma(mt[b][:], self.MG.rearrange("(c p) n -> p c n", p=128)[:, :, i * 128:(i + 1) * 128])
                self.dma(xt[b][:], self.XR[i * 128:(i + 1) * 128, :], r=[("XR", i)])
                for hf in range(2):
                    p = self.ps()
                    for k in range(8):
                        self.mm(p[:], mt[b][:, k, :], wo[:, k, hf * 512:(hf + 1) * 512], start=(k == 0), stop=(k == 7))
                    self.tt(yy[:, hf * 512:(hf + 1) * 512], p[:], G[j][:, hf * 512:(hf + 1) * 512], ALU.mult)
                self.tt(xt[b][:], xt[b][:], yy[:], ALU.add, eng="pool")
                self.dma(self.XR[i * 128:(i + 1) * 128, :], xt[b][:], w=[("XR", i)])
        self.P.barrier()

    def phase_moe(self, l):
        io = self.io
        t0 = 2 if l == DEPTH - 1 else 0
        with contextlib.ExitStack() as es_outer:
            SL = self.sb(es_outer, "SL", [128, NT, 4], I32)
            WS = self.sb(es_outer, "WS", [128, NT, 4])
            CNT = self.sb(es_outer, "CNT", [128, NE], I32)
            with contextlib.ExitStack() as es:
                rw = self.sb(es, "rw", [128, 8, NE])
                self.dma(rw[:], io["router_w"][l].rearrange("(k p) n -> p k n", p=128))
                rb = self.sb(es, "rb", [128, NE])
                self.bc_row(rb[:], io["router_b"][l:l + 1, :])
                ebase = self.sb(es, "ebase", [128, NE])
                self.dma(ebase[:], io["cst_ebase"])
                carry = self.sb(es, "carry", [128, NE])
                self.memset(carry[:], 0.0)
                hTf = self.sb(es, "hTf", [128, 8, 128])
                lg = self.sb(es, "lg", [128, NE]); mx = self.sb(es, "mx", [128, 8]); msk = self.sb(es, "msk", [128, NE])
                mskb = self.sb(es, "mskb", [128, NE], BF16)
                ex = self.sb(es, "ex", [128, NE]); den = self.sb(es, "den", [128, 1]); nmx = self.sb(es, "nmx", [128, 1])
                pos = self.sb(es, "pos", [128, NE]); okm = self.sb(es, "okm", [128, NE]); sv = self.sb(es, "sv", [128, NE])
                mx2 = self.sb(es, "mx2", [128, 8]); slf = self.sb(es, "slf", [128, 4]); junk = self.sb(es, "junk", [128, NE])

                sidx = self.p_idx

                def route(i, hf, hb):
                    for hh in range(2):
                        p = self.ps()
                        for k in range(4):
                            self.tr(p[:, k * 128:(k + 1) * 128], hf[:, (hh * 4 + k) * 128:(hh * 4 + k + 1) * 128], self.identf[:])
                        self.cp(hTf[:, hh * 4:(hh + 1) * 4, :], p[:].rearrange("p (k n) -> p k n", k=4), eng=("act" if hh else "dve"))
                    p = self.ps()
                    for k in range(8):
                        self.mm(p[:, 0:NE], hTf[:, k, :], rw[:, k, :], start=(k == 0), stop=(k == 7))
                    self.tt(lg[:], p[:, 0:NE], rb[:], ALU.add)
                    self.vmax(mx[:], lg[:])
                    self.ts(msk[:], lg[:], mx[:, 3:4], None, ALU.is_ge)
                    self.ts(nmx[:], mx[:, 0:1], -1.0, None, ALU.mult)
                    self.act(ex[:], lg[:], AF.Exp, bias=nmx[:, 0:1], scale=1.0)
                    self.tt(ex[:], ex[:], msk[:], ALU.mult)
                    self.rsum(den[:], ex[:])
                    self.recip(den[:], den[:])
                    self.ts(ex[:], ex[:], den[:, 0:1], None, ALU.mult)
                    self.cp(mskb[:], msk[:])
                    p = self.ps()
                    self.mm(p[:, 0:NE], self.trib[:, 2, :], mskb[:])
                    self.mm(p[:, NE:2 * NE], self.trib[:, 3, :], mskb[:])
                    self.tt(pos[:], p[:, 0:NE], carry[:], ALU.add)
                    self.tt(carry[:], carry[:], p[:, NE:2 * NE], ALU.add)
                    self.ts(okm[:], pos[:], float(CAP), None, ALU.is_lt)
                    self.tt(ex[:], ex[:], okm[:], ALU.mult)
                    self.ts(pos[:], pos[:], float(CAP), None, ALU.min)
                    self.tt(pos[:], pos[:], ebase[:], ALU.add)
                    self.ts(sv[:], pos[:], -1.0, BIG, ALU.mult, ALU.add)
                    self.tt(sv[:], sv[:], msk[:], ALU.mult)
                    self.vmax(mx2[:], sv[:])
                    self.ts(slf[:], mx2[:, 0:4], -1.0, BIG, ALU.mult, ALU.add)
                    self.cp(SL[:, i, :], slf[:])
                    for k in range(4):
                        self.stt(junk[:], sv[:], mx2[:, k:k + 1], ex[:], ALU.is_equal, ALU.mult, accum_out=WS[:, i, k:k + 1])
                    for k in range(4):
                        idxt = sidx[(i % 2) * 4 + k]
                        self.cp(idxt[:], SL[:, i, k:k + 1])
                        idx = idxt[:, :]
                        rr, ww = self._rw([], [hb[:], idx], (), ())
                        self.P.op("pool", lambda e, idx=idx, hb=hb: e.indirect_dma_start(
                            out=self.XG[:, :], out_offset=bass.IndirectOffsetOnAxis(ap=idx, axis=0),
                            in_=hb[:], in_offset=None),
                            rr, [("XGs", i, k)], dma=True)

                self.phase_norm(l, 2, None, t0=t0, extra=route, after=lambda: self.cp(CNT[:], carry[:]))
            self.P.barrier()
            with contextlib.ExitStack() as es:
                wu = [self.sb(es, f"wu{j}", [128, 8, 2048], BF16) for j in range(2)]
                wd = [self.sb(es, f"wd{j}", [128, 8, D], BF16) for j in range(2)]
                bu = [self.sb(es, f"bu{j}", [128, 16]) for j in range(2)]
                bd_ = [self.sb(es, f"bdn{j}", [128, D]) for j in range(2)]
                xg = [self.sb(es, f"xg{j}", [128, D], BF16) for j in range(2)]
                xgT = [self.sb(es, f"xgT{j}", [128, 8, 512], BF16) for j in range(2)]
                actT = self.sb(es, "actT", [128, 8, 512], BF16)
                gq = [self.sb(es, f"gq{j}", [128, 512], BF16) for j in range(3)]
                sg = [self.sb(es, f"sg{j}", [128, 512], BF16) for j in range(3)]
                lq = [self.sb(es, f"lq{j}", [128, 512], BF16) for j in range(3)]
                yo = [self.sb(es, f"yo{j}", [128, D]) for j in range(2)]
                blocks = []
                c0 = 0
                while c0 < CAP:
                    w_ = min(512, CAP - c0)
                    blocks.append((c0, w_))
                    c0 += w_
                nb = 0
                ny = 0
                stg = [self.sb(es, f"stg{j}", [128, 2048]) for j in range(3)]
                nst = [0]

                def loader(e):
                    eb_ = e % 2
                    self.dma(bu[eb_][:], io["exp_b_up"][l, e:e + 1, :].rearrange("o (c p) -> (o p) c", p=128), allow_slow_non_contiguous=True)
                    self.bc_row(bd_[eb_][:], io["exp_b_down"][l, e:e + 1, :])
                    yield
                    for k in range(8):
                        st = stg[nst[0] % 3]
                        nst[0] += 1
                        self.dma(st[:], io["exp_w_up"][l, e, k * 128:(k + 1) * 128, :])
                        self.cp(wu[eb_][:, k, :], st[:], eng="act")
                        yield
                    for k in range(8):
                        st = stg[nst[0] % 3]
                        nst[0] += 1
                        self.dma(st[:, 0:D], io["exp_w_down"][l, e, k * 128:(k + 1) * 128, :])
                        self.cp(wd[eb_][:, k, :], st[:, 0:D], eng="act")
                        yield

                for _ in loader(0):
                    pass
                for e in range(NE):
                    eb = e % 2
                    nxt = loader(e + 1) if e + 1 < NE else iter(())
                    for (c0, w_) in blocks:
                        xT = xgT[nb % 2]
                        nb += 1
                        cnt_ap = CNT[0:1, e:e + 1]
                        for s in range(w_ // 128):
                            r0 = e * ESTR + c0 + s * 128
                            xt_ = xg[s % 2]
                            self.dma(xt_[:], self.XG[r0:r0 + 128, :], r=[("XGl", r0)])
                            pb = self.psb[s % 2]
                            self.P.set_pe_cond((cnt_ap, c0 + s * 128 + 1))
                            for k in range(8):
                                self.tr(pb[:, k * 128:(k + 1) * 128], xt_[:, k * 128:(k + 1) * 128], self.identb[:])
                            self.P.set_pe_cond(None)
                            self.cp(xT[:, :, s * 128:(s + 1) * 128], pb[:].rearrange("p (k n) -> p k n", k=8), eng=("act" if s % 2 else "dve"))
                        def stage_a(fc):
                            self.P.set_pe_cond((cnt_ap, c0 + 1))
                            pg_ = self.ps()
                            for k in range(8):
                                self.mm(pg_[:, 0:w_], wu[eb][:, k, fc * 128:(fc + 1) * 128], xT[:, k, 0:w_], start=(k == 0), stop=(k == 7))
                            pl_ = self.ps()
                            for k in range(8):
                                self.mm(pl_[:, 0:w_], wu[eb][:, k, 1024 + fc * 128:1024 + (fc + 1) * 128], xT[:, k, 0:w_], start=(k == 0), stop=(k == 7))
                            self.P.set_pe_cond(None)
                            g_ = gq[fc % 3]; s_ = sg[fc % 3]; l_ = lq[fc % 3]
                            self.ts(g_[:, 0:w_], pg_[:, 0:w_], bu[eb][:, fc:fc + 1], 7.0, ALU.add, ALU.min)
                            self.act(s_[:, 0:w_], g_[:, 0:w_], AF.Sigmoid, scale=1.702)
                            self.ts(l_[:, 0:w_], pl_[:, 0:w_], bu[eb][:, 8 + fc:9 + fc], 7.0, ALU.add, ALU.min)
                            next(nxt, None)

                        def stage_b(fc):
                            g_ = gq[fc % 3]; s_ = sg[fc % 3]; l_ = lq[fc % 3]
                            self.ts(l_[:, 0:w_], l_[:, 0:w_], -7.0, 1.0, ALU.max, ALU.add)
                            self.tt(g_[:, 0:w_], g_[:, 0:w_], s_[:, 0:w_], ALU.mult)
                            self.tt(actT[:, fc, 0:w_], g_[:, 0:w_], l_[:, 0:w_], ALU.mult)

                        stage_a(0)
                        for fc in range(1, 8):
                            stage_a(fc)
                            stage_b(fc - 1)
                        stage_b(7)
                        for s in range(w_ // 128):
                            r0 = e * ESTR + c0 + s * 128
                            o = yo[ny % 2]
                            ny += 1
                            cond_s = (cnt_ap, c0 + s * 128 + 1)
                            for hf in range(2):
                                p = self.ps()
                                self.P.set_pe_cond(cond_s)
                                for fc in range(8):
                                    self.mm(p[:], actT[:, fc, s * 128:(s + 1) * 128], wd[eb][:, fc, hf * 512:(hf + 1) * 512], start=(fc == 0), stop=(fc == 7))
                                self.P.set_pe_cond(None)
                                self.tt(o[:, hf * 512:(hf + 1) * 512], p[:], bd_[eb][:, hf * 512:(hf + 1) * 512], ALU.add)
                            self.dma(self.YG[r0:r0 + 128, :], o[:], w=[("YGs", r0)])
                    for _ in nxt:
                        pass
            self.P.barrier()
            with contextlib.ExitStack() as es:
                G2 = [self.sb(es, f"G2{j}", [128, D]) for j in range(2)]
                for j in range(2):
                    self.bc_row(G2[j][:], self.MOD[l, 1 - j:2 - j, 5 * D:6 * D])
                gks = [[self.sb(es, f"gk{j}_{k}", [128, D]) for k in range(4)] for j in range(2)]
                xt = [self.sb(es, f"xc{j}", [128, D]) for j in range(2)]
                accs = [self.sb(es, f"cacc{j}", [128, D]) for j in range(2)]
                cidx = self.p_idx

                def cgen(i):
                    b = i % 2
                    j = 0 if i < 2 else 1
                    gk = gks[b]
                    acc = accs[b]
                    self.dma(xt[b][:], self.XR[i * 128:(i + 1) * 128, :], r=[("XR", i)])
                    for k in range(4):
                        idxt = cidx[b * 4 + k]
                        self.cp(idxt[:], SL[:, i, k:k + 1])
                        idx = idxt[:, :]
                        gk_ = gk[k]
                        rr, ww = self._rw([gk_[:]], [idx], (), ())
                        self.P.op("pool", lambda e, idx=idx, gk_=gk_: e.indirect_dma_start(
                            out=gk_[:], out_offset=None, in_=self.YG[:, :],
                            in_offset=bass.IndirectOffsetOnAxis(ap=idx, axis=0)), rr, ww, dma=True)
                    yield
                    self.act(acc[:], gk[0][:], AF.Copy, scale=WS[:, i, 0:1])
                    yield
                    for k in range(1, 4):
                        self.stt(acc[:], gk[k][:], WS[:, i, k:k + 1], acc[:], ALU.mult, ALU.add)
                        yield
                    self.tt(acc[:], acc[:], G2[j][:], ALU.mult)
                    yield
                    self.tt(xt[b][:], xt[b][:], acc[:], ALU.add, eng="pool")
                    self.dma(self.XR[i * 128:(i + 1) * 128, :], xt[b][:], w=[("XR", i)])
                    yield

                for i0 in range(t0, NT, 2):
                    g2 = [cgen(i0), cgen(i0 + 1)]
                    alive = [True, True]
                    while any(alive):
                        for gi in range(2):
                            if alive[gi]:
                                try:
                                    next(g2[gi])
                                except StopIteration:
                                    alive[gi] = False
        self.P.barrier()

    def phase_final(self):
        io = self.io
        with contextlib.ExitStack() as es:
            g = self.sb(es, "gfin", [128, D])
            self.bc_row(g[:], io["norm_final"])
            xt = [self.sb(es, f"xf{j}", [128, D]) for j in range(2)]
            sq = self.sb(es, "sqf", [128, D])
            ss = [self.sb(es, f"ssf{j}", [128, 1]) for j in range(2)]
            o = [self.sb(es, f"of{j}", [128, D]) for j in range(2)]
            for i in range(2, NT):
                b = i % 2
                self.dma(xt[b][:], self.XR[i * 128:(i + 1) * 128, :], r=[("XR", i)])
                self.act(sq[:], xt[b][:], AF.Square, accum_out=ss[b][:])
                self.ts(ss[b][:], ss[b][:], 1.0 / D, EPS, ALU.mult, ALU.add)
                self.act(ss[b][:], ss[b][:], AF.Sqrt)
                self.recip(ss[b][:], ss[b][:])
                self.stt(o[b][:], xt[b][:], ss[b][:, 0:1], g[:], ALU.mult, ALU.mult)
                self.dma(io["out"][(i - 2) * 128:(i - 1) * 128, :], o[b][:], w=[("out", i)])


W_NAMES = ['w_mod', 'b_mod', 'norm_mix', 'norm_ffn', 'w_in', 'hy_conv_w', 'hy_conv_b', 'hy_ffn_w1', 'hy_ffn_b1',
           'hy_ffn_w2', 'hy_ffn_b2', 'hy_ffn_w3', 'hy_bias', 'pool_w', 'pool_scale', 'ssm_conv_w', 'ssm_conv_b',
           'ssm_dt_bias', 'ssm_a_log', 'ssm_d', 'ssm_norm', 'w_branch', 'w_gate', 'w_out', 'router_w', 'router_b',
           'exp_w_up', 'exp_b_up', 'exp_w_down', 'exp_b_down']

_CONST_CACHE = {}


def make_constants():
    if _CONST_CACHE:
        return _CONST_CACHE
    bf = ml_dtypes.bfloat16
    c = {}
    c["cst_ident"] = np.eye(128, dtype=np.float32)
    t = np.arange(128)
    tri = np.zeros((4, 128, 128), np.float32)
    tri[0] = (t[:, None] <= t[None, :])
    tri[1] = (t[:, None] >= t[None, :])
    tri[2] = (t[:, None] < t[None, :])
    tri[3] = 1.0
    c["cst_tri"] = tri
    misc = np.ones((128, 8), np.float32)
    misc[:, 0] = (-1.0) ** t
    misc[0, 1] = 0.0
    misc[0, 2] = 0.5
    c["cst_misc"] = misc
    c["cst_altrow"] = ((-1.0) ** np.arange(4096)).astype(np.float32).reshape(1, 4096).astype(bf)
    m = np.arange(64)
    a64 = 2 * np.pi * np.outer(m, m) / 64.0
    bd = np.zeros((2, 128, 128), np.float32)
    for g in range(2):
        bd[0, g * 64:(g + 1) * 64, g * 64:(g + 1) * 64] = np.cos(a64)
        bd[1, g * 64:(g + 1) * 64, g * 64:(g + 1) * 64] = np.sin(a64)
    c["cst_bd64"] = bd

    def dft(n, period):
        k = np.arange(n, dtype=np.int64)
        ph = (np.outer(k, k) % period).astype(np.float64) * (2 * np.pi / period)
        out = np.empty((2, n, n), bf)
        out[0] = np.cos(ph).astype(np.float32).astype(bf)
        out[1] = (-np.sin(ph)).astype(np.float32).astype(bf)
        return out

    def tiled(t):
        return np.ascontiguousarray(t.reshape(2, 32, 128, 16, 256).transpose(3, 2, 0, 1, 4))

    c["cst_dft4"] = tiled(dft(NLAT, NLAT))
    c["cst_dft4c"] = dft(NCTX, NCTX)
    c["cst_dft8"] = tiled(dft(NLAT, 2 * NLAT))
    c["cst_dft8c"] = dft(NCTX, 2 * NCTX)

    def poolmat(row_len, nrows):
        n = row_len * nrows
        out = np.zeros((4, n, n), np.float32)
        pos = np.arange(row_len)
        for gi, win in enumerate((2, 4, 8, 16)):
            lo = np.clip(pos - win // 2, 0, row_len)
            hi = np.clip(pos + win // 2, 0, row_len)
            blk = np.zeros((row_len, row_len), np.float32)
            for tt in range(row_len):
                blk[tt, lo[tt]:hi[tt]] = 1.0 / float(hi[tt] - lo[tt])
            blk -= np.eye(row_len, dtype=np.float32)
            for r in range(nrows):
                out[gi, r * row_len:(r + 1) * row_len, r * row_len:(r + 1) * row_len] = blk.T
        return out

    c["cst_pool"] = poolmat(64, 2)
    c["cst_poolc"] = poolmat(256, 1)

    def feats(n):
        pos = np.arange(n, dtype=np.float32)
        tt = pos / np.float32(n - 1)
        ang = (np.float32(2.0 * math.pi) * pos / np.float32(n)).astype(np.float32)
        freqs = np.linspace(1e-4, 15, 16, dtype=np.float32)
        f = np.concatenate([tt[:, None], np.cos(ang[:, None] * freqs), -np.sin(ang[:, None] * freqs)], axis=-1).astype(np.float32)
        return np.ascontiguousarray(f.T), tt

    fl, tl = feats(NLAT)
    fc, tc = feats(NCTX)
    c["cst_feat"] = fl
    c["cst_featc"] = fc
    tneg = np.zeros((128, NT), np.float32)
    tneg[:, 0:2] = -tc.reshape(2, 128).T
    tneg[:, 2:] = -tl.reshape(32, 128).T
    c["cst_tneg"] = tneg
    deltas = np.linspace(HY_MIN, HY_MAX, 256, dtype=np.float32)
    c["cst_delta2"] = np.ascontiguousarray(np.broadcast_to(np.concatenate([deltas, deltas])[None, :], (128, 512))).astype(np.float32)
    c["cst_ebase"] = np.ascontiguousarray(np.broadcast_to((np.arange(NE) * ESTR).astype(np.float32)[None, :], (128, NE)))
    _CONST_CACHE.update(c)
    return c


def build_program(cfg):
    nc = bass.Bass("TRN2", target_bir_lowering=False)
    io = {}

    def inp(name, shape, dt=F32):
        io[name] = nc.dram_tensor(name, list(shape), dt, kind="ExternalInput").ap()

    inp("x", [NLAT, D]); inp("ctx", [NCTX, D]); inp("c", [1, D]); inp("c_ctx", [1, D])
    shapes = cfg["shapes"]
    for n in W_NAMES:
        inp(n, shapes[n])
    inp("norm_final", [1, D])
    consts = make_constants()
    for n, a in consts.items():
        inp(n, a.shape, BF16 if a.dtype == ml_dtypes.bfloat16 else F32)
    io["out"] = nc.dram_tensor("out", [NLAT, D], F32, kind="ExternalOutput").ap()
    k = Kern(nc, io, cfg)
    k.build()
    return nc, k


def kernel(**inputs):
    cfg = {"shapes": {n: list(inputs[n].shape) for n in W_NAMES}}
    env = os.environ
    if env.get("MK_LAYERS"):
        cfg["layers"] = int(env["MK_LAYERS"])
    if env.get("MK_PHASES"):
        cfg["phases"] = env["MK_PHASES"]
    if env.get("MK_DUMP"):
        cfg["dump"] = tuple(env["MK_DUMP"].split(","))
    nc, k = build_program(cfg)
    consts = make_constants()
    shared = {n: np.ascontiguousarray(inputs[n], dtype=np.float32) for n in W_NAMES}
    shared["norm_final"] = np.ascontiguousarray(inputs["norm_final"], dtype=np.float32).reshape(1, D)
    shared["c_ctx"] = np.ascontiguousarray(inputs["c_ctx"], dtype=np.float32).reshape(1, D)
    shared.update(consts)
    ncore = int(env.get("MK_CORES", "8"))
    in_maps = []
    for b in range(ncore):
        m = dict(shared)
        m["x"] = np.ascontiguousarray(inputs["x"][b], dtype=np.float32)
        m["ctx"] = np.ascontiguousarray(inputs["ctx"][b], dtype=np.float32)
        m["c"] = np.ascontiguousarray(inputs["c"][b], dtype=np.float32).reshape(1, D)
        in_maps.append(m)
    res = run_bass_kernel_spmd(nc, in_maps, core_ids=list(range(ncore)))
    out = np.zeros((8, NLAT, D), np.float32)
    for b in range(ncore):
        out[b] = res.results[b]["out"]
    if cfg.get("dump"):
        kernel.last_results = res.results
    return out
```
